# Optimizing a Trainium2 kernel written in Bass

```python
import jax, jax.numpy as jnp
from jax import lax
import numpy as np

D_MODEL = 4096
BATCH = 4
SEQ = 2048
DEPTH = 1

N_META = 16
MIX_WIDTH = D_MODEL
LRU_WIDTH = MIX_WIDTH // 2
LRU_HEADS = 16
LRU_HEAD_DIM = LRU_WIDTH // LRU_HEADS
LRU_CONV_WIDTH = 4
LRU_C = 8.0
CONF_WIDTH = MIX_WIDTH - LRU_WIDTH
CONF_GROUPS = 16
CONF_GROUP_DIM = CONF_WIDTH // CONF_GROUPS
CONF_KERNEL = 31
N_EXPERTS = 32
TOP_K = 4
EXPERT_FF = D_MODEL // 2
SWIGLU_ALPHA = 1.702
SWIGLU_LIMIT = 7.0
MOE_BLOCK = 128
RMS_EPS = 1e-5
LN_EPS = 1e-5

kernel_name = "hybrid_rglru_conformer_moe_encoder"


def rmsnorm(x, g):
    x32 = x.astype(jnp.float32)
    y = x32 * lax.rsqrt(jnp.mean(x32 * x32, axis=-1, keepdims=True) + RMS_EPS)
    return (y * g.astype(jnp.float32)).astype(x.dtype)


def dwconv(x, w, b, pad):
    y = lax.conv_general_dilated(
        x, w[:, None, :].astype(x.dtype), window_strides=(1,), padding=[pad],
        dimension_numbers=("NWC", "WIO", "NWC"), feature_group_count=x.shape[-1])
    return y + b.astype(x.dtype)


def rglru_direction(u_in, conv_w, conv_b, w_a, b_a, w_i, b_i, lam):
    B, S, C = u_in.shape
    u = dwconv(u_in, conv_w, conv_b, (LRU_CONV_WIDTH - 1, 0))
    uh = u.reshape(B, S, LRU_HEADS, LRU_HEAD_DIM).astype(jnp.float32)
    r = jax.nn.sigmoid(jnp.einsum("bshd,hde->bshe", uh, w_a.astype(jnp.float32)) + b_a.astype(jnp.float32)).reshape(B, S, C)
    i = jax.nn.sigmoid(jnp.einsum("bshd,hde->bshe", uh, w_i.astype(jnp.float32)) + b_i.astype(jnp.float32)).reshape(B, S, C)
    log_a = -LRU_C * r * jax.nn.softplus(-lam.astype(jnp.float32))
    a = jnp.exp(log_a)
    bterm = jnp.sqrt(-jnp.expm1(2.0 * log_a)) * (i * u.astype(jnp.float32))

    def step(h, ab):
        a_t, b_t = ab
        h = a_t * h + b_t
        return h, h

    _, hs = lax.scan(step, jnp.zeros((B, C), jnp.float32),
                     (jnp.swapaxes(a, 0, 1), jnp.swapaxes(bterm, 0, 1)))
    return jnp.swapaxes(hs, 0, 1).astype(u_in.dtype)


def group_layernorm(x, g, b):
    B, S, C = x.shape
    x32 = x.astype(jnp.float32).reshape(B, S, CONF_GROUPS, CONF_GROUP_DIM)
    mu = jnp.mean(x32, axis=-1, keepdims=True)
    var = jnp.mean(jnp.square(x32 - mu), axis=-1, keepdims=True)
    y = ((x32 - mu) * lax.rsqrt(var + LN_EPS)).reshape(B, S, C)
    return (y * g.astype(jnp.float32) + b.astype(jnp.float32)).astype(x.dtype)


def moe(x2, w_router, b_router, w_gate, b_gate, w_up, b_up, w_down, b_down):
    T, D = x2.shape
    E, K, BLK = N_EXPERTS, TOP_K, MOE_BLOCK
    A = T * K
    logits = x2.astype(jnp.float32) @ w_router.astype(jnp.float32) + b_router.astype(jnp.float32)
    top_val, top_idx = lax.top_k(logits, K)
    gates = jax.nn.softmax(top_val, axis=-1)
    flat_e = top_idx.reshape(A).astype(jnp.int32)
    flat_tok = jnp.repeat(jnp.arange(T, dtype=jnp.int32), K)
    flat_g = gates.reshape(A)
    order = jnp.argsort(flat_e, stable=True)
    se, stok, sg = flat_e[order], flat_tok[order], flat_g[order]
    counts = jnp.bincount(flat_e, length=E).astype(jnp.int32)
    padded = (counts + BLK - 1) // BLK * BLK
    pend = jnp.cumsum(padded)
    pstart = pend - padded
    ustart = jnp.cumsum(counts) - counts
    dest = pstart[se] + jnp.arange(A, dtype=jnp.int32) - ustart[se]
    NB = -(-A // BLK) + E
    P = NB * BLK
    row_tok = jnp.full((P,), T, jnp.int32).at[dest].set(stok)
    row_g = jnp.zeros((P,), jnp.float32).at[dest].set(sg)
    block_e = jnp.minimum(jnp.searchsorted(pend, jnp.arange(NB, dtype=jnp.int32) * BLK, side="right"), E - 1)
    x_pad = jnp.concatenate([x2, jnp.zeros((1, D), x2.dtype)], axis=0)

    def block_fn(args):
        tok_b, e = args
        xb = x_pad[tok_b]
        hg = jnp.minimum(xb @ w_gate[e] + b_gate[e], SWIGLU_LIMIT)
        hu = jnp.clip(xb @ w_up[e] + b_up[e], -SWIGLU_LIMIT, SWIGLU_LIMIT)
        act = hg * jax.nn.sigmoid(SWIGLU_ALPHA * hg) * (hu + 1.0)
        return act @ w_down[e] + b_down[e]

    y_rows = lax.map(block_fn, (row_tok.reshape(NB, BLK), block_e)).reshape(P, D)
    y_rows = y_rows * row_g[:, None].astype(y_rows.dtype)
    return jax.ops.segment_sum(y_rows, row_tok, num_segments=T + 1)[:T]


def setup_inputs(seed: int = 0) -> dict:
    key = jax.random.key(seed)
    ks = jax.random.split(key, 32)
    L, D, F, E = DEPTH, D_MODEL, EXPERT_FF, N_EXPERTS
    n_in = 2 * LRU_WIDTH + 2 * CONF_WIDTH
    nrm = lambda k, shape, s: jax.random.normal(k, shape, jnp.float32) * s
    u = jax.random.uniform(ks[10], (L, 2, LRU_WIDTH), jnp.float32, 0.9, 0.999)
    s = u ** (1.0 / LRU_C)
    return {
        "x": nrm(ks[0], (BATCH, SEQ, D), 1.0),
        "meta_tokens": nrm(ks[1], (N_META, D), 1.0),
        "norm1_g": 1.0 + nrm(ks[2], (L, D), 0.02),
        "w_in": nrm(ks[3], (L, D, n_in), D ** -0.5),
        "lru_conv_w": nrm(ks[4], (L, 2, LRU_CONV_WIDTH, LRU_WIDTH), LRU_CONV_WIDTH ** -0.5),
        "lru_conv_b": nrm(ks[5], (L, 2, LRU_WIDTH), 0.01),
        "lru_w_a": nrm(ks[6], (L, 2, LRU_HEADS, LRU_HEAD_DIM, LRU_HEAD_DIM), LRU_HEAD_DIM ** -0.5),
        "lru_b_a": nrm(ks[7], (L, 2, LRU_HEADS, LRU_HEAD_DIM), 0.01),
        "lru_w_i": nrm(ks[8], (L, 2, LRU_HEADS, LRU_HEAD_DIM, LRU_HEAD_DIM), LRU_HEAD_DIM ** -0.5),
        "lru_b_i": nrm(ks[9], (L, 2, LRU_HEADS, LRU_HEAD_DIM), 0.01),
        "lru_lambda": jnp.log(s) - jnp.log1p(-s),
        "conf_conv_w": nrm(ks[11], (L, CONF_KERNEL, CONF_WIDTH), CONF_KERNEL ** -0.5),
        "conf_conv_b": nrm(ks[12], (L, CONF_WIDTH), 0.01),
        "conf_norm_g": 1.0 + nrm(ks[13], (L, CONF_WIDTH), 0.02),
        "conf_norm_b": nrm(ks[14], (L, CONF_WIDTH), 0.01),
        "w_out": nrm(ks[15], (L, MIX_WIDTH, D), MIX_WIDTH ** -0.5),
        "norm2_g": 1.0 + nrm(ks[16], (L, D), 0.02),
        "w_router": nrm(ks[17], (L, D, E), D ** -0.5),
        "b_router": nrm(ks[18], (L, E), 0.01),
        "w_gate": nrm(ks[19], (L, E, D, F), D ** -0.5),
        "b_gate": nrm(ks[20], (L, E, F), 0.01),
        "w_up": nrm(ks[21], (L, E, D, F), D ** -0.5),
        "b_up": nrm(ks[22], (L, E, F), 0.01),
        "w_down": nrm(ks[23], (L, E, F, D), F ** -0.5),
        "b_down": nrm(ks[24], (L, E, D), 0.01),
        "final_norm_g": 1.0 + nrm(ks[25], (D,), 0.02),
    }


def reference(x, meta_tokens, norm1_g, w_in, lru_conv_w, lru_conv_b, lru_w_a, lru_b_a,
              lru_w_i, lru_b_i, lru_lambda, conf_conv_w, conf_conv_b, conf_norm_g,
              conf_norm_b, w_out, norm2_g, w_router, b_router, w_gate, b_gate, w_up,
              b_up, w_down, b_down, final_norm_g):
    B, S, D = x.shape
    meta = jnp.broadcast_to(meta_tokens.astype(x.dtype)[None], (B, N_META, D))
    x = jnp.concatenate([meta, x], axis=1)
    St = S + N_META
    for l in range(DEPTH):
        h = rmsnorm(x, norm1_g[l])
        z = h @ w_in[l]
        lru_x, lru_gate, conf_a, conf_b = jnp.split(
            z, [LRU_WIDTH, 2 * LRU_WIDTH, 2 * LRU_WIDTH + CONF_WIDTH], axis=-1)
        h_fwd = rglru_direction(lru_x, lru_conv_w[l, 0], lru_conv_b[l, 0], lru_w_a[l, 0],
                                lru_b_a[l, 0], lru_w_i[l, 0], lru_b_i[l, 0], lru_lambda[l, 0])
        h_bwd = rglru_direction(lru_x[:, ::-1], lru_conv_w[l, 1], lru_conv_b[l, 1], lru_w_a[l, 1],
                                lru_b_a[l, 1], lru_w_i[l, 1], lru_b_i[l, 1], lru_lambda[l, 1])[:, ::-1]
        y_lru = (h_fwd + h_bwd) * jax.nn.gelu(lru_gate)
        c = conf_a * jax.nn.sigmoid(conf_b)
        c = dwconv(c, conf_conv_w[l], conf_conv_b[l], (CONF_KERNEL // 2, CONF_KERNEL // 2))
        c = jax.nn.silu(group_layernorm(c, conf_norm_g[l], conf_norm_b[l]))
        x = x + jnp.concatenate([y_lru, c], axis=-1) @ w_out[l]
        h2 = rmsnorm(x, norm2_g[l]).reshape(B * St, D)
        y_moe = moe(h2, w_router[l], b_router[l], w_gate[l], b_gate[l], w_up[l], b_up[l],
                    w_down[l], b_down[l])
        x = x + y_moe.reshape(B, St, D)
    y = rmsnorm(x, final_norm_g)
    return y[:, N_META:]
```

```python
import numpy as np
import concourse.bass as bass
import concourse.mybir as mybir
from concourse.bass_utils import run_bass_kernel_spmd

F32 = mybir.dt.float32
BF16 = mybir.dt.bfloat16
I32 = mybir.dt.int32
AF = mybir.ActivationFunctionType
ALU = mybir.AluOpType
AX = mybir.AxisListType

REAL_CFG = dict(D=4096, SEQ=2048, B=4, E=32, K=4, CAP=256, HALO=16, NMETA=16)


class _Op:
    __slots__ = ("eng", "fn", "deps", "signal", "sigval", "dma", "dsem", "dtarget", "idx")

    def __init__(self, eng, fn, dma):
        self.eng = eng
        self.fn = fn
        self.deps = []
        self.signal = False
        self.sigval = 0
        self.dma = dma
        self.dsem = None
        self.dtarget = 0


class Prog:
    ENGS = ("pe", "act", "dve", "pool", "sp")
    NDSEM = 8

    def __init__(self):
        self.ops = {e: [] for e in self.ENGS}
        self.last_write = {}
        self.readers = {}
        self.dma_rr = {e: 0 for e in self.ENGS}
        self.dma_last = {}
        self.dma_count = {}
        self.n = 0

    def op(self, eng, fn, reads=(), writes=(), dma=False):
        o = _Op(eng, fn, dma)
        o.idx = self.n
        self.n += 1
        deps = {}
        for k in reads:
            lw = self.last_write.get(k)
            if lw is not None:
                deps[id(lw)] = lw
        for k in writes:
            lw = self.last_write.get(k)
            if lw is not None:
                deps[id(lw)] = lw
            for r in self.readers.get(k, {}).values():
                deps[id(r)] = r
        rkey = eng
        if dma:
            i = self.dma_rr[eng]
            self.dma_rr[eng] = (i + 1) % self.NDSEM
            o.dsem = (eng, i)
            prev = self.dma_last.get(o.dsem)
            if prev is not None:
                deps[id(prev)] = prev
            self.dma_last[o.dsem] = o
            c = self.dma_count.get(o.dsem, 0) + 1
            self.dma_count[o.dsem] = c
            o.dtarget = 16 * c
            rkey = o.dsem
        for d in deps.values():
            if d is o:
                continue
            if (not d.dma) and d.eng == "pe" and eng == "pe" and not dma:
                continue
            o.deps.append(d)
        for k in reads:
            self.readers.setdefault(k, {})[rkey] = o
        for k in writes:
            self.last_write[k] = o
            self.readers[k] = {}
        self.ops[eng].append(o)
        return o

    def barrier(self):
        lasts = []
        for e in self.ENGS:
            for o in reversed(self.ops[e]):
                if not o.dma and o.fn is not None:
                    lasts.append(o)
                    break
        lasts += list(self.dma_last.values())
        for e in self.ENGS:
            o = _Op(e, None, False)
            o.idx = self.n
            self.n += 1
            o.deps = [d for d in lasts if not (d.eng == e and not d.dma and e == "pe")]
            self.ops[e].append(o)

    def emit(self, nc):
        for e in self.ENGS:
            for o in self.ops[e]:
                for d in o.deps:
                    d.signal = True
        for e in self.ENGS:
            c = 0
            for o in self.ops[e]:
                if o.signal and not o.dma:
                    c += 1
                    o.sigval = c
        from contextlib import ExitStack
        with ExitStack() as st:
            esem = {e: st.enter_context(nc.semaphore("es_" + e)) for e in self.ENGS}
            dsem = {}
            for e in self.ENGS:
                if self.dma_count and any(k[0] == e for k in self.dma_count):
                    for i in range(self.NDSEM):
                        dsem[(e, i)] = st.enter_context(nc.semaphore("ds_%s%d" % (e, i)))
            block = st.enter_context(nc.Block())

            def run(eng_name, e):
                waited = {}
                for o in self.ops[eng_name]:
                    for d in o.deps:
                        if d.dma:
                            sem, val, key = dsem[d.dsem], d.dtarget, d.dsem
                        else:
                            sem, val, key = esem[d.eng], d.sigval, d.eng
                        if waited.get(key, 0) < val:
                            e.wait_ge(sem, val)
                            waited[key] = val
                    if o.fn is None:
                        continue
                    inst = o.fn(e)
                    if o.dma:
                        inst.then_inc(dsem[o.dsem], 16)
                    elif o.signal:
                        inst.then_inc(esem[eng_name], 1)

            @block.tensor
            def _(e):
                run("pe", e)

            @block.scalar
            def _(e):
                run("act", e)

            @block.vector
            def _(e):
                run("dve", e)

            @block.gpsimd
            def _(e):
                run("pool", e)

            @block.sync
            def _(e):
                run("sp", e)


class Arena:
    def __init__(self, nc, lo=16512, hi=229344):
        self.nc = nc
        self.lo = lo
        self.hi = hi
        self.cur = lo
        self.cnt = 0

    def alloc(self, shape, dtype, name="t"):
        sz = 4 if dtype in (F32, I32) else 2
        n = 1
        for s in shape[1:]:
            n *= s
        nbytes = (n * sz + 63) // 64 * 64
        off = self.cur
        assert off + nbytes <= self.hi, "SBUF arena overflow: %s %s need %d at %d" % (name, shape, nbytes, off)
        self.cur += nbytes
        self.cnt += 1
        return self.nc.alloc_sbuf_tensor_at("%s_%d" % (name, self.cnt), list(shape), dtype, offset=off)

    def mark(self):
        return self.cur

    def release(self, m):
        self.cur = m


def _chunks(total, size=128):
    out = []
    s = 0
    while s < total:
        n = min(size, total - s)
        out.append((s, n))
        s += n
    return out


def build_program(cfg, debug=False):
    D = cfg["D"]; SEQ = cfg["SEQ"]; E = cfg["E"]; K = cfg["K"]; CAP = cfg["CAP"]; HALO = cfg["HALO"]
    ST = SEQ + cfg["NMETA"]
    TH = ST // 2
    LW = D // 2; CW = D // 2; FF = D // 2
    NH = LW // 128; NG = CW // 128; DC = D // 128; FC = FF // 128; CC = CAP // 128
    CK = 31
    TW = HALO + TH
    DB = D // 512
    FB = FF // 256
    tch = _chunks(TH)
    NCH = len(tch)
    EPS = 1e-5

    nc = bass.Bass("TRN2", target_bir_lowering=False)
    P = Prog()
    A = Arena(nc)

    STOP = cfg.get("STOP", "")
    LVL = cfg.get("LVL", 9)

    def finish():
        P.barrier()
        P.emit(nc)
        return nc

    def din(name, shape, dt=F32):
        return nc.dram_tensor(name, list(shape), dt, kind="ExternalInput").ap()

    xo_d = din("xo", [TH, D])
    xn_d = din("xn", [TW, D])
    NSP = (2 * DC + 2 * (NH * 4 + 4 * NH) + NG * CK + 3 * NG + 2 * E * FC)
    sp_d = din("smallp", [128, NSP])
    NCST = 128 * 4 + CAP + 2 * E
    cst_d = din("consts", [128, NCST])
    g1b_d = din("g1b", [128, D])
    gfb_d = din("gfb", [128, D])
    NU = 4 * LW // 128
    win_d = din("w_in", [NU, 128, DC * 128])
    wa_d = [din("wa_f", [NH * 128, 128]), din("wa_b", [NH * 128, 128])]
    wi_d = [din("wi_f", [NH * 128, 128]), din("wi_b", [NH * 128, 128])]
    wout_d = din("w_out", [D // 512, 128, 2 * NH * 512])
    wr_d = din("w_router", [D, E])
    wg_d = din("w_gate", [E * (FF // 256), 128, (D // 128) * 256])
    wu_d = din("w_up", [E * (FF // 256), 128, (D // 128) * 256])
    wd_d = din("w_down", [E * (D // 512), 128, (FF // 128) * 512])
    bd_d = din("b_down", [E, D])
    out_d = nc.dram_tensor("out", [TH, D], F32, kind="ExternalOutput").ap()
    skind = "ExternalOutput" if debug else "Internal"
    yT_d = nc.dram_tensor("yT_scr", [2 * NH, 128, TH], BF16, kind=skind).ap()
    x1_d = nc.dram_tensor("x1_scr", [TH, D], F32, kind=skind).ap()
    Y_d = nc.dram_tensor("Y_scr", [E * CAP, D], F32, kind=skind).ap()
    NPRE = cfg.get("NPRE", 5)
    wgb_d = nc.dram_tensor("wg_bf", [NPRE * (FF // 256), 128, (D // 128) * 256], BF16, kind="Internal").ap()
    wub_d = nc.dram_tensor("wu_bf", [NPRE * (FF // 256), 128, (D // 128) * 256], BF16, kind="Internal").ap()
    wdb_d = nc.dram_tensor("wd_bf", [NPRE * (D // 512), 128, (FF // 128) * 512], BF16, kind="Internal").ap()
    pre_list = []
    for e_ in range(NPRE):
        for fb in range(FF // 256):
            pre_list.append(("g", e_ * (FF // 256) + fb))
            pre_list.append(("u", e_ * (FF // 256) + fb))
        for db in range(D // 512):
            pre_list.append(("d", e_ * (D // 512) + db))
    pre_pos = [0]

    def pre_issue(n):
        for _ in range(n):
            if pre_pos[0] >= len(pre_list):
                return
            kind, idx = pre_list[pre_pos[0]]
            pre_pos[0] += 1
            src = {"g": wg_d, "u": wu_d, "d": wd_d}[kind][idx]
            dst = {"g": wgb_d, "u": wub_d, "d": wdb_d}[kind][idx]
            P.op("pool", lambda e, src=src, dst=dst: e.dma_start(out=dst, in_=src), writes=[("pre", kind, idx)],
                 dma=True)

    if debug:
        dbg_lg = nc.dram_tensor("dbg_logits", [TH, E], F32, kind="ExternalOutput").ap()
        dbg_G = nc.dram_tensor("dbg_G", [TH, E], F32, kind="ExternalOutput").ap()
        dbg_idx = nc.dram_tensor("dbg_idx", [TH, K], I32, kind="ExternalOutput").ap()
        dbg_st = nc.dram_tensor("dbg_state", [128, NH], F32, kind="ExternalOutput").ap()

    PS = [nc.alloc_psum_tensor("ps%d" % i, [128, 512], F32) for i in range(8)]
    ps_rr = [0]

    def bank():
        i = ps_rr[0]
        ps_rr[0] = (i + 1) % 8
        return i

    smallp = A.alloc([128, NSP], F32, "smallp")
    cst = A.alloc([128, NCST], F32, "consts")
    cbf = A.alloc([128, 3 * 128], BF16, "cbf")
    stA = A.alloc([128, NH], F32, "stateA")
    cA = A.alloc([128, 2, 2 * NH], F32, "cA")
    P.op("sp", lambda e: e.dma_start(out=smallp[:], in_=sp_d), writes=["smallp"], dma=True)
    P.op("sp", lambda e: e.dma_start(out=cst[:], in_=cst_d), writes=["cst"], dma=True)
    ident = cst[:, 0:128]
    Utri = cst[:, 128:256]
    onesM = cst[:, 256:384]
    ones1 = cst[:, 384:512]
    iotaC = cst[:, 512:512 + CAP]
    eoff = cst[:, 512 + CAP:512 + CAP + E]
    brt = cst[:, 512 + CAP + E:512 + CAP + 2 * E]
    P.op("dve", lambda e: e.tensor_copy(out=cbf[:, 0:128], in_=ident), reads=["cst"], writes=["cbf"])
    P.op("dve", lambda e: e.tensor_copy(out=cbf[:, 128:256], in_=Utri), reads=["cst"], writes=["cbf"])
    P.op("dve", lambda e: e.tensor_copy(out=cbf[:, 256:384], in_=ones1), reads=["cst"], writes=["cbf"])
    identb = cbf[:, 0:128]
    Ub = cbf[:, 128:256]
    onesb = cbf[:, 256:384]

    o = 0
    O_G1 = o; o += DC
    O_G2 = o; o += DC
    O_DIR = []
    for _ in range(2):
        d = {}
        d["cw"] = o; o += NH * 4
        d["cb"] = o; o += NH
        d["ba"] = o; o += NH
        d["bi"] = o; o += NH
        d["lam"] = o; o += NH
        O_DIR.append(d)
    O_CW = o; o += NG * CK
    O_CB = o; o += NG
    O_CG = o; o += NG
    O_CBETA = o; o += NG
    O_BG = o; o += E * FC
    O_BU = o; o += E * FC
    assert o == NSP

    def spc(off, n=1):
        return smallp[:, off:off + n]

    for dr in range(2):
        lam = spc(O_DIR[dr]["lam"], NH)
        P.op("act", lambda e, dr=dr, lam=lam: e.activation(out=cA[:, dr, 0:NH], in_=lam, func=AF.Exp, scale=-1.0),
             reads=["smallp"], writes=[("cA", dr)])
        P.op("act", lambda e, dr=dr: e.activation(out=cA[:, dr, 0:NH], in_=cA[:, dr, 0:NH], func=AF.Ln, bias=1.0),
             reads=[("cA", dr)], writes=[("cA", dr)])
        P.op("dve", lambda e, dr=dr: e.tensor_scalar(out=cA[:, dr, NH:2 * NH], in0=cA[:, dr, 0:NH], scalar1=-16.0,
                                                     scalar2=None, op0=ALU.mult),
             reads=[("cA", dr)], writes=[("cA2", dr)])
        P.op("dve", lambda e, dr=dr: e.tensor_scalar(out=cA[:, dr, 0:NH], in0=cA[:, dr, 0:NH], scalar1=-8.0,
                                                     scalar2=None, op0=ALU.mult),
             reads=[("cA", dr), ("cA2", dr)], writes=[("cA", dr)])

    m_persist = A.mark()

    hT = A.alloc([128, DC, TW], BF16, "hT")
    RING_IN = 4
    win_t = [A.alloc([128, DC, 128], BF16, "win") for _ in range(RING_IN)]
    wgt = [[A.alloc([128, NH, 128], BF16, "wa"), A.alloc([128, NH, 128], BF16, "wi")] for _ in range(2)]
    for dr in range(2):
        P.op("pool", lambda e, dr=dr: e.dma_start(out=wgt[dr][0][:], in_=wa_d[dr].rearrange("(h d) c -> d h c", d=128)),
             writes=[("wgt", dr, 0)], dma=True)
        P.op("pool", lambda e, dr=dr: e.dma_start(out=wgt[dr][1][:], in_=wi_d[dr].rearrange("(h d) c -> d h c", d=128)),
             writes=[("wgt", dr, 1)], dma=True)
    m_over = A.mark()

    win_rr = [0]

    def load_win(col0):
        s = win_rr[0]
        win_rr[0] = (s + 1) % RING_IN
        src = win_d[col0 // 128]
        dst = win_t[s][:, :, :].rearrange("p k c -> p (k c)")
        P.op("pool", lambda e: e.dma_start(out=dst, in_=src), writes=[("win", s)], dma=True)
        return s

    def norm_transpose(src_d, rows, col_off, xt, g1b, ssq, rstd, diag, junk):
        for ci, (r0, n) in enumerate(rows):
            b = ci % 2
            P.op("sp", lambda e, b=b, r0=r0, n=n: e.dma_start(out=xt[b][0:n, :], in_=src_d[r0:r0 + n, :]),
                 writes=[("xt", b)], dma=True)
            P.op("pool", lambda e, b=b: e.memset(ssq[b][:, :], 0.0), writes=[("ssq", b)])
            P.op("act", lambda e, b=b, n=n: e.activation(out=junk[0:n, :], in_=xt[b][0:n, :], func=AF.Square,
                                                       accum_out=ssq[b][0:n, :]),
                 reads=[("xt", b)], writes=[("ssq", b), "junk"])
            P.op("dve", lambda e, b=b, n=n: e.tensor_scalar(out=rstd[b][0:n, :], in0=ssq[b][0:n, :], scalar1=1.0 / D,
                                                          scalar2=EPS, op0=ALU.mult, op1=ALU.add),
                 reads=[("ssq", b)], writes=[("rstd", b)])
            P.op("act", lambda e, b=b, n=n: e.activation(out=rstd[b][0:n, :], in_=rstd[b][0:n, :], func=AF.Ln),
                 reads=[("rstd", b)], writes=[("rstd", b)])
            P.op("act", lambda e, b=b, n=n: e.activation(out=rstd[b][0:n, :], in_=rstd[b][0:n, :], func=AF.Exp,
                                                       scale=-0.5),
                 reads=[("rstd", b)], writes=[("rstd", b)])
            P.op("dve", lambda e, b=b, n=n: e.tensor_scalar(out=diag[b][0:n, 0:n], in0=ident[0:n, 0:n],
                                                          scalar1=rstd[b][0:n, :], scalar2=None, op0=ALU.mult),
                 reads=[("rstd", b), "cst"], writes=[("diag", b)])
            P.op("dve", lambda e, b=b, n=n: e.tensor_tensor(out=xt[b][0:n, :], in0=xt[b][0:n, :], in1=g1b[0:n, :],
                                                          op=ALU.mult),
                 reads=[("xt", b), "g1b"], writes=[("xt", b)])
            for q in range(DC // 4):
                bk = bank()
                for j in range(4):
                    dc = q * 4 + j
                    P.op("pe", lambda e, b=b, n=n, dc=dc, bk=bk, j=j: e.matmul(
                        PS[bk][:, j * 128:j * 128 + n], lhsT=xt[b][0:n, dc * 128:(dc + 1) * 128],
                        rhs=diag[b][0:n, 0:n], start=True, stop=True),
                        reads=[("xt", b), ("diag", b)], writes=[("ps", bk)])
                src = PS[bk][:, :].rearrange("p (j t) -> p j t", j=4)[:, :, 0:n]
                dst = hT[:, q * 4:q * 4 + 4, col_off + r0:col_off + r0 + n]
                if q % 2 == 0:
                    P.op("act", lambda e, src=src, dst=dst: e.activation(out=dst, in_=src, func=AF.Copy),
                         reads=[("ps", bk)], writes=[("hT", q)])
                else:
                    P.op("dve", lambda e, src=src, dst=dst: e.tensor_copy(out=dst, in_=src),
                         reads=[("ps", bk)], writes=[("hT", q)])

    def zmm(slot, c0, ncols):
        res = []
        for (t0, n) in _chunks(ncols, 512):
            bk = bank()
            for dc in range(DC):
                P.op("pe", lambda e, bk=bk, dc=dc, t0=t0, n=n: e.matmul(
                    PS[bk][:, 0:n], lhsT=win_t[slot][:, dc, :], rhs=hT[:, dc, c0 + t0:c0 + t0 + n],
                    start=(dc == 0), stop=(dc == DC - 1)),
                    reads=[("win", slot), ("hT", dc // 4)], writes=[("ps", bk)])
            res.append((bk, t0, n))
        return res

    def lru_dir(W, h, dr, xs, base, nT, sign, init, hout, rev, hkey):
        od = O_DIR[dr]
        u, ub, r, ig, a, q = W["u"], W["ub"], W["r"], W["i"], W["a"], W["q"]
        for k in range(4):
            off = base + (k if sign > 0 else -k)
            src = xs[:, off:off + nT]
            wk = spc(od["cw"] + h * 4 + k)
            if k == 0:
                P.op("dve", lambda e, src=src, wk=wk: e.tensor_scalar(
                    out=u[:, 0:nT], in0=src, scalar1=wk, scalar2=spc(od["cb"] + h), op0=ALU.mult, op1=ALU.add),
                    reads=["xs", "smallp"], writes=["u"])
            else:
                P.op("dve", lambda e, src=src, wk=wk: e.scalar_tensor_tensor(
                    out=u[:, 0:nT], in0=src, scalar=wk, in1=u[:, 0:nT], op0=ALU.mult, op1=ALU.add),
                    reads=["xs", "u", "smallp"], writes=["u"])
        P.op("act", lambda e: e.activation(out=ub[:, 0:nT], in_=u[:, 0:nT], func=AF.Copy), reads=["u"], writes=["ub"])
        for gi, (dst, bo) in enumerate(((r, od["ba"]), (ig, od["bi"]))):
            for (t0, n) in _chunks(nT, 512):
                bk = bank()
                P.op("pe", lambda e, bk=bk, t0=t0, n=n, gi=gi: e.matmul(
                    PS[bk][:, 0:n], lhsT=wgt[dr][gi][:, h, :], rhs=ub[:, t0:t0 + n], start=True, stop=True),
                    reads=["ub", ("wgt", dr, gi)], writes=[("ps", bk)])
                P.op("act", lambda e, bk=bk, t0=t0, n=n, dst=dst, bo=bo: e.activation(
                    out=dst[:, t0:t0 + n], in_=PS[bk][:, 0:n], func=AF.Sigmoid, bias=spc(bo + h)),
                    reads=[("ps", bk), "smallp"], writes=["r" if gi == 0 else "i"])
        P.op("act", lambda e: e.activation(out=a[:, 0:nT], in_=r[:, 0:nT], func=AF.Exp, scale=cA[:, dr, h:h + 1]),
             reads=["r", ("cA", dr)], writes=["a"])
        P.op("act", lambda e: e.activation(out=q[:, 0:nT], in_=r[:, 0:nT], func=AF.Exp,
                                           scale=cA[:, dr, NH + h:NH + h + 1]),
             reads=["r", ("cA2", dr)], writes=["q"])
        P.op("act", lambda e: e.activation(out=q[:, 0:nT], in_=q[:, 0:nT], func=AF.Sqrt, scale=-1.0, bias=1.0),
             reads=["q"], writes=["q"])
        P.op("pool", lambda e: e.tensor_tensor(out=ig[:, 0:nT], in0=ig[:, 0:nT], in1=u[:, 0:nT], op=ALU.mult),
             reads=["i", "u"], writes=["i"])
        P.op("dve", lambda e: e.tensor_tensor(out=q[:, 0:nT], in0=q[:, 0:nT], in1=ig[:, 0:nT], op=ALU.mult),
             reads=["q", "i"], writes=["q"])
        if rev:
            P.op("dve", lambda e: e.tensor_tensor_scan(out=hout[:, 0:nT][:, ::-1],
                                                       data0=a[:, 0:nT][:, ::-1], data1=q[:, 0:nT][:, ::-1],
                                                       initial=init, op0=ALU.mult, op1=ALU.add),
                 reads=["a", "q"], writes=[hkey])
        else:
            P.op("dve", lambda e: e.tensor_tensor_scan(out=hout[:, 0:nT], data0=a[:, 0:nT], data1=q[:, 0:nT],
                                                       initial=init, op0=ALU.mult, op1=ALU.add),
                 reads=["a", "q", "stA"], writes=[hkey])

    xt = [A.alloc([128, D], F32, "xt") for _ in range(2)]
    g1b = A.alloc([128, D], F32, "g1b")
    junk = A.alloc([128, D], F32, "junk")
    ssq = [A.alloc([128, 1], F32, "ssq") for _ in range(2)]
    rstd = [A.alloc([128, 1], F32, "rstd") for _ in range(2)]
    diag = [A.alloc([128, 128], F32, "diag") for _ in range(2)]
    P.op("sp", lambda e: e.dma_start(out=g1b[:], in_=g1b_d), writes=["g1b"], dma=True)
    norm_transpose(xo_d, tch, 0, xt, g1b, ssq, rstd, diag, junk)
    A.release(m_over)
    P.barrier()

    WB = {}
    XSW = TW + 4
    for nm in ("xs", "u", "r", "i", "a", "q", "hf", "hb", "g1", "g2"):
        WB[nm] = A.alloc([128, XSW], F32, nm)
    WB["ub"] = A.alloc([128, XSW], BF16, "ub")
    ysp = [A.alloc([128, TH], BF16, "ysp") for _ in range(2)]
    cbuf = A.alloc([128, TW + 16], BF16, "cbuf")
    dgt = A.alloc([128, CK, 128], BF16, "dgt")
    xs = WB["xs"]
    P.op("pool", lambda e: e.memset(xs[:, :], 0.0), writes=["xs"])
    P.op("pool", lambda e: e.memset(cbuf[:, :], 0.0), writes=["cbuf"])

    pending = [load_win(h * 128) for h in range(min(2, NH))]
    for h in range(NH):
        slot = pending.pop(0)
        if h + 2 < NH:
            pending.append(load_win((h + 2) * 128))
        zs = zmm(slot, 0, TH)
        for (bk, t0, n) in zs:
            P.op("act", lambda e, bk=bk, t0=t0, n=n: e.activation(out=xs[:, 3 + t0:3 + t0 + n], in_=PS[bk][:, 0:n],
                                                                  func=AF.Copy),
                 reads=[("ps", bk)], writes=["xs"])
        pre_issue(cfg.get('PRE_PER', 3))
        lru_dir(WB, h, 0, xs, 0, TH, +1, 0.0, WB["hf"], False, "hf")
        P.op("act", lambda e, h=h: e.activation(out=stA[:, h:h + 1], in_=WB["hf"][:, TH - 1:TH], func=AF.Copy),
             reads=["hf"], writes=["stA"])
    if debug:
        P.op("sp", lambda e: e.dma_start(out=dbg_st, in_=stA[:]), reads=["stA"], dma=True)
    P.barrier()

    if STOP == "A2":
        return finish()
    m_work = A.mark()
    A.release(m_over)
    xt = [A.alloc([128, D], F32, "xt") for _ in range(2)]
    g1b2 = A.alloc([128, D], F32, "g1b")
    junk = A.alloc([128, D], F32, "junk")
    ssq = [A.alloc([128, 1], F32, "ssq") for _ in range(2)]
    rstd = [A.alloc([128, 1], F32, "rstd") for _ in range(2)]
    diag = [A.alloc([128, 128], F32, "diag") for _ in range(2)]
    P.op("sp", lambda e: e.dma_start(out=g1b2[:], in_=g1b_d), writes=["g1b"], dma=True)
    rowsB = [(0, HALO)] + [(HALO + r0, n) for (r0, n) in tch]
    norm_transpose(xn_d, rowsB, 0, xt, g1b2, ssq, rstd, diag, junk)
    P.barrier()
    A.release(m_work)
    P.op("pool", lambda e: e.memset(xs[:, :], 0.0), writes=["xs"])
    P.op("pool", lambda e: e.memset(cbuf[:, :], 0.0), writes=["cbuf"])

    units = []
    for h in range(NH):
        units.append(("x", h, h * 128))
        units.append(("g", h, LW + h * 128))
    for g in range(NG):
        units.append(("a", g, 2 * LW + g * 128))
        units.append(("b", g, 2 * LW + CW + g * 128))
    LOOK = 3
    loaded = {}
    nxt = [0]

    def ensure(i):
        while nxt[0] < len(units) and nxt[0] <= i + LOOK - 1:
            loaded[nxt[0]] = load_win(units[nxt[0]][2])
            nxt[0] += 1
        return loaded[i]

    ui = 0
    for h in range(NH):
        sx = ensure(ui); ui += 1
        zs = zmm(sx, 0, TW)
        for (bk, t0, n) in zs:
            P.op("act", lambda e, bk=bk, t0=t0, n=n: e.activation(out=xs[:, t0:t0 + n], in_=PS[bk][:, 0:n],
                                                                  func=AF.Copy),
                 reads=[("ps", bk)], writes=["xs"])
        pre_issue(cfg.get('PRE_PER', 3))
        lru_dir(WB, h, 0, xs, HALO - 3, TH, +1, stA[:, h:h + 1], WB["hf"], False, "hf")
        lru_dir(WB, h, 1, xs, HALO + 3, TH, -1, 0.0, WB["hb"], True, "hb")
        sg = ensure(ui); ui += 1
        zg = zmm(sg, HALO, TH)
        g1t, g2t = WB["g1"], WB["g2"]
        for (bk, t0, n) in zg:
            P.op("act", lambda e, bk=bk, t0=t0, n=n: e.activation(out=g1t[:, t0:t0 + n], in_=PS[bk][:, 0:n],
                                                                  func=AF.Copy),
                 reads=[("ps", bk)], writes=["g1t"])
        P.op("pool", lambda e: e.tensor_tensor(out=g2t[:, 0:TH], in0=g1t[:, 0:TH], in1=g1t[:, 0:TH], op=ALU.mult),
             reads=["g1t"], writes=["g2t"])
        P.op("dve", lambda e: e.tensor_scalar(out=g2t[:, 0:TH], in0=g2t[:, 0:TH], scalar1=0.044715, scalar2=1.0,
                                              op0=ALU.mult, op1=ALU.add), reads=["g2t"], writes=["g2t"])
        P.op("pool", lambda e: e.tensor_tensor(out=g2t[:, 0:TH], in0=g2t[:, 0:TH], in1=g1t[:, 0:TH], op=ALU.mult),
             reads=["g1t", "g2t"], writes=["g2t"])
        P.op("act", lambda e: e.activation(out=g2t[:, 0:TH], in_=g2t[:, 0:TH], func=AF.Sigmoid, scale=1.5957691216),
             reads=["g2t"], writes=["g2t"])
        P.op("dve", lambda e: e.tensor_tensor(out=g2t[:, 0:TH], in0=g2t[:, 0:TH], in1=g1t[:, 0:TH], op=ALU.mult),
             reads=["g1t", "g2t"], writes=["g2t"])
        hf, hb = WB["hf"], WB["hb"]
        P.op("pool", lambda e: e.tensor_tensor(out=hf[:, 0:TH], in0=hf[:, 0:TH], in1=hb[:, 0:TH], op=ALU.add),
             reads=["hf", "hb"], writes=["hf"])
        yb = h % 2
        P.op("dve", lambda e, yb=yb: e.tensor_tensor(out=ysp[yb][:, :], in0=hf[:, 0:TH], in1=g2t[:, 0:TH], op=ALU.mult),
             reads=["hf", "g2t"], writes=[("ysp", yb)])
        P.op("sp", lambda e, yb=yb, h=h: e.dma_start(out=yT_d[h], in_=ysp[yb][:, :]), reads=[("ysp", yb)], dma=True)

    if STOP == "B2":
        return finish()
    co, xc, sq = WB["u"], WB["r"], WB["i"]
    for g in range(NG):
        sa = ensure(ui); ui += 1
        sb = ensure(ui); ui += 1
        pre_issue(cfg.get('PRE_PER', 3))
        za = zmm(sa, 0, TW)
        zb = zmm(sb, 0, TW)
        sgt = WB["a"]
        for (bk, t0, n) in zb:
            P.op("act", lambda e, bk=bk, t0=t0, n=n: e.activation(out=sgt[:, t0:t0 + n], in_=PS[bk][:, 0:n],
                                                                  func=AF.Sigmoid),
                 reads=[("ps", bk)], writes=["sgt"])
        for (bk, t0, n) in za:
            P.op("dve", lambda e, bk=bk, t0=t0, n=n: e.tensor_tensor(out=cbuf[:, t0:t0 + n], in0=sgt[:, t0:t0 + n],
                                                                    in1=PS[bk][:, 0:n], op=ALU.mult),
                 reads=[("ps", bk), "sgt"], writes=["cbuf"])
        for k in range(CK):
            P.op("dve", lambda e, k=k, g=g: e.tensor_scalar(out=dgt[:, k, :], in0=ident, scalar1=spc(O_CW + g * CK + k),
                                                           scalar2=None, op0=ALU.mult),
                 reads=["cst", "smallp"], writes=["dgt"])
        cz = []
        for (t0, n) in _chunks(TH, 512):
            bk = bank()
            for k in range(CK):
                P.op("pe", lambda e, bk=bk, k=k, t0=t0, n=n: e.matmul(
                    PS[bk][:, 0:n], lhsT=dgt[:, k, :], rhs=cbuf[:, HALO - 15 + k + t0:HALO - 15 + k + t0 + n],
                    start=(k == 0), stop=(k == CK - 1)), reads=["dgt", "cbuf"], writes=[("ps", bk)])
            cz.append((bk, t0, n))
        for (bk, t0, n) in cz:
            P.op("act", lambda e, bk=bk, t0=t0, n=n, g=g: e.activation(out=co[:, t0:t0 + n], in_=PS[bk][:, 0:n],
                                                                       func=AF.Identity, bias=spc(O_CB + g)),
                 reads=[("ps", bk), "smallp"], writes=["co"])
        for (t0, n) in _chunks(TH, 512):
            bk = bank()
            P.op("pe", lambda e, bk=bk, t0=t0, n=n: e.matmul(PS[bk][:, 0:n], lhsT=onesM, rhs=co[:, t0:t0 + n],
                                                            start=True, stop=True),
                 reads=["co", "cst"], writes=[("ps", bk)])
            P.op("dve", lambda e, bk=bk, t0=t0, n=n: e.tensor_tensor(out=xc[:, t0:t0 + n], in0=co[:, t0:t0 + n],
                                                                    in1=PS[bk][:, 0:n], op=ALU.subtract),
                 reads=[("ps", bk), "co"], writes=["xc"])
            P.op("act", lambda e, t0=t0, n=n: e.activation(out=sq[:, t0:t0 + n], in_=xc[:, t0:t0 + n], func=AF.Square),
                 reads=["xc"], writes=["sq"])
            bk2 = bank()
            P.op("pe", lambda e, bk2=bk2, t0=t0, n=n: e.matmul(PS[bk2][:, 0:n], lhsT=onesM, rhs=sq[:, t0:t0 + n],
                                                              start=True, stop=True),
                 reads=["sq", "cst"], writes=[("ps", bk2)])
            P.op("dve", lambda e, bk2=bk2, t0=t0, n=n: e.tensor_scalar(out=sq[:, t0:t0 + n], in0=PS[bk2][:, 0:n],
                                                                      scalar1=EPS, scalar2=None, op0=ALU.add),
                 reads=[("ps", bk2)], writes=["sq"])
            P.op("act", lambda e, t0=t0, n=n: e.activation(out=sq[:, t0:t0 + n], in_=sq[:, t0:t0 + n], func=AF.Ln),
                 reads=["sq"], writes=["sq"])
            P.op("act", lambda e, t0=t0, n=n: e.activation(out=sq[:, t0:t0 + n], in_=sq[:, t0:t0 + n], func=AF.Exp,
                                                           scale=-0.5), reads=["sq"], writes=["sq"])
            P.op("pool", lambda e, t0=t0, n=n: e.tensor_tensor(out=xc[:, t0:t0 + n], in0=xc[:, t0:t0 + n],
                                                              in1=sq[:, t0:t0 + n], op=ALU.mult),
                 reads=["xc", "sq"], writes=["xc"])
        yb = g % 2
        P.op("act", lambda e, yb=yb, g=g: e.activation(out=ysp[yb][:, :], in_=xc[:, 0:TH], func=AF.Silu,
                                                       scale=spc(O_CG + g), bias=spc(O_CBETA + g)),
             reads=["xc", "smallp"], writes=[("ysp", yb)])
        P.op("sp", lambda e, yb=yb, g=g: e.dma_start(out=yT_d[NH + g], in_=ysp[yb][:, :]), reads=[("ysp", yb)],
             dma=True)
    P.barrier()

    if STOP == "B3":
        return finish()
    A.release(m_persist)
    yT = A.alloc([128, 2 * NH, TH], BF16, "yT")
    wo_t = [A.alloc([128, 2 * NH, 512], BF16, "wo") for _ in range(2)]
    xblk = [A.alloc([128, 512], F32, "xblk") for _ in range(3)]
    x1blk = [A.alloc([128, 512], F32, "x1blk") for _ in range(3)]
    jk5 = A.alloc([128, 512], F32, "jk5")
    ssq2 = A.alloc([128, NCH, DB], F32, "ssq2")
    rstd2 = A.alloc([128, NCH], F32, "rstd2")
    m_b4 = A.mark()
    for u_ in range(2 * NH):
        P.op("sp", lambda e, u_=u_: e.dma_start(out=yT[:, u_, :], in_=yT_d[u_]), writes=["yT"], dma=True)
    P.op("dve", lambda e: e.memset(ssq2[:], 0.0), writes=["ssq2"])

    def load_wo(db):
        s = db % 2
        src = wout_d[db]
        dst = wo_t[s][:, :, :].rearrange("p k c -> p (k c)")
        P.op("pool", lambda e: e.dma_start(out=dst, in_=src), writes=[("wo", s)], dma=True)

    load_wo(0)
    it = 0
    for db in range(DB):
        pre_issue((len(pre_list) - pre_pos[0] + DB - db - 1) // (DB - db))
        if db + 1 < DB:
            load_wo(db + 1)
        s = db % 2
        for ci, (r0, n) in enumerate(tch):
            b3 = it % 3
            it += 1
            P.op("sp", lambda e, b3=b3, r0=r0, n=n, db=db: e.dma_start(
                out=xblk[b3][0:n, :], in_=xn_d[HALO + r0:HALO + r0 + n, db * 512:(db + 1) * 512]),
                writes=[("xblk", b3)], dma=True)
            bk = bank()
            for cc in range(2 * NH):
                P.op("pe", lambda e, bk=bk, cc=cc, r0=r0, n=n, s=s: e.matmul(
                    PS[bk][0:n, :], lhsT=yT[:, cc, r0:r0 + n], rhs=wo_t[s][:, cc, :],
                    start=(cc == 0), stop=(cc == 2 * NH - 1)), reads=["yT", ("wo", s)], writes=[("ps", bk)])
            P.op("dve", lambda e, bk=bk, b3=b3, n=n: e.tensor_tensor(out=x1blk[b3][0:n, :], in0=xblk[b3][0:n, :],
                                                                    in1=PS[bk][0:n, :], op=ALU.add),
                 reads=[("ps", bk), ("xblk", b3)], writes=[("x1blk", b3)])
            P.op("act", lambda e, b3=b3, n=n, ci=ci, db=db: e.activation(
                out=jk5[0:n, :], in_=x1blk[b3][0:n, :], func=AF.Square, accum_out=ssq2[0:n, ci, db:db + 1]),
                reads=[("x1blk", b3)], writes=["jk5", "ssq2"])
            P.op("sp", lambda e, b3=b3, r0=r0, n=n, db=db: e.dma_start(
                out=x1_d[r0:r0 + n, db * 512:(db + 1) * 512], in_=x1blk[b3][0:n, :]),
                reads=[("x1blk", b3)], writes=["x1_d"], dma=True)
    P.op("dve", lambda e: e.tensor_reduce(out=rstd2[:, :], in_=ssq2[:, :, :], axis=AX.X, op=ALU.add),
         reads=["ssq2"], writes=["rstd2"])
    P.op("dve", lambda e: e.tensor_scalar(out=rstd2[:, :], in0=rstd2[:, :], scalar1=1.0 / D, scalar2=EPS,
                                          op0=ALU.mult, op1=ALU.add), reads=["rstd2"], writes=["rstd2"])
    P.op("act", lambda e: e.activation(out=rstd2[:, :], in_=rstd2[:, :], func=AF.Ln), reads=["rstd2"], writes=["rstd2"])
    P.op("act", lambda e: e.activation(out=rstd2[:, :], in_=rstd2[:, :], func=AF.Exp, scale=-0.5), reads=["rstd2"],
         writes=["rstd2"])
    P.barrier()

    if STOP == "B4":
        return finish()
    A.release(m_persist)
    rstd2b = A.alloc([128, NCH], F32, "rstd2b")
    P.op("dve", lambda e: e.tensor_copy(out=rstd2b[:, :], in_=rstd2[:, :]), reads=["rstd2"], writes=["rstd2b"])
    P.barrier()
    Gt = A.alloc([128, NCH, E], F32, "G")
    GHL = A.alloc([128, NCH, E, 2], BF16, "GHL")
    maskf = A.alloc([128, NCH, E], F32, "maskf")
    maskb = A.alloc([128, NCH, E], BF16, "maskb")
    pos = A.alloc([128, NCH, E], F32, "pos")
    idxi = A.alloc([128, NCH, K], I32, "idxi")
    m_tables = A.mark()
    h2 = A.alloc([128, NCH, D], BF16, "h2")
    m_moe = A.mark()
    x1c = [A.alloc([128, D], F32, "x1c") for _ in range(2)]
    hT2 = A.alloc([128, DC, 128], F32, "hT2")
    wr_t = A.alloc([128, DC, E], F32, "wr")
    diag2 = A.alloc([128, 128], F32, "diag2")
    lg = A.alloc([128, E], F32, "lg")
    wk = A.alloc([128, E], F32, "wk")
    eq = A.alloc([128, E], F32, "eq")
    mx = A.alloc([128, 8], F32, "mx")
    rk = A.alloc([128, E], F32, "rk")
    sl = A.alloc([128, E], F32, "sl")
    idxf = A.alloc([128, K], F32, "idxf")
    P.op("sp", lambda e: e.dma_start(out=wr_t[:], in_=wr_d.rearrange("(k p) c -> p k c", p=128)), writes=["wr"],
         dma=True)
    for ci, (r0, n) in enumerate(tch):
        b = ci % 2
        P.op("sp", lambda e, b=b, r0=r0, n=n: e.dma_start(out=x1c[b][0:n, :], in_=x1_d[r0:r0 + n, :]),
             reads=["x1_d"], writes=[("x1c", b)], dma=True)
        if LVL < -2:
            continue
        P.op("act", lambda e, b=b, n=n, ci=ci: e.activation(out=h2[0:n, ci, :], in_=x1c[b][0:n, :], func=AF.Identity,
                                                          scale=rstd2b[0:n, ci:ci + 1]),
             reads=[("x1c", b), "rstd2b"], writes=["h2"])
        if LVL < -1:
            continue
        P.op("dve", lambda e, n=n, ci=ci: e.tensor_scalar(out=diag2[0:n, 0:n], in0=ident[0:n, 0:n],
                                                        scalar1=rstd2b[0:n, ci:ci + 1], scalar2=None, op0=ALU.mult),
             reads=["rstd2b", "cst"], writes=["diag2"])
        for q in range(DC // 4):
            bk = bank()
            for j in range(4):
                dc = q * 4 + j
                P.op("pe", lambda e, b=b, n=n, dc=dc, bk=bk, j=j: e.matmul(
                    PS[bk][:, j * 128:j * 128 + n], lhsT=x1c[b][0:n, dc * 128:(dc + 1) * 128],
                    rhs=diag2[0:n, 0:n], start=True, stop=True),
                    reads=[("x1c", b), "diag2"], writes=[("ps", bk)])
            for j in range(4):
                dc = q * 4 + j
                eng = "act" if (j % 2 == 0 and cfg.get("EVAC", "dve") == "mix") else "dve"
                if eng == "act":
                    P.op("act", lambda e, bk=bk, j=j, dc=dc, n=n: e.activation(
                        out=hT2[:, dc, 0:n], in_=PS[bk][:, j * 128:j * 128 + n], func=AF.Identity, scale=spc(O_G2 + dc)),
                        reads=[("ps", bk), "smallp"], writes=[("hT2", dc)])
                else:
                    P.op("dve", lambda e, bk=bk, j=j, dc=dc, n=n: e.tensor_scalar(
                        out=hT2[:, dc, 0:n], in0=PS[bk][:, j * 128:j * 128 + n], scalar1=spc(O_G2 + dc), scalar2=None,
                        op0=ALU.mult), reads=[("ps", bk), "smallp"], writes=[("hT2", dc)])
        if LVL < 0:
            continue
        bk = bank()
        for dc in range(DC):
            P.op("pe", lambda e, bk=bk, dc=dc, n=n: e.matmul(PS[bk][0:n, 0:E], lhsT=hT2[:, dc, 0:n], rhs=wr_t[:, dc, :],
                                                            start=(dc == 0), stop=(dc == DC - 1)),
                 reads=[("hT2", dc), "wr"], writes=[("ps", bk)])
        P.op("dve", lambda e, bk=bk, n=n: e.tensor_tensor(out=lg[0:n, :], in0=PS[bk][0:n, 0:E], in1=brt[0:n, :],
                                                         op=ALU.add), reads=[("ps", bk), "cst"], writes=["lg"])
        if debug:
            P.op("sp", lambda e, r0=r0, n=n: e.dma_start(out=dbg_lg[r0:r0 + n, :], in_=lg[0:n, :]), reads=["lg"],
                 dma=True)
        if LVL < 1:
            continue
        P.op("dve", lambda e, n=n: e.tensor_copy(out=wk[0:n, :], in_=lg[0:n, :]), reads=["lg"], writes=["wk"])
        for kk in range(K):
            P.op("dve", lambda e, n=n, kk=kk: e.tensor_reduce(out=mx[0:n, kk:kk + 1], in_=wk[0:n, :], axis=AX.X,
                                                            op=ALU.max), reads=["wk"], writes=["mx"])
            if kk < K - 1:
                P.op("dve", lambda e, n=n, kk=kk: e.tensor_scalar(out=eq[0:n, :], in0=wk[0:n, :],
                                                                scalar1=mx[0:n, kk:kk + 1], scalar2=-1e30,
                                                                op0=ALU.is_equal, op1=ALU.mult),
                     reads=["wk", "mx"], writes=["eq"])
                P.op("dve", lambda e, n=n: e.tensor_tensor(out=wk[0:n, :], in0=wk[0:n, :], in1=eq[0:n, :], op=ALU.add),
                     reads=["wk", "eq"], writes=["wk"])
        P.op("dve", lambda e, n=n, ci=ci: e.tensor_scalar(out=maskf[0:n, ci, :], in0=lg[0:n, :],
                                                        scalar1=mx[0:n, K - 1:K], scalar2=None, op0=ALU.is_ge),
             reads=["lg", "mx"], writes=["maskf"])
        P.op("dve", lambda e, n=n, ci=ci: e.tensor_copy(out=maskb[0:n, ci, :], in_=maskf[0:n, ci, :]),
             reads=["maskf"], writes=["maskb"])
        P.op("dve", lambda e, n=n: e.tensor_scalar(out=mx[0:n, 4:5], in0=mx[0:n, 0:1], scalar1=-1.0, scalar2=None,
                                                 op0=ALU.mult), reads=["mx"], writes=["mx"])
        P.op("act", lambda e, n=n: e.activation(out=wk[0:n, :], in_=lg[0:n, :], func=AF.Exp, bias=mx[0:n, 4:5]),
             reads=["lg", "mx"], writes=["wk"])
        P.op("dve", lambda e, n=n, ci=ci: e.tensor_tensor(out=wk[0:n, :], in0=wk[0:n, :], in1=maskf[0:n, ci, :],
                                                        op=ALU.mult), reads=["wk", "maskf"], writes=["wk"])
        P.op("dve", lambda e, n=n: e.tensor_reduce(out=mx[0:n, 5:6], in_=wk[0:n, :], axis=AX.X, op=ALU.add),
             reads=["wk"], writes=["mx"])
        P.op("dve", lambda e, n=n: e.reciprocal(out=mx[0:n, 6:7], in_=mx[0:n, 5:6]), reads=["mx"], writes=["mx"])
        P.op("dve", lambda e, n=n, ci=ci: e.tensor_scalar(out=Gt[0:n, ci, :], in0=wk[0:n, :], scalar1=mx[0:n, 6:7],
                                                        scalar2=None, op0=ALU.mult),
             reads=["wk", "mx"], writes=["G"])
        if LVL < 2:
            continue
        P.op("dve", lambda e, n=n, ci=ci: e.tensor_copy(out=GHL[0:n, ci, :, 0], in_=Gt[0:n, ci, :]),
             reads=["G"], writes=["GHL"])
        P.op("dve", lambda e, n=n, ci=ci: e.tensor_tensor(out=eq[0:n, :], in0=Gt[0:n, ci, :], in1=GHL[0:n, ci, :, 0],
                                                        op=ALU.subtract), reads=["G", "GHL"], writes=["eq"])
        P.op("dve", lambda e, n=n, ci=ci: e.tensor_copy(out=GHL[0:n, ci, :, 1], in_=eq[0:n, :]),
             reads=["eq"], writes=["GHL"])
        if debug:
            P.op("sp", lambda e, r0=r0, n=n, ci=ci: e.dma_start(out=dbg_G[r0:r0 + n, :], in_=Gt[0:n, ci, :]),
                 reads=["G"], dma=True)
        if LVL < 3:
            continue
        bk = bank()
        for kc in range(ci + 1):
            m = tch[kc][1]
            lhs = onesb[0:m, 0:n] if kc < ci else Ub[0:m, 0:n]
            P.op("pe", lambda e, bk=bk, kc=kc, m=m, n=n, lhs=lhs, ci=ci: e.matmul(
                PS[bk][0:n, 0:E], lhsT=lhs, rhs=maskb[0:m, kc, :], start=(kc == 0), stop=(kc == ci)),
                reads=["maskb", "cbf"], writes=[("ps", bk)])
        P.op("act", lambda e, bk=bk, n=n, ci=ci: e.activation(out=pos[0:n, ci, :], in_=PS[bk][0:n, 0:E], func=AF.Copy),
             reads=[("ps", bk)], writes=["pos"])
        if LVL < 4:
            continue
        P.op("dve", lambda e, n=n, ci=ci: e.tensor_tensor(out=sl[0:n, :], in0=pos[0:n, ci, :], in1=eoff[0:n, :],
                                                        op=ALU.add), reads=["pos", "cst"], writes=["sl"])
        P.op("dve", lambda e, n=n, ci=ci: e.tensor_tensor_scan(out=rk[0:n, :], data0=ones1[0:n, 0:E],
                                                             data1=maskf[0:n, ci, :], initial=0.0, op0=ALU.mult,
                                                             op1=ALU.add), reads=["maskf", "cst"], writes=["rk"])
        for kk in range(K):
            P.op("dve", lambda e, n=n, ci=ci, kk=kk: e.scalar_tensor_tensor(
                out=eq[0:n, :], in0=rk[0:n, :], scalar=float(kk + 1), in1=maskf[0:n, ci, :], op0=ALU.is_equal,
                op1=ALU.mult), reads=["rk", "maskf"], writes=["eq"])
            P.op("dve", lambda e, n=n: e.tensor_tensor(out=eq[0:n, :], in0=eq[0:n, :], in1=sl[0:n, :], op=ALU.mult),
                 reads=["eq", "sl"], writes=["eq"])
            P.op("dve", lambda e, n=n, kk=kk: e.tensor_reduce(out=idxf[0:n, kk:kk + 1], in_=eq[0:n, :], axis=AX.X,
                                                            op=ALU.add), reads=["eq"], writes=["idxf"])
        P.op("dve", lambda e, n=n, ci=ci: e.tensor_copy(out=idxi[0:n, ci, :], in_=idxf[0:n, :]),
             reads=["idxf"], writes=["idxi"])
        if debug:
            P.op("sp", lambda e, r0=r0, n=n, ci=ci: e.dma_start(out=dbg_idx[r0:r0 + n, :], in_=idxi[0:n, ci, :]),
                 reads=["idxi"], dma=True)
    P.barrier()

    if STOP == "B5":
        return finish()
    A.release(m_moe)
    RING = cfg.get("RING", 5)
    wt = [A.alloc([128, 16 * 512], BF16, "wt") for _ in range(RING)]
    xT = A.alloc([128, DC, CAP], BF16, "xT")
    actT = A.alloc([128, FC, CAP], BF16, "actT")
    sel_one = A.alloc([128, NCH, CAP], BF16, "sel")
    sel = [sel_one, sel_one]
    gsl = [A.alloc([128, CC], F32, "gsl") for _ in range(2)]
    NT = 2
    hgt = [A.alloc([128, CAP], F32, "hg") for _ in range(NT)]
    sgt2 = [A.alloc([128, CAP], F32, "sg") for _ in range(NT)]
    hut = [A.alloc([128, CAP], F32, "hu") for _ in range(NT)]
    yst = [A.alloc([128, 512], F32, "yst") for _ in range(2)]
    if cfg.get("VERBOSE"):
        print("MoE phase SBUF end", A.cur, "of", A.hi)

    tiles = []
    for e_ in range(E):
        for fb in range(FB):
            tiles.append(("g", e_, fb))
            tiles.append(("u", e_, fb))
        for db in range(DB):
            tiles.append(("d", e_, db))
    tslot = {}
    tnext = [0]

    def tile_view(s, kind):
        if kind == "d":
            return wt[s][:, 0:FC * 512].rearrange("p (k c) -> p k c", k=FC)
        return wt[s][:, 0:DC * 256].rearrange("p (k c) -> p k c", k=DC)

    def ensure_tile(i):
        while tnext[0] < len(tiles) and tnext[0] <= i + RING - 2:
            j = tnext[0]
            kind, e_, blk = tiles[j]
            s = j % RING
            pre = e_ < NPRE
            if kind == "d":
                src = (wdb_d if pre else wd_d)[e_ * DB + blk]
                dst = wt[s][:, 0:FC * 512]
                pk = ("pre", "d", e_ * DB + blk)
            elif kind == "g":
                src = (wgb_d if pre else wg_d)[e_ * FB + blk]
                dst = wt[s][:, 0:DC * 256]
                pk = ("pre", "g", e_ * FB + blk)
            else:
                src = (wub_d if pre else wu_d)[e_ * FB + blk]
                dst = wt[s][:, 0:DC * 256]
                pk = ("pre", "u", e_ * FB + blk)
            P.op("pool", lambda e, dst=dst, src=src: e.dma_start(out=dst, in_=src), reads=([pk] if pre else []),
                 writes=[("wt", s)], dma=True)
            tslot[j] = s
            tnext[0] += 1
        return tslot[i]

    def build_sel(e_):
        sb = e_ % 2
        for ci, (r0, n) in enumerate(tch):
            P.op("dve", lambda e, sb=sb, ci=ci, n=n, e_=e_: e.tensor_scalar(
                out=sel[sb][0:n, ci, :], in0=iotaC[0:n, :], scalar1=pos[0:n, ci, e_:e_ + 1],
                scalar2=maskf[0:n, ci, e_:e_ + 1], op0=ALU.is_equal, op1=ALU.mult),
                reads=["pos", "maskf", "cst"], writes=["sel"])
        bk = bank()
        for cc in range(CC):
            for ci, (r0, n) in enumerate(tch):
                P.op("pe", lambda e, sb=sb, ci=ci, n=n, cc=cc, bk=bk, e_=e_: e.matmul(
                    PS[bk][:, cc * 2:cc * 2 + 2], lhsT=sel[sb][0:n, ci, cc * 128:(cc + 1) * 128],
                    rhs=GHL[0:n, ci, e_, :], start=(ci == 0 and cc == 0), stop=(ci == NCH - 1),
                    skip_group_check=True),
                    reads=["sel", "GHL"], writes=[("ps", bk)])
        for cc in range(CC):
            P.op("dve", lambda e, sb=sb, cc=cc, bk=bk: e.tensor_reduce(
                out=gsl[sb][:, cc:cc + 1], in_=PS[bk][:, cc * 2:cc * 2 + 2], axis=AX.X, op=ALU.add),
                reads=[("ps", bk)], writes=[("gsl", sb)])

    def gather(e_):
        sb = e_ % 2
        for dc in range(DC):
            bk = bank()
            for ci, (r0, n) in enumerate(tch):
                P.op("pe", lambda e, sb=sb, ci=ci, n=n, dc=dc, bk=bk: e.matmul(
                    PS[bk][:, 0:CAP], lhsT=h2[0:n, ci, dc * 128:(dc + 1) * 128], rhs=sel[sb][0:n, ci, :],
                    start=(ci == 0), stop=(ci == NCH - 1)), reads=["h2", "sel"], writes=[("ps", bk)])
            if False:
                pass
            else:
                P.op("dve", lambda e, dc=dc, bk=bk: e.tensor_scalar(out=xT[:, dc, :], in0=PS[bk][:, 0:CAP],
                                                                   scalar1=spc(O_G2 + dc), scalar2=None, op0=ALU.mult),
                     reads=[("ps", bk), "smallp"], writes=[("xT", dc)])

    ti = 0
    build_sel(0)
    gather(0)
    tcount = 0
    ycount = 0
    for e_ in range(E):
        for fb in range(FB):
            sg_ = ensure_tile(ti); ti += 1
            su_ = ensure_tile(ti); ti += 1
            wgv = tile_view(sg_, "g")
            wuv = tile_view(su_, "u")
            for fcl in range(2):
                fc = fb * 2 + fcl
                bg_ = bank()
                for dc in range(DC):
                    P.op("pe", lambda e, bg_=bg_, dc=dc, fcl=fcl, wgv=wgv: e.matmul(
                        PS[bg_][:, 0:CAP], lhsT=wgv[:, dc, fcl * 128:(fcl + 1) * 128], rhs=xT[:, dc, :],
                        start=(dc == 0), stop=(dc == DC - 1)), reads=[("wt", sg_), ("xT", dc)], writes=[("ps", bg_)])
                bu_ = bank()
                for dc in range(DC):
                    P.op("pe", lambda e, bu_=bu_, dc=dc, fcl=fcl, wuv=wuv: e.matmul(
                        PS[bu_][:, 0:CAP], lhsT=wuv[:, dc, fcl * 128:(fcl + 1) * 128], rhs=xT[:, dc, :],
                        start=(dc == 0), stop=(dc == DC - 1)), reads=[("wt", su_), ("xT", dc)], writes=[("ps", bu_)])
                tb = tcount % NT
                tcount += 1
                bgc = spc(O_BG + e_ * FC + fc)
                buc = spc(O_BU + e_ * FC + fc)
                P.op("dve", lambda e, tb=tb, bg_=bg_, bgc=bgc: e.tensor_scalar(
                    out=hgt[tb][:, :], in0=PS[bg_][:, 0:CAP], scalar1=bgc, scalar2=7.0, op0=ALU.add, op1=ALU.min),
                    reads=[("ps", bg_), "smallp"], writes=[("hg", tb)])
                P.op("act", lambda e, tb=tb: e.activation(out=sgt2[tb][:, :], in_=hgt[tb][:, :], func=AF.Sigmoid,
                                                         scale=1.702), reads=[("hg", tb)], writes=[("sg", tb)])
                P.op("dve", lambda e, tb=tb, bu_=bu_, buc=buc: e.tensor_scalar(
                    out=hut[tb][:, :], in0=PS[bu_][:, 0:CAP], scalar1=buc, scalar2=7.0, op0=ALU.add, op1=ALU.min),
                    reads=[("ps", bu_), "smallp"], writes=[("hu", tb)])
                P.op("dve", lambda e, tb=tb: e.tensor_scalar(out=hut[tb][:, :], in0=hut[tb][:, :], scalar1=-7.0,
                                                            scalar2=1.0, op0=ALU.max, op1=ALU.add),
                     reads=[("hu", tb)], writes=[("hu", tb)])
                P.op("dve", lambda e, tb=tb: e.tensor_tensor(out=hgt[tb][:, :], in0=hgt[tb][:, :], in1=sgt2[tb][:, :],
                                                            op=ALU.mult), reads=[("hg", tb), ("sg", tb)],
                     writes=[("hg", tb)])
                P.op("dve", lambda e, tb=tb, fc=fc: e.tensor_tensor(out=actT[:, fc, :], in0=hgt[tb][:, :],
                                                                   in1=hut[tb][:, :], op=ALU.mult),
                     reads=[("hg", tb), ("hu", tb)], writes=["actT"])
        if e_ + 1 < E:
            build_sel(e_ + 1)
            gather(e_ + 1)
        sb = e_ % 2
        for db in range(DB):
            sd_ = ensure_tile(ti); ti += 1
            wdv = tile_view(sd_, "d")
            for cc in range(CC):
                yb = ycount % 2
                ycount += 1
                bk = bank()
                for fc in range(FC):
                    P.op("pe", lambda e, bk=bk, fc=fc, cc=cc, wdv=wdv: e.matmul(
                        PS[bk][:, :], lhsT=actT[:, fc, cc * 128:(cc + 1) * 128], rhs=wdv[:, fc, :],
                        start=(fc == 0), stop=(fc == FC - 1)), reads=["actT", ("wt", sd_)], writes=[("ps", bk)])
                P.op("dve", lambda e, bk=bk, cc=cc, yb=yb, sb=sb: e.tensor_scalar(
                    out=yst[yb][:, :], in0=PS[bk][:, :], scalar1=gsl[sb][:, cc:cc + 1], scalar2=None, op0=ALU.mult),
                    reads=[("ps", bk), ("gsl", sb)], writes=[("yst", yb)])
                dst = Y_d[e_ * CAP + cc * 128:e_ * CAP + (cc + 1) * 128, db * 512:(db + 1) * 512]
                P.op("sp", lambda e, yb=yb, dst=dst: e.dma_start(out=dst, in_=yst[yb][:, :]), reads=[("yst", yb)],
                     writes=["Y_d"], dma=True)
    P.barrier()

    if STOP == "B6":
        return finish()
    A.release(m_tables)
    ga2 = [[A.alloc([128, D], F32, "ga") for _ in range(K)] for _ in range(2)]
    x1f = A.alloc([128, D], F32, "x1f")
    gfb = A.alloc([128, D], F32, "gfb")
    bdn = A.alloc([128, D], F32, "bdn")
    GT = A.alloc([128, 128], F32, "GT")
    s7 = A.alloc([128, 4], F32, "s7")
    P.op("sp", lambda e: e.dma_start(out=gfb[:], in_=gfb_d), writes=["gfb"], dma=True)
    P.op("sp", lambda e: e.dma_start(out=bdn[0:E, :], in_=bd_d), writes=["bdn"], dma=True)
    for ci, (r0, n) in enumerate(tch):
        ga = ga2[ci % 2]
        gp = ci % 2
        P.op("sp", lambda e, r0=r0, n=n: e.dma_start(out=x1f[0:n, :], in_=x1_d[r0:r0 + n, :]), reads=["x1_d"],
             writes=["x1f"], dma=True)
        for kk in range(K):
            P.op("pool", lambda e, kk=kk, n=n, ci=ci, ga=ga: e.indirect_dma_start(
                out=ga[kk][0:n, :], out_offset=None, in_=Y_d,
                in_offset=bass.IndirectOffsetOnAxis(ap=idxi[0:n, ci, kk:kk + 1], axis=0)),
                reads=["Y_d", "idxi"], writes=[("ga", gp, kk)], dma=True)
        bk = bank()
        P.op("pe", lambda e, bk=bk, n=n, ci=ci: e.matmul(PS[bk][0:E, 0:n], lhsT=Gt[0:n, ci, :], rhs=ident[0:n, 0:n],
                                                        start=True, stop=True), reads=["G", "cst"], writes=[("ps", bk)])
        P.op("act", lambda e, bk=bk, n=n: e.activation(out=GT[0:E, 0:n], in_=PS[bk][0:E, 0:n], func=AF.Copy),
             reads=[("ps", bk)], writes=["GT"])
        for db in range(DB):
            bk = bank()
            P.op("pe", lambda e, bk=bk, n=n, db=db: e.matmul(PS[bk][0:n, :], lhsT=GT[0:E, 0:n],
                                                            rhs=bdn[0:E, db * 512:(db + 1) * 512], start=True,
                                                            stop=True), reads=["GT", "bdn"], writes=[("ps", bk)])
            P.op("dve", lambda e, bk=bk, n=n, db=db: e.tensor_tensor(
                out=x1f[0:n, db * 512:(db + 1) * 512], in0=x1f[0:n, db * 512:(db + 1) * 512], in1=PS[bk][0:n, :],
                op=ALU.add), reads=[("ps", bk), "x1f"], writes=["x1f"])
        P.op("pool", lambda e, n=n, ga=ga: e.tensor_tensor(out=ga[0][0:n, :], in0=ga[0][0:n, :], in1=ga[1][0:n, :],
                                                   op=ALU.add), reads=[("ga", gp, 0), ("ga", gp, 1)], writes=[("ga", gp, 0)])
        P.op("dve", lambda e, n=n, ga=ga: e.tensor_tensor(out=ga[2][0:n, :], in0=ga[2][0:n, :], in1=ga[3][0:n, :],
                                                  op=ALU.add), reads=[("ga", gp, 2), ("ga", gp, 3)], writes=[("ga", gp, 2)])
        P.op("pool", lambda e, n=n, ga=ga: e.tensor_tensor(out=ga[0][0:n, :], in0=ga[0][0:n, :], in1=ga[2][0:n, :],
                                                   op=ALU.add), reads=[("ga", gp, 0), ("ga", gp, 2)], writes=[("ga", gp, 0)])
        P.op("dve", lambda e, n=n, ga=ga: e.tensor_tensor(out=x1f[0:n, :], in0=x1f[0:n, :], in1=ga[0][0:n, :], op=ALU.add),
             reads=[("ga", gp, 0), "x1f"], writes=["x1f"])
        P.op("dve", lambda e: e.memset(s7[:, 0:1], 0.0), writes=["s7"])
        jk7 = ga[3]
        P.op("act", lambda e, n=n, jk7=jk7: e.activation(out=jk7[0:n, :], in_=x1f[0:n, :], func=AF.Square,
                                                        accum_out=s7[0:n, 0:1]),
             reads=["x1f", ("ga", gp, 2)], writes=[("ga", gp, 3), "s7"])
        P.op("dve", lambda e, n=n: e.tensor_scalar(out=s7[0:n, 1:2], in0=s7[0:n, 0:1], scalar1=1.0 / D, scalar2=EPS,
                                                 op0=ALU.mult, op1=ALU.add), reads=["s7"], writes=["s7b"])
        P.op("act", lambda e, n=n: e.activation(out=s7[0:n, 3:4], in_=s7[0:n, 1:2], func=AF.Ln), reads=["s7b"],
             writes=["s7d"])
        P.op("act", lambda e, n=n: e.activation(out=s7[0:n, 2:3], in_=s7[0:n, 3:4], func=AF.Exp, scale=-0.5),
             reads=["s7d"], writes=["s7c"])
        P.op("dve", lambda e, n=n, jk7=jk7: e.scalar_tensor_tensor(out=jk7[0:n, :], in0=x1f[0:n, :],
                                                                 scalar=s7[0:n, 2:3], in1=gfb[0:n, :], op0=ALU.mult,
                                                                 op1=ALU.mult),
             reads=["x1f", "s7c", "gfb", ("ga", gp, 3)], writes=[("ga", gp, 3)])
        P.op("sp", lambda e, r0=r0, n=n, jk7=jk7: e.dma_start(out=out_d[r0:r0 + n, :], in_=jk7[0:n, :]),
             reads=[("ga", gp, 3)], dma=True)
    P.barrier()
    P.emit(nc)
    return nc


def _pcol(v):
    v = np.asarray(v, np.float32)
    return np.ascontiguousarray(v.reshape(-1, 128).T)


def make_in_maps(cfg, inp):
    D = cfg["D"]; SEQ = cfg["SEQ"]; B = cfg["B"]; E = cfg["E"]; CAP = cfg["CAP"]; HALO = cfg["HALO"]
    ST = SEQ + cfg["NMETA"]; TH = ST // 2
    LW = D // 2; CW = D // 2; FF = D // 2
    NH = LW // 128; NG = CW // 128; DC = D // 128; FC = FF // 128
    f32 = np.float32
    x = np.asarray(inp["x"], f32)
    meta = np.asarray(inp["meta_tokens"], f32)
    g1 = np.asarray(inp["norm1_g"], f32)[0]
    g2 = np.asarray(inp["norm2_g"], f32)[0]
    gfin = np.asarray(inp["final_norm_g"], f32)
    lcw = np.asarray(inp["lru_conv_w"], f32)[0]
    lcb = np.asarray(inp["lru_conv_b"], f32)[0]
    lwa = np.asarray(inp["lru_w_a"], f32)[0]
    lba = np.asarray(inp["lru_b_a"], f32)[0]
    lwi = np.asarray(inp["lru_w_i"], f32)[0]
    lbi = np.asarray(inp["lru_b_i"], f32)[0]
    lam = np.asarray(inp["lru_lambda"], f32)[0]
    ccw = np.asarray(inp["conf_conv_w"], f32)[0]
    ccb = np.asarray(inp["conf_conv_b"], f32)[0]
    cng = np.asarray(inp["conf_norm_g"], f32)[0]
    cnb = np.asarray(inp["conf_norm_b"], f32)[0]
    bg = np.asarray(inp["b_gate"], f32)[0]
    bu = np.asarray(inp["b_up"], f32)[0]
    brt = np.asarray(inp["b_router"], f32)[0]

    ident = np.eye(128, dtype=f32)
    U = np.triu(np.ones((128, 128), f32), 1)
    onesM = np.full((128, 128), 1.0 / 128, f32)
    ones1 = np.ones((128, 128), f32)
    iotaC = np.tile(np.arange(CAP, dtype=f32)[None, :], (128, 1))
    eoff = np.tile((np.arange(E, dtype=f32) * CAP)[None, :], (128, 1))
    brtb = np.tile(brt[None, :], (128, 1))
    consts = np.ascontiguousarray(np.concatenate([ident, U, onesM, ones1, iotaC, eoff, brtb], axis=1))

    shared = dict(
        consts=consts,
        g1b=np.ascontiguousarray(np.tile(g1[None, :], (128, 1))),
        gfb=np.ascontiguousarray(np.tile(gfin[None, :], (128, 1))),
        w_in=np.ascontiguousarray(np.asarray(inp["w_in"], f32)[0].reshape(DC, 128, 4 * LW // 128, 128)
                                  .transpose(2, 1, 0, 3)).reshape(4 * LW // 128, 128, DC * 128),
        w_out=np.ascontiguousarray(np.asarray(inp["w_out"], f32)[0].reshape(D // 128, 128, D // 512, 512)
                                   .transpose(2, 1, 0, 3)).reshape(D // 512, 128, (D // 128) * 512),
        w_router=np.asarray(inp["w_router"], f32)[0],
        w_gate=np.ascontiguousarray(np.asarray(inp["w_gate"], f32)[0].reshape(E, DC, 128, FF // 256, 256)
                                    .transpose(0, 3, 2, 1, 4)).reshape(E * (FF // 256), 128, DC * 256),
        w_up=np.ascontiguousarray(np.asarray(inp["w_up"], f32)[0].reshape(E, DC, 128, FF // 256, 256)
                                  .transpose(0, 3, 2, 1, 4)).reshape(E * (FF // 256), 128, DC * 256),
        w_down=np.ascontiguousarray(np.asarray(inp["w_down"], f32)[0].reshape(E, FC, 128, D // 512, 512)
                                    .transpose(0, 3, 2, 1, 4)).reshape(E * (D // 512), 128, FC * 512),
        b_down=np.asarray(inp["b_down"], f32)[0],
    )

    def smallp(dirs, rev_taps):
        cols = [_pcol(g1), _pcol(g2)]
        for dr in dirs:
            cw = lcw[dr].reshape(4, NH, 128)
            cols.append(np.ascontiguousarray(cw.transpose(2, 1, 0)).reshape(128, NH * 4))
            cols.append(_pcol(lcb[dr]))
            cols.append(_pcol(lba[dr].reshape(-1)))
            cols.append(_pcol(lbi[dr].reshape(-1)))
            cols.append(_pcol(lam[dr]))
        w = ccw[::-1] if rev_taps else ccw
        cw = w.reshape(31, NG, 128)
        cols.append(np.ascontiguousarray(cw.transpose(2, 1, 0)).reshape(128, NG * 31))
        cols += [_pcol(ccb), _pcol(cng), _pcol(cnb)]
        cols.append(np.ascontiguousarray(bg.reshape(E, FC, 128).transpose(2, 0, 1)).reshape(128, E * FC))
        cols.append(np.ascontiguousarray(bu.reshape(E, FC, 128).transpose(2, 0, 1)).reshape(128, E * FC))
        return np.ascontiguousarray(np.concatenate(cols, axis=1))

    maps = []
    for b in range(B):
        S = np.concatenate([meta, x[b]], axis=0)
        for half in range(2):
            Sl = S if half == 1 else S[::-1]
            dirs = (0, 1) if half == 1 else (1, 0)
            m = dict(shared)
            m["xo"] = np.ascontiguousarray(Sl[:TH])
            m["xn"] = np.ascontiguousarray(Sl[TH - HALO:])
            m["smallp"] = smallp(dirs, rev_taps=(half == 0))
            m["wa_f"] = np.ascontiguousarray(lwa[dirs[0]].reshape(NH * 128, 128))
            m["wa_b"] = np.ascontiguousarray(lwa[dirs[1]].reshape(NH * 128, 128))
            m["wi_f"] = np.ascontiguousarray(lwi[dirs[0]].reshape(NH * 128, 128))
            m["wi_b"] = np.ascontiguousarray(lwi[dirs[1]].reshape(NH * 128, 128))
            maps.append(m)
    return maps


def assemble(cfg, results):
    D = cfg["D"]; SEQ = cfg["SEQ"]; B = cfg["B"]; NM = cfg["NMETA"]
    ST = SEQ + NM; TH = ST // 2
    out = np.empty((B, SEQ, D), np.float32)
    i = 0
    for b in range(B):
        full = np.empty((ST, D), np.float32)
        for half in range(2):
            o = np.asarray(results[i]["out"], np.float32)
            i += 1
            if half == 1:
                full[TH:] = o
            else:
                full[:TH] = o[::-1]
        out[b] = full[NM:]
    return out


_NC_CACHE = {}


def run(cfg, inp, debug=False):
    key = (tuple(sorted(cfg.items())), debug)
    if key not in _NC_CACHE:
        _NC_CACHE[key] = build_program(cfg, debug=debug)
    nc = _NC_CACHE[key]
    maps = make_in_maps(cfg, inp)
    n = len(maps)
    res = run_bass_kernel_spmd(nc, maps, core_ids=list(range(n)))
    return res


def kernel(**inputs):
    cfg = dict(REAL_CFG)
    res = run(cfg, inputs)
    return assemble(cfg, res.results)
```

```python
import numpy as np
import concourse.bass as bass
import concourse.mybir as mybir
from concourse.bass_utils import run_bass_kernel_spmd

F32 = mybir.dt.float32
BF16 = mybir.dt.bfloat16
I32 = mybir.dt.int32
AF = mybir.ActivationFunctionType
ALU = mybir.AluOpType
AX = mybir.AxisListType

REAL_CFG = dict(D=4096, SEQ=2048, B=4, E=32, K=4, CAP=256, HALO=16, NMETA=16)


class _Op:
    __slots__ = ("eng", "fn", "deps", "signal", "sigval", "dma", "dsem", "dtarget", "idx")

    def __init__(self, eng, fn, dma):
        self.eng = eng
        self.fn = fn
        self.deps = []
        self.signal = False
        self.sigval = 0
        self.dma = dma
        self.dsem = None
        self.dtarget = 0


class Prog:
    ENGS = ("pe", "act", "dve", "pool", "sp")
    NDSEM = 8

    def __init__(self):
        self.ops = {e: [] for e in self.ENGS}
        self.last_write = {}
        self.readers = {}
        self.dma_rr = {e: 0 for e in self.ENGS}
        self.dma_last = {}
        self.dma_count = {}
        self.n = 0

    def op(self, eng, fn, reads=(), writes=(), dma=False):
        o = _Op(eng, fn, dma)
        o.idx = self.n
        self.n += 1
        deps = {}
        for k in reads:
            lw = self.last_write.get(k)
            if lw is not None:
                deps[id(lw)] = lw
        for k in writes:
            lw = self.last_write.get(k)
            if lw is not None:
                deps[id(lw)] = lw
            for r in self.readers.get(k, {}).values():
                deps[id(r)] = r
        rkey = eng
        if dma:
            i = self.dma_rr[eng]
            self.dma_rr[eng] = (i + 1) % self.NDSEM
            o.dsem = (eng, i)
            prev = self.dma_last.get(o.dsem)
            if prev is not None:
                deps[id(prev)] = prev
            self.dma_last[o.dsem] = o
            c = self.dma_count.get(o.dsem, 0) + 1
            self.dma_count[o.dsem] = c
            o.dtarget = 16 * c
            rkey = o.dsem
        for d in deps.values():
            if d is o:
                continue
            if (not d.dma) and d.eng == "pe" and eng == "pe" and not dma:
                continue
            o.deps.append(d)
        for k in reads:
            self.readers.setdefault(k, {})[rkey] = o
        for k in writes:
            self.last_write[k] = o
            self.readers[k] = {}
        self.ops[eng].append(o)
        return o

    def barrier(self):
        lasts = []
        for e in self.ENGS:
            for o in reversed(self.ops[e]):
                if not o.dma and o.fn is not None:
                    lasts.append(o)
                    break
        lasts += list(self.dma_last.values())
        for e in self.ENGS:
            o = _Op(e, None, False)
            o.idx = self.n
            self.n += 1
            o.deps = [d for d in lasts if not (d.eng == e and not d.dma and e == "pe")]
            self.ops[e].append(o)

    def emit(self, nc):
        for e in self.ENGS:
            for o in self.ops[e]:
                for d in o.deps:
                    d.signal = True
        for e in self.ENGS:
            c = 0
            for o in self.ops[e]:
                if o.signal and not o.dma:
                    c += 1
                    o.sigval = c
        from contextlib import ExitStack
        with ExitStack() as st:
            esem = {e: st.enter_context(nc.semaphore("es_" + e)) for e in self.ENGS}
            dsem = {}
            for e in self.ENGS:
                if self.dma_count and any(k[0] == e for k in self.dma_count):
                    for i in range(self.NDSEM):
                        dsem[(e, i)] = st.enter_context(nc.semaphore("ds_%s%d" % (e, i)))
            block = st.enter_context(nc.Block())

            def run(eng_name, e):
                waited = {}
                for o in self.ops[eng_name]:
                    for d in o.deps:
                        if d.dma:
                            sem, val, key = dsem[d.dsem], d.dtarget, d.dsem
                        else:
                            sem, val, key = esem[d.eng], d.sigval, d.eng
                        if waited.get(key, 0) < val:
                            e.wait_ge(sem, val)
                            waited[key] = val
                    if o.fn is None:
                        continue
                    inst = o.fn(e)
                    if o.dma:
                        inst.then_inc(dsem[o.dsem], 16)
                    elif o.signal:
                        inst.then_inc(esem[eng_name], 1)

            @block.tensor
            def _(e):
                run("pe", e)

            @block.scalar
            def _(e):
                run("act", e)

            @block.vector
            def _(e):
                run("dve", e)

            @block.gpsimd
            def _(e):
                run("pool", e)

            @block.sync
            def _(e):
                run("sp", e)


class Arena:
    def __init__(self, nc, lo=16512, hi=229344):
        self.nc = nc
        self.lo = lo
        self.hi = hi
        self.cur = lo
        self.cnt = 0

    def alloc(self, shape, dtype, name="t"):
        sz = 4 if dtype in (F32, I32) else 2
        n = 1
        for s in shape[1:]:
            n *= s
        nbytes = (n * sz + 63) // 64 * 64
        off = self.cur
        assert off + nbytes <= self.hi, "SBUF arena overflow: %s %s need %d at %d" % (name, shape, nbytes, off)
        self.cur += nbytes
        self.cnt += 1
        return self.nc.alloc_sbuf_tensor_at("%s_%d" % (name, self.cnt), list(shape), dtype, offset=off)

    def mark(self):
        return self.cur

    def release(self, m):
        self.cur = m


def _chunks(total, size=128):
    out = []
    s = 0
    while s < total:
        n = min(size, total - s)
        out.append((s, n))
        s += n
    return out


def build_program(cfg, debug=False):
    D = cfg["D"]; SEQ = cfg["SEQ"]; E = cfg["E"]; K = cfg["K"]; CAP = cfg["CAP"]; HALO = cfg["HALO"]
    ST = SEQ + cfg["NMETA"]
    TH = ST // 2
    LW = D // 2; CW = D // 2; FF = D // 2
    NH = LW // 128; NG = CW // 128; DC = D // 128; FC = FF // 128; CC = CAP // 128
    CK = 31
    TW = HALO + TH
    DB = D // 512
    FB = FF // 256
    tch = _chunks(TH)
    NCH = len(tch)
    EPS = 1e-5

    nc = bass.Bass("TRN2", target_bir_lowering=False)
    P = Prog()
    A = Arena(nc)

    STOP = cfg.get("STOP", "")
    LVL = cfg.get("LVL", 9)

    def finish():
        P.barrier()
        P.emit(nc)
        return nc

    def din(name, shape, dt=F32):
        return nc.dram_tensor(name, list(shape), dt, kind="ExternalInput").ap()

    xo_d = din("xo", [TH, D])
    xn_d = din("xn", [TW, D])
    NSP = (2 * DC + 2 * (NH * 4 + 4 * NH) + NG * CK + 3 * NG + 2 * E * FC)
    sp_d = din("smallp", [128, NSP])
    NCST = 128 * 4 + CAP + 2 * E
    cst_d = din("consts", [128, NCST])
    g1b_d = din("g1b", [128, D])
    gfb_d = din("gfb", [128, D])
    NU = 4 * LW // 128
    win_d = din("w_in", [NU, 128, DC * 128])
    wa_d = [din("wa_f", [NH * 128, 128]), din("wa_b", [NH * 128, 128])]
    wi_d = [din("wi_f", [NH * 128, 128]), din("wi_b", [NH * 128, 128])]
    wout_d = din("w_out", [D // 512, 128, 2 * NH * 512])
    wr_d = din("w_router", [D, E])
    wg_d = din("w_gate", [E * (FF // 256), 128, (D // 128) * 256])
    wu_d = din("w_up", [E * (FF // 256), 128, (D // 128) * 256])
    wd_d = din("w_down", [E * (D // 512), 128, (FF // 128) * 512])
    bd_d = din("b_down", [E, D])
    out_d = nc.dram_tensor("out", [TH, D], F32, kind="ExternalOutput").ap()
    skind = "ExternalOutput" if debug else "Internal"
    yT_d = nc.dram_tensor("yT_scr", [2 * NH, 128, TH], BF16, kind=skind).ap()
    x1_d = nc.dram_tensor("x1_scr", [TH, D], F32, kind=skind).ap()
    Y_d = nc.dram_tensor("Y_scr", [E * CAP, D], F32, kind=skind).ap()
    NPRE = cfg.get("NPRE", 5)
    wgb_d = nc.dram_tensor("wg_bf", [NPRE * (FF // 256), 128, (D // 128) * 256], BF16, kind="Internal").ap()
    wub_d = nc.dram_tensor("wu_bf", [NPRE * (FF // 256), 128, (D // 128) * 256], BF16, kind="Internal").ap()
    wdb_d = nc.dram_tensor("wd_bf", [NPRE * (D // 512), 128, (FF // 128) * 512], BF16, kind="Internal").ap()
    pre_list = []
    for e_ in range(NPRE):
        for fb in range(FF // 256):
            pre_list.append(("g", e_ * (FF // 256) + fb))
            pre_list.append(("u", e_ * (FF // 256) + fb))
        for db in range(D // 512):
            pre_list.append(("d", e_ * (D // 512) + db))
    pre_pos = [0]

    def pre_issue(n):
        for _ in range(n):
            if pre_pos[0] >= len(pre_list):
                return
            kind, idx = pre_list[pre_pos[0]]
            pre_pos[0] += 1
            src = {"g": wg_d, "u": wu_d, "d": wd_d}[kind][idx]
            dst = {"g": wgb_d, "u": wub_d, "d": wdb_d}[kind][idx]
            P.op("pool", lambda e, src=src, dst=dst: e.dma_start(out=dst, in_=src), writes=[("pre", kind, idx)],
                 dma=True)

    if debug:
        dbg_lg = nc.dram_tensor("dbg_logits", [TH, E], F32, kind="ExternalOutput").ap()
        dbg_G = nc.dram_tensor("dbg_G", [TH, E], F32, kind="ExternalOutput").ap()
        dbg_idx = nc.dram_tensor("dbg_idx", [TH, K], I32, kind="ExternalOutput").ap()
        dbg_st = nc.dram_tensor("dbg_state", [128, NH], F32, kind="ExternalOutput").ap()

    PS = [nc.alloc_psum_tensor("ps%d" % i, [128, 512], F32) for i in range(8)]
    ps_rr = [0]

    def bank():
        i = ps_rr[0]
        ps_rr[0] = (i + 1) % 8
        return i

    smallp = A.alloc([128, NSP], F32, "smallp")
    cst = A.alloc([128, NCST], F32, "consts")
    cbf = A.alloc([128, 3 * 128], BF16, "cbf")
    stA = A.alloc([128, NH], F32, "stateA")
    cA = A.alloc([128, 2, 2 * NH], F32, "cA")
    P.op("sp", lambda e: e.dma_start(out=smallp[:], in_=sp_d), writes=["smallp"], dma=True)
    P.op("sp", lambda e: e.dma_start(out=cst[:], in_=cst_d), writes=["cst"], dma=True)
    ident = cst[:, 0:128]
    Utri = cst[:, 128:256]
    onesM = cst[:, 256:384]
    ones1 = cst[:, 384:512]
    iotaC = cst[:, 512:512 + CAP]
    eoff = cst[:, 512 + CAP:512 + CAP + E]
    brt = cst[:, 512 + CAP + E:512 + CAP + 2 * E]
    P.op("dve", lambda e: e.tensor_copy(out=cbf[:, 0:128], in_=ident), reads=["cst"], writes=["cbf"])
    P.op("dve", lambda e: e.tensor_copy(out=cbf[:, 128:256], in_=Utri), reads=["cst"], writes=["cbf"])
    P.op("dve", lambda e: e.tensor_copy(out=cbf[:, 256:384], in_=ones1), reads=["cst"], writes=["cbf"])
    identb = cbf[:, 0:128]
    Ub = cbf[:, 128:256]
    onesb = cbf[:, 256:384]

    o = 0
    O_G1 = o; o += DC
    O_G2 = o; o += DC
    O_DIR = []
    for _ in range(2):
        d = {}
        d["cw"] = o; o += NH * 4
        d["cb"] = o; o += NH
        d["ba"] = o; o += NH
        d["bi"] = o; o += NH
        d["lam"] = o; o += NH
        O_DIR.append(d)
    O_CW = o; o += NG * CK
    O_CB = o; o += NG
    O_CG = o; o += NG
    O_CBETA = o; o += NG
    O_BG = o; o += E * FC
    O_BU = o; o += E * FC
    assert o == NSP

    def spc(off, n=1):
        return smallp[:, off:off + n]

    for dr in range(2):
        lam = spc(O_DIR[dr]["lam"], NH)
        P.op("act", lambda e, dr=dr, lam=lam: e.activation(out=cA[:, dr, 0:NH], in_=lam, func=AF.Exp, scale=-1.0),
             reads=["smallp"], writes=[("cA", dr)])
        P.op("act", lambda e, dr=dr: e.activation(out=cA[:, dr, 0:NH], in_=cA[:, dr, 0:NH], func=AF.Ln, bias=1.0),
             reads=[("cA", dr)], writes=[("cA", dr)])
        P.op("dve", lambda e, dr=dr: e.tensor_scalar(out=cA[:, dr, NH:2 * NH], in0=cA[:, dr, 0:NH], scalar1=-16.0,
                                                     scalar2=None, op0=ALU.mult),
             reads=[("cA", dr)], writes=[("cA2", dr)])
        P.op("dve", lambda e, dr=dr: e.tensor_scalar(out=cA[:, dr, 0:NH], in0=cA[:, dr, 0:NH], scalar1=-8.0,
                                                     scalar2=None, op0=ALU.mult),
             reads=[("cA", dr), ("cA2", dr)], writes=[("cA", dr)])

    m_persist = A.mark()

    hT = A.alloc([128, DC, TW], BF16, "hT")
    RING_IN = 4
    win_t = [A.alloc([128, DC, 128], BF16, "win") for _ in range(RING_IN)]
    wgt = [[A.alloc([128, NH, 128], BF16, "wa"), A.alloc([128, NH, 128], BF16, "wi")] for _ in range(2)]
    for dr in range(2):
        P.op("pool", lambda e, dr=dr: e.dma_start(out=wgt[dr][0][:], in_=wa_d[dr].rearrange("(h d) c -> d h c", d=128)),
             writes=[("wgt", dr, 0)], dma=True)
        P.op("pool", lambda e, dr=dr: e.dma_start(out=wgt[dr][1][:], in_=wi_d[dr].rearrange("(h d) c -> d h c", d=128)),
             writes=[("wgt", dr, 1)], dma=True)
    m_over = A.mark()

    win_rr = [0]

    def load_win(col0):
        s = win_rr[0]
        win_rr[0] = (s + 1) % RING_IN
        src = win_d[col0 // 128]
        dst = win_t[s][:, :, :].rearrange("p k c -> p (k c)")
        P.op("pool", lambda e: e.dma_start(out=dst, in_=src), writes=[("win", s)], dma=True)
        return s

    def norm_transpose(src_d, rows, col_off, xt, g1b, ssq, rstd, diag, junk):
        for ci, (r0, n) in enumerate(rows):
            b = ci % 2
            P.op("sp", lambda e, b=b, r0=r0, n=n: e.dma_start(out=xt[b][0:n, :], in_=src_d[r0:r0 + n, :]),
                 writes=[("xt", b)], dma=True)
            P.op("pool", lambda e, b=b: e.memset(ssq[b][:, :], 0.0), writes=[("ssq", b)])
            P.op("act", lambda e, b=b, n=n: e.activation(out=junk[0:n, :], in_=xt[b][0:n, :], func=AF.Square,
                                                       accum_out=ssq[b][0:n, :]),
                 reads=[("xt", b)], writes=[("ssq", b), "junk"])
            P.op("dve", lambda e, b=b, n=n: e.tensor_scalar(out=rstd[b][0:n, :], in0=ssq[b][0:n, :], scalar1=1.0 / D,
                                                          scalar2=EPS, op0=ALU.mult, op1=ALU.add),
                 reads=[("ssq", b)], writes=[("rstd", b)])
            P.op("act", lambda e, b=b, n=n: e.activation(out=rstd[b][0:n, :], in_=rstd[b][0:n, :], func=AF.Ln),
                 reads=[("rstd", b)], writes=[("rstd", b)])
            P.op("act", lambda e, b=b, n=n: e.activation(out=rstd[b][0:n, :], in_=rstd[b][0:n, :], func=AF.Exp,
                                                       scale=-0.5),
                 reads=[("rstd", b)], writes=[("rstd", b)])
            P.op("dve", lambda e, b=b, n=n: e.tensor_scalar(out=diag[b][0:n, 0:n], in0=ident[0:n, 0:n],
                                                          scalar1=rstd[b][0:n, :], scalar2=None, op0=ALU.mult),
                 reads=[("rstd", b), "cst"], writes=[("diag", b)])
            P.op("dve", lambda e, b=b, n=n: e.tensor_tensor(out=xt[b][0:n, :], in0=xt[b][0:n, :], in1=g1b[0:n, :],
                                                          op=ALU.mult),
                 reads=[("xt", b), "g1b"], writes=[("xt", b)])
            for q in range(DC // 4):
                bk = bank()
                for j in range(4):
                    dc = q * 4 + j
                    P.op("pe", lambda e, b=b, n=n, dc=dc, bk=bk, j=j: e.matmul(
                        PS[bk][:, j * 128:j * 128 + n], lhsT=xt[b][0:n, dc * 128:(dc + 1) * 128],
                        rhs=diag[b][0:n, 0:n], start=True, stop=True),
                        reads=[("xt", b), ("diag", b)], writes=[("ps", bk)])
                src = PS[bk][:, :].rearrange("p (j t) -> p j t", j=4)[:, :, 0:n]
                dst = hT[:, q * 4:q * 4 + 4, col_off + r0:col_off + r0 + n]
                if q % 2 == 0:
                    P.op("act", lambda e, src=src, dst=dst: e.activation(out=dst, in_=src, func=AF.Copy),
                         reads=[("ps", bk)], writes=[("hT", q)])
                else:
                    P.op("dve", lambda e, src=src, dst=dst: e.tensor_copy(out=dst, in_=src),
                         reads=[("ps", bk)], writes=[("hT", q)])

    def zmm(slot, c0, ncols):
        res = []
        for (t0, n) in _chunks(ncols, 512):
            bk = bank()
            for dc in range(DC):
                P.op("pe", lambda e, bk=bk, dc=dc, t0=t0, n=n: e.matmul(
                    PS[bk][:, 0:n], lhsT=win_t[slot][:, dc, :], rhs=hT[:, dc, c0 + t0:c0 + t0 + n],
                    start=(dc == 0), stop=(dc == DC - 1)),
                    reads=[("win", slot), ("hT", dc // 4)], writes=[("ps", bk)])
            res.append((bk, t0, n))
        return res

    def lru_dir(W, h, dr, xs, base, nT, sign, init, hout, rev, hkey):
        od = O_DIR[dr]
        u, ub, r, ig, a, q = W["u"], W["ub"], W["r"], W["i"], W["a"], W["q"]
        for k in range(4):
            off = base + (k if sign > 0 else -k)
            src = xs[:, off:off + nT]
            wk = spc(od["cw"] + h * 4 + k)
            if k == 0:
                P.op("dve", lambda e, src=src, wk=wk: e.tensor_scalar(
                    out=u[:, 0:nT], in0=src, scalar1=wk, scalar2=spc(od["cb"] + h), op0=ALU.mult, op1=ALU.add),
                    reads=["xs", "smallp"], writes=["u"])
            else:
                P.op("dve", lambda e, src=src, wk=wk: e.scalar_tensor_tensor(
                    out=u[:, 0:nT], in0=src, scalar=wk, in1=u[:, 0:nT], op0=ALU.mult, op1=ALU.add),
                    reads=["xs", "u", "smallp"], writes=["u"])
        P.op("act", lambda e: e.activation(out=ub[:, 0:nT], in_=u[:, 0:nT], func=AF.Copy), reads=["u"], writes=["ub"])
        for gi, (dst, bo) in enumerate(((r, od["ba"]), (ig, od["bi"]))):
            for (t0, n) in _chunks(nT, 512):
                bk = bank()
                P.op("pe", lambda e, bk=bk, t0=t0, n=n, gi=gi: e.matmul(
                    PS[bk][:, 0:n], lhsT=wgt[dr][gi][:, h, :], rhs=ub[:, t0:t0 + n], start=True, stop=True),
                    reads=["ub", ("wgt", dr, gi)], writes=[("ps", bk)])
                P.op("act", lambda e, bk=bk, t0=t0, n=n, dst=dst, bo=bo: e.activation(
                    out=dst[:, t0:t0 + n], in_=PS[bk][:, 0:n], func=AF.Sigmoid, bias=spc(bo + h)),
                    reads=[("ps", bk), "smallp"], writes=["r" if gi == 0 else "i"])
        P.op("act", lambda e: e.activation(out=a[:, 0:nT], in_=r[:, 0:nT], func=AF.Exp, scale=cA[:, dr, h:h + 1]),
             reads=["r", ("cA", dr)], writes=["a"])
        P.op("act", lambda e: e.activation(out=q[:, 0:nT], in_=r[:, 0:nT], func=AF.Exp,
                                           scale=cA[:, dr, NH + h:NH + h + 1]),
             reads=["r", ("cA2", dr)], writes=["q"])
        P.op("act", lambda e: e.activation(out=q[:, 0:nT], in_=q[:, 0:nT], func=AF.Sqrt, scale=-1.0, bias=1.0),
             reads=["q"], writes=["q"])
        P.op("pool", lambda e: e.tensor_tensor(out=ig[:, 0:nT], in0=ig[:, 0:nT], in1=u[:, 0:nT], op=ALU.mult),
             reads=["i", "u"], writes=["i"])
        P.op("dve", lambda e: e.tensor_tensor(out=q[:, 0:nT], in0=q[:, 0:nT], in1=ig[:, 0:nT], op=ALU.mult),
             reads=["q", "i"], writes=["q"])
        if rev:
            P.op("dve", lambda e: e.tensor_tensor_scan(out=hout[:, 0:nT][:, ::-1],
                                                       data0=a[:, 0:nT][:, ::-1], data1=q[:, 0:nT][:, ::-1],
                                                       initial=init, op0=ALU.mult, op1=ALU.add),
                 reads=["a", "q"], writes=[hkey])
        else:
            P.op("dve", lambda e: e.tensor_tensor_scan(out=hout[:, 0:nT], data0=a[:, 0:nT], data1=q[:, 0:nT],
                                                       initial=init, op0=ALU.mult, op1=ALU.add),
                 reads=["a", "q", "stA"], writes=[hkey])

    xt = [A.alloc([128, D], F32, "xt") for _ in range(2)]
    g1b = A.alloc([128, D], F32, "g1b")
    junk = A.alloc([128, D], F32, "junk")
    ssq = [A.alloc([128, 1], F32, "ssq") for _ in range(2)]
    rstd = [A.alloc([128, 1], F32, "rstd") for _ in range(2)]
    diag = [A.alloc([128, 128], F32, "diag") for _ in range(2)]
    P.op("sp", lambda e: e.dma_start(out=g1b[:], in_=g1b_d), writes=["g1b"], dma=True)
    norm_transpose(xo_d, tch, 0, xt, g1b, ssq, rstd, diag, junk)
    A.release(m_over)
    P.barrier()

    WB = {}
    XSW = TW + 4
    for nm in ("xs", "u", "r", "i", "a", "q", "hf", "hb", "g1", "g2"):
        WB[nm] = A.alloc([128, XSW], F32, nm)
    WB["ub"] = A.alloc([128, XSW], BF16, "ub")
    ysp = [A.alloc([128, TH], BF16, "ysp") for _ in range(2)]
    cbuf = A.alloc([128, TW + 16], BF16, "cbuf")
    dgt = A.alloc([128, CK, 128], BF16, "dgt")
    xs = WB["xs"]
    P.op("pool", lambda e: e.memset(xs[:, :], 0.0), writes=["xs"])
    P.op("pool", lambda e: e.memset(cbuf[:, :], 0.0), writes=["cbuf"])

    pending = [load_win(h * 128) for h in range(min(2, NH))]
    for h in range(NH):
        slot = pending.pop(0)
        if h + 2 < NH:
            pending.append(load_win((h + 2) * 128))
        zs = zmm(slot, 0, TH)
        for (bk, t0, n) in zs:
            P.op("act", lambda e, bk=bk, t0=t0, n=n: e.activation(out=xs[:, 3 + t0:3 + t0 + n], in_=PS[bk][:, 0:n],
                                                                  func=AF.Copy),
                 reads=[("ps", bk)], writes=["xs"])
        pre_issue(cfg.get('PRE_PER', 3))
        lru_dir(WB, h, 0, xs, 0, TH, +1, 0.0, WB["hf"], False, "hf")
        P.op("act", lambda e, h=h: e.activation(out=stA[:, h:h + 1], in_=WB["hf"][:, TH - 1:TH], func=AF.Copy),
             reads=["hf"], writes=["stA"])
    if debug:
        P.op("sp", lambda e: e.dma_start(out=dbg_st, in_=stA[:]), reads=["stA"], dma=True)
    P.barrier()

    if STOP == "A2":
        return finish()
    m_work = A.mark()
    A.release(m_over)
    xt = [A.alloc([128, D], F32, "xt") for _ in range(2)]
    g1b2 = A.alloc([128, D], F32, "g1b")
    junk = A.alloc([128, D], F32, "junk")
    ssq = [A.alloc([128, 1], F32, "ssq") for _ in range(2)]
    rstd = [A.alloc([128, 1], F32, "rstd") for _ in range(2)]
    diag = [A.alloc([128, 128], F32, "diag") for _ in range(2)]
    P.op("sp", lambda e: e.dma_start(out=g1b2[:], in_=g1b_d), writes=["g1b"], dma=True)
    rowsB = [(0, HALO)] + [(HALO + r0, n) for (r0, n) in tch]
    norm_transpose(xn_d, rowsB, 0, xt, g1b2, ssq, rstd, diag, junk)
    P.barrier()
    A.release(m_work)
    P.op("pool", lambda e: e.memset(xs[:, :], 0.0), writes=["xs"])
    P.op("pool", lambda e: e.memset(cbuf[:, :], 0.0), writes=["cbuf"])

    units = []
    for h in range(NH):
        units.append(("x", h, h * 128))
        units.append(("g", h, LW + h * 128))
    for g in range(NG):
        units.append(("a", g, 2 * LW + g * 128))
        units.append(("b", g, 2 * LW + CW + g * 128))
    LOOK = 3
    loaded = {}
    nxt = [0]

    def ensure(i):
        while nxt[0] < len(units) and nxt[0] <= i + LOOK - 1:
            loaded[nxt[0]] = load_win(units[nxt[0]][2])
            nxt[0] += 1
        return loaded[i]

    ui = 0
    for h in range(NH):
        sx = ensure(ui); ui += 1
        zs = zmm(sx, 0, TW)
        for (bk, t0, n) in zs:
            P.op("act", lambda e, bk=bk, t0=t0, n=n: e.activation(out=xs[:, t0:t0 + n], in_=PS[bk][:, 0:n],
                                                                  func=AF.Copy),
                 reads=[("ps", bk)], writes=["xs"])
        pre_issue(cfg.get('PRE_PER', 3))
        lru_dir(WB, h, 0, xs, HALO - 3, TH, +1, stA[:, h:h + 1], WB["hf"], False, "hf")
        lru_dir(WB, h, 1, xs, HALO + 3, TH, -1, 0.0, WB["hb"], True, "hb")
        sg = ensure(ui); ui += 1
        zg = zmm(sg, HALO, TH)
        g1t, g2t = WB["g1"], WB["g2"]
        for (bk, t0, n) in zg:
            P.op("act", lambda e, bk=bk, t0=t0, n=n: e.activation(out=g1t[:, t0:t0 + n], in_=PS[bk][:, 0:n],
                                                                  func=AF.Copy),
                 reads=[("ps", bk)], writes=["g1t"])
        P.op("pool", lambda e: e.tensor_tensor(out=g2t[:, 0:TH], in0=g1t[:, 0:TH], in1=g1t[:, 0:TH], op=ALU.mult),
             reads=["g1t"], writes=["g2t"])
        P.op("dve", lambda e: e.tensor_scalar(out=g2t[:, 0:TH], in0=g2t[:, 0:TH], scalar1=0.044715, scalar2=1.0,
                                              op0=ALU.mult, op1=ALU.add), reads=["g2t"], writes=["g2t"])
        P.op("pool", lambda e: e.tensor_tensor(out=g2t[:, 0:TH], in0=g2t[:, 0:TH], in1=g1t[:, 0:TH], op=ALU.mult),
             reads=["g1t", "g2t"], writes=["g2t"])
        P.op("act", lambda e: e.activation(out=g2t[:, 0:TH], in_=g2t[:, 0:TH], func=AF.Sigmoid, scale=1.5957691216),
             reads=["g2t"], writes=["g2t"])
        P.op("dve", lambda e: e.tensor_tensor(out=g2t[:, 0:TH], in0=g2t[:, 0:TH], in1=g1t[:, 0:TH], op=ALU.mult),
             reads=["g1t", "g2t"], writes=["g2t"])
        hf, hb = WB["hf"], WB["hb"]
        P.op("pool", lambda e: e.tensor_tensor(out=hf[:, 0:TH], in0=hf[:, 0:TH], in1=hb[:, 0:TH], op=ALU.add),
             reads=["hf", "hb"], writes=["hf"])
        yb = h % 2
        P.op("dve", lambda e, yb=yb: e.tensor_tensor(out=ysp[yb][:, :], in0=hf[:, 0:TH], in1=g2t[:, 0:TH], op=ALU.mult),
             reads=["hf", "g2t"], writes=[("ysp", yb)])
        P.op("sp", lambda e, yb=yb, h=h: e.dma_start(out=yT_d[h], in_=ysp[yb][:, :]), reads=[("ysp", yb)], dma=True)

    if STOP == "B2":
        return finish()
    co, xc, sq = WB["u"], WB["r"], WB["i"]
    for g in range(NG):
        sa = ensure(ui); ui += 1
        sb = ensure(ui); ui += 1
        pre_issue(cfg.get('PRE_PER', 3))
        za = zmm(sa, 0, TW)
        zb = zmm(sb, 0, TW)
        sgt = WB["a"]
        for (bk, t0, n) in zb:
            P.op("act", lambda e, bk=bk, t0=t0, n=n: e.activation(out=sgt[:, t0:t0 + n], in_=PS[bk][:, 0:n],
                                                                  func=AF.Sigmoid),
                 reads=[("ps", bk)], writes=["sgt"])
        for (bk, t0, n) in za:
            P.op("dve", lambda e, bk=bk, t0=t0, n=n: e.tensor_tensor(out=cbuf[:, t0:t0 + n], in0=sgt[:, t0:t0 + n],
                                                                    in1=PS[bk][:, 0:n], op=ALU.mult),
                 reads=[("ps", bk), "sgt"], writes=["cbuf"])
        for k in range(CK):
            P.op("dve", lambda e, k=k, g=g: e.tensor_scalar(out=dgt[:, k, :], in0=ident, scalar1=spc(O_CW + g * CK + k),
                                                           scalar2=None, op0=ALU.mult),
                 reads=["cst", "smallp"], writes=["dgt"])
        cz = []
        for (t0, n) in _chunks(TH, 512):
            bk = bank()
            for k in range(CK):
                P.op("pe", lambda e, bk=bk, k=k, t0=t0, n=n: e.matmul(
                    PS[bk][:, 0:n], lhsT=dgt[:, k, :], rhs=cbuf[:, HALO - 15 + k + t0:HALO - 15 + k + t0 + n],
                    start=(k == 0), stop=(k == CK - 1)), reads=["dgt", "cbuf"], writes=[("ps", bk)])
            cz.append((bk, t0, n))
        for (bk, t0, n) in cz:
            P.op("act", lambda e, bk=bk, t0=t0, n=n, g=g: e.activation(out=co[:, t0:t0 + n], in_=PS[bk][:, 0:n],
                                                                       func=AF.Identity, bias=spc(O_CB + g)),
                 reads=[("ps", bk), "smallp"], writes=["co"])
        for (t0, n) in _chunks(TH, 512):
            bk = bank()
            P.op("pe", lambda e, bk=bk, t0=t0, n=n: e.matmul(PS[bk][:, 0:n], lhsT=onesM, rhs=co[:, t0:t0 + n],
                                                            start=True, stop=True),
                 reads=["co", "cst"], writes=[("ps", bk)])
            P.op("dve", lambda e, bk=bk, t0=t0, n=n: e.tensor_tensor(out=xc[:, t0:t0 + n], in0=co[:, t0:t0 + n],
                                                                    in1=PS[bk][:, 0:n], op=ALU.subtract),
                 reads=[("ps", bk), "co"], writes=["xc"])
            P.op("act", lambda e, t0=t0, n=n: e.activation(out=sq[:, t0:t0 + n], in_=xc[:, t0:t0 + n], func=AF.Square),
                 reads=["xc"], writes=["sq"])
            bk2 = bank()
            P.op("pe", lambda e, bk2=bk2, t0=t0, n=n: e.matmul(PS[bk2][:, 0:n], lhsT=onesM, rhs=sq[:, t0:t0 + n],
                                                              start=True, stop=True),
                 reads=["sq", "cst"], writes=[("ps", bk2)])
            P.op("dve", lambda e, bk2=bk2, t0=t0, n=n: e.tensor_scalar(out=sq[:, t0:t0 + n], in0=PS[bk2][:, 0:n],
                                                                      scalar1=EPS, scalar2=None, op0=ALU.add),
                 reads=[("ps", bk2)], writes=["sq"])
            P.op("act", lambda e, t0=t0, n=n: e.activation(out=sq[:, t0:t0 + n], in_=sq[:, t0:t0 + n], func=AF.Ln),
                 reads=["sq"], writes=["sq"])
            P.op("act", lambda e, t0=t0, n=n: e.activation(out=sq[:, t0:t0 + n], in_=sq[:, t0:t0 + n], func=AF.Exp,
                                                           scale=-0.5), reads=["sq"], writes=["sq"])
            P.op("pool", lambda e, t0=t0, n=n: e.tensor_tensor(out=xc[:, t0:t0 + n], in0=xc[:, t0:t0 + n],
                                                              in1=sq[:, t0:t0 + n], op=ALU.mult),
                 reads=["xc", "sq"], writes=["xc"])
        yb = g % 2
        P.op("act", lambda e, yb=yb, g=g: e.activation(out=ysp[yb][:, :], in_=xc[:, 0:TH], func=AF.Silu,
                                                       scale=spc(O_CG + g), bias=spc(O_CBETA + g)),
             reads=["xc", "smallp"], writes=[("ysp", yb)])
        P.op("sp", lambda e, yb=yb, g=g: e.dma_start(out=yT_d[NH + g], in_=ysp[yb][:, :]), reads=[("ysp", yb)],
             dma=True)
    P.barrier()

    if STOP == "B3":
        return finish()
    A.release(m_persist)
    yT = A.alloc([128, 2 * NH, TH], BF16, "yT")
    wo_t = [A.alloc([128, 2 * NH, 512], BF16, "wo") for _ in range(2)]
    xblk = [A.alloc([128, 512], F32, "xblk") for _ in range(3)]
    x1blk = [A.alloc([128, 512], F32, "x1blk") for _ in range(3)]
    jk5 = A.alloc([128, 512], F32, "jk5")
    ssq2 = A.alloc([128, NCH, DB], F32, "ssq2")
    rstd2 = A.alloc([128, NCH], F32, "rstd2")
    m_b4 = A.mark()
    for u_ in range(2 * NH):
        P.op("sp", lambda e, u_=u_: e.dma_start(out=yT[:, u_, :], in_=yT_d[u_]), writes=["yT"], dma=True)
    P.op("dve", lambda e: e.memset(ssq2[:], 0.0), writes=["ssq2"])

    def load_wo(db):
        s = db % 2
        src = wout_d[db]
        dst = wo_t[s][:, :, :].rearrange("p k c -> p (k c)")
        P.op("pool", lambda e: e.dma_start(out=dst, in_=src), writes=[("wo", s)], dma=True)

    load_wo(0)
    it = 0
    for db in range(DB):
        pre_issue((len(pre_list) - pre_pos[0] + DB - db - 1) // (DB - db))
        if db + 1 < DB:
            load_wo(db + 1)
        s = db % 2
        for ci, (r0, n) in enumerate(tch):
            b3 = it % 3
            it += 1
            P.op("sp", lambda e, b3=b3, r0=r0, n=n, db=db: e.dma_start(
                out=xblk[b3][0:n, :], in_=xn_d[HALO + r0:HALO + r0 + n, db * 512:(db + 1) * 512]),
                writes=[("xblk", b3)], dma=True)
            bk = bank()
            for cc in range(2 * NH):
                P.op("pe", lambda e, bk=bk, cc=cc, r0=r0, n=n, s=s: e.matmul(
                    PS[bk][0:n, :], lhsT=yT[:, cc, r0:r0 + n], rhs=wo_t[s][:, cc, :],
                    start=(cc == 0), stop=(cc == 2 * NH - 1)), reads=["yT", ("wo", s)], writes=[("ps", bk)])
            P.op("dve", lambda e, bk=bk, b3=b3, n=n: e.tensor_tensor(out=x1blk[b3][0:n, :], in0=xblk[b3][0:n, :],
                                                                    in1=PS[bk][0:n, :], op=ALU.add),
                 reads=[("ps", bk), ("xblk", b3)], writes=[("x1blk", b3)])
            P.op("act", lambda e, b3=b3, n=n, ci=ci, db=db: e.activation(
                out=jk5[0:n, :], in_=x1blk[b3][0:n, :], func=AF.Square, accum_out=ssq2[0:n, ci, db:db + 1]),
                reads=[("x1blk", b3)], writes=["jk5", "ssq2"])
            P.op("sp", lambda e, b3=b3, r0=r0, n=n, db=db: e.dma_start(
                out=x1_d[r0:r0 + n, db * 512:(db + 1) * 512], in_=x1blk[b3][0:n, :]),
                reads=[("x1blk", b3)], writes=["x1_d"], dma=True)
    P.op("dve", lambda e: e.tensor_reduce(out=rstd2[:, :], in_=ssq2[:, :, :], axis=AX.X, op=ALU.add),
         reads=["ssq2"], writes=["rstd2"])
    P.op("dve", lambda e: e.tensor_scalar(out=rstd2[:, :], in0=rstd2[:, :], scalar1=1.0 / D, scalar2=EPS,
                                          op0=ALU.mult, op1=ALU.add), reads=["rstd2"], writes=["rstd2"])
    P.op("act", lambda e: e.activation(out=rstd2[:, :], in_=rstd2[:, :], func=AF.Ln), reads=["rstd2"], writes=["rstd2"])
    P.op("act", lambda e: e.activation(out=rstd2[:, :], in_=rstd2[:, :], func=AF.Exp, scale=-0.5), reads=["rstd2"],
         writes=["rstd2"])
    P.barrier()

    if STOP == "B4":
        return finish()
    A.release(m_persist)
    rstd2b = A.alloc([128, NCH], F32, "rstd2b")
    P.op("dve", lambda e: e.tensor_copy(out=rstd2b[:, :], in_=rstd2[:, :]), reads=["rstd2"], writes=["rstd2b"])
    P.barrier()
    Gt = A.alloc([128, NCH, E], F32, "G")
    GHL = A.alloc([128, NCH, E, 2], BF16, "GHL")
    maskf = A.alloc([128, NCH, E], F32, "maskf")
    maskb = A.alloc([128, NCH, E], BF16, "maskb")
    pos = A.alloc([128, NCH, E], F32, "pos")
    idxi = A.alloc([128, NCH, K], I32, "idxi")
    m_tables = A.mark()
    h2 = A.alloc([128, NCH, D], BF16, "h2")
    m_moe = A.mark()
    x1c = [A.alloc([128, D], F32, "x1c") for _ in range(2)]
    hT2 = A.alloc([128, DC, 128], F32, "hT2")
    wr_t = A.alloc([128, DC, E], F32, "wr")
    diag2 = A.alloc([128, 128], F32, "diag2")
    lg = A.alloc([128, E], F32, "lg")
    wk = A.alloc([128, E], F32, "wk")
    eq = A.alloc([128, E], F32, "eq")
    mx = A.alloc([128, 8], F32, "mx")
    rk = A.alloc([128, E], F32, "rk")
    sl = A.alloc([128, E], F32, "sl")
    idxf = A.alloc([128, K], F32, "idxf")
    P.op("sp", lambda e: e.dma_start(out=wr_t[:], in_=wr_d.rearrange("(k p) c -> p k c", p=128)), writes=["wr"],
         dma=True)
    for ci, (r0, n) in enumerate(tch):
        b = ci % 2
        P.op("sp", lambda e, b=b, r0=r0, n=n: e.dma_start(out=x1c[b][0:n, :], in_=x1_d[r0:r0 + n, :]),
             reads=["x1_d"], writes=[("x1c", b)], dma=True)
        if LVL < -2:
            continue
        P.op("act", lambda e, b=b, n=n, ci=ci: e.activation(out=h2[0:n, ci, :], in_=x1c[b][0:n, :], func=AF.Identity,
                                                          scale=rstd2b[0:n, ci:ci + 1]),
             reads=[("x1c", b), "rstd2b"], writes=["h2"])
        if LVL < -1:
            continue
        P.op("dve", lambda e, n=n, ci=ci: e.tensor_scalar(out=diag2[0:n, 0:n], in0=ident[0:n, 0:n],
                                                        scalar1=rstd2b[0:n, ci:ci + 1], scalar2=None, op0=ALU.mult),
             reads=["rstd2b", "cst"], writes=["diag2"])
        for q in range(DC // 4):
            bk = bank()
            for j in range(4):
                dc = q * 4 + j
                P.op("pe", lambda e, b=b, n=n, dc=dc, bk=bk, j=j: e.matmul(
                    PS[bk][:, j * 128:j * 128 + n], lhsT=x1c[b][0:n, dc * 128:(dc + 1) * 128],
                    rhs=diag2[0:n, 0:n], start=True, stop=True),
                    reads=[("x1c", b), "diag2"], writes=[("ps", bk)])
            for j in range(4):
                dc = q * 4 + j
                eng = "act" if (j % 2 == 0 and cfg.get("EVAC", "dve") == "mix") else "dve"
                if eng == "act":
                    P.op("act", lambda e, bk=bk, j=j, dc=dc, n=n: e.activation(
                        out=hT2[:, dc, 0:n], in_=PS[bk][:, j * 128:j * 128 + n], func=AF.Identity, scale=spc(O_G2 + dc)),
                        reads=[("ps", bk), "smallp"], writes=[("hT2", dc)])
                else:
                    P.op("dve", lambda e, bk=bk, j=j, dc=dc, n=n: e.tensor_scalar(
                        out=hT2[:, dc, 0:n], in0=PS[bk][:, j * 128:j * 128 + n], scalar1=spc(O_G2 + dc), scalar2=None,
                        op0=ALU.mult), reads=[("ps", bk), "smallp"], writes=[("hT2", dc)])
        if LVL < 0:
            continue
        bk = bank()
        for dc in range(DC):
            P.op("pe", lambda e, bk=bk, dc=dc, n=n: e.matmul(PS[bk][0:n, 0:E], lhsT=hT2[:, dc, 0:n], rhs=wr_t[:, dc, :],
                                                            start=(dc == 0), stop=(dc == DC - 1)),
                 reads=[("hT2", dc), "wr"], writes=[("ps", bk)])
        P.op("dve", lambda e, bk=bk, n=n: e.tensor_tensor(out=lg[0:n, :], in0=PS[bk][0:n, 0:E], in1=brt[0:n, :],
                                                         op=ALU.add), reads=[("ps", bk), "cst"], writes=["lg"])
        if debug:
            P.op("sp", lambda e, r0=r0, n=n: e.dma_start(out=dbg_lg[r0:r0 + n, :], in_=lg[0:n, :]), reads=["lg"],
                 dma=True)
        if LVL < 1:
            continue
        P.op("dve", lambda e, n=n: e.tensor_copy(out=wk[0:n, :], in_=lg[0:n, :]), reads=["lg"], writes=["wk"])
        for kk in range(K):
            P.op("dve", lambda e, n=n, kk=kk: e.tensor_reduce(out=mx[0:n, kk:kk + 1], in_=wk[0:n, :], axis=AX.X,
                                                            op=ALU.max), reads=["wk"], writes=["mx"])
            if kk < K - 1:
                P.op("dve", lambda e, n=n, kk=kk: e.tensor_scalar(out=eq[0:n, :], in0=wk[0:n, :],
                                                                scalar1=mx[0:n, kk:kk + 1], scalar2=-1e30,
                                                                op0=ALU.is_equal, op1=ALU.mult),
                     reads=["wk", "mx"], writes=["eq"])
                P.op("dve", lambda e, n=n: e.tensor_tensor(out=wk[0:n, :], in0=wk[0:n, :], in1=eq[0:n, :], op=ALU.add),
                     reads=["wk", "eq"], writes=["wk"])
        P.op("dve", lambda e, n=n, ci=ci: e.tensor_scalar(out=maskf[0:n, ci, :], in0=lg[0:n, :],
                                                        scalar1=mx[0:n, K - 1:K], scalar2=None, op0=ALU.is_ge),
             reads=["lg", "mx"], writes=["maskf"])
        P.op("dve", lambda e, n=n, ci=ci: e.tensor_copy(out=maskb[0:n, ci, :], in_=maskf[0:n, ci, :]),
             reads=["maskf"], writes=["maskb"])
        P.op("dve", lambda e, n=n: e.tensor_scalar(out=mx[0:n, 4:5], in0=mx[0:n, 0:1], scalar1=-1.0, scalar2=None,
                                                 op0=ALU.mult), reads=["mx"], writes=["mx"])
        P.op("act", lambda e, n=n: e.activation(out=wk[0:n, :], in_=lg[0:n, :], func=AF.Exp, bias=mx[0:n, 4:5]),
             reads=["lg", "mx"], writes=["wk"])
        P.op("dve", lambda e, n=n, ci=ci: e.tensor_tensor(out=wk[0:n, :], in0=wk[0:n, :], in1=maskf[0:n, ci, :],
                                                        op=ALU.mult), reads=["wk", "maskf"], writes=["wk"])
        P.op("dve", lambda e, n=n: e.tensor_reduce(out=mx[0:n, 5:6], in_=wk[0:n, :], axis=AX.X, op=ALU.add),
             reads=["wk"], writes=["mx"])
        P.op("dve", lambda e, n=n: e.reciprocal(out=mx[0:n, 6:7], in_=mx[0:n, 5:6]), reads=["mx"], writes=["mx"])
        P.op("dve", lambda e, n=n, ci=ci: e.tensor_scalar(out=Gt[0:n, ci, :], in0=wk[0:n, :], scalar1=mx[0:n, 6:7],
                                                        scalar2=None, op0=ALU.mult),
             reads=["wk", "mx"], writes=["G"])
        if LVL < 2:
            continue
        P.op("dve", lambda e, n=n, ci=ci: e.tensor_copy(out=GHL[0:n, ci, :, 0], in_=Gt[0:n, ci, :]),
             reads=["G"], writes=["GHL"])
        P.op("dve", lambda e, n=n, ci=ci: e.tensor_tensor(out=eq[0:n, :], in0=Gt[0:n, ci, :], in1=GHL[0:n, ci, :, 0],
                                                        op=ALU.subtract), reads=["G", "GHL"], writes=["eq"])
        P.op("dve", lambda e, n=n, ci=ci: e.tensor_copy(out=GHL[0:n, ci, :, 1], in_=eq[0:n, :]),
             reads=["eq"], writes=["GHL"])
        if debug:
            P.op("sp", lambda e, r0=r0, n=n, ci=ci: e.dma_start(out=dbg_G[r0:r0 + n, :], in_=Gt[0:n, ci, :]),
                 reads=["G"], dma=True)
        if LVL < 3:
            continue
        bk = bank()
        for kc in range(ci + 1):
            m = tch[kc][1]
            lhs = onesb[0:m, 0:n] if kc < ci else Ub[0:m, 0:n]
            P.op("pe", lambda e, bk=bk, kc=kc, m=m, n=n, lhs=lhs, ci=ci: e.matmul(
                PS[bk][0:n, 0:E], lhsT=lhs, rhs=maskb[0:m, kc, :], start=(kc == 0), stop=(kc == ci)),
                reads=["maskb", "cbf"], writes=[("ps", bk)])
        P.op("act", lambda e, bk=bk, n=n, ci=ci: e.activation(out=pos[0:n, ci, :], in_=PS[bk][0:n, 0:E], func=AF.Copy),
             reads=[("ps", bk)], writes=["pos"])
        if LVL < 4:
            continue
        P.op("dve", lambda e, n=n, ci=ci: e.tensor_tensor(out=sl[0:n, :], in0=pos[0:n, ci, :], in1=eoff[0:n, :],
                                                        op=ALU.add), reads=["pos", "cst"], writes=["sl"])
        P.op("dve", lambda e, n=n, ci=ci: e.tensor_tensor_scan(out=rk[0:n, :], data0=ones1[0:n, 0:E],
                                                             data1=maskf[0:n, ci, :], initial=0.0, op0=ALU.mult,
                                                             op1=ALU.add), reads=["maskf", "cst"], writes=["rk"])
        for kk in range(K):
            P.op("dve", lambda e, n=n, ci=ci, kk=kk: e.scalar_tensor_tensor(
                out=eq[0:n, :], in0=rk[0:n, :], scalar=float(kk + 1), in1=maskf[0:n, ci, :], op0=ALU.is_equal,
                op1=ALU.mult), reads=["rk", "maskf"], writes=["eq"])
            P.op("dve", lambda e, n=n: e.tensor_tensor(out=eq[0:n, :], in0=eq[0:n, :], in1=sl[0:n, :], op=ALU.mult),
                 reads=["eq", "sl"], writes=["eq"])
            P.op("dve", lambda e, n=n, kk=kk: e.tensor_reduce(out=idxf[0:n, kk:kk + 1], in_=eq[0:n, :], axis=AX.X,
                                                            op=ALU.add), reads=["eq"], writes=["idxf"])
        P.op("dve", lambda e, n=n, ci=ci: e.tensor_copy(out=idxi[0:n, ci, :], in_=idxf[0:n, :]),
             reads=["idxf"], writes=["idxi"])
        if debug:
            P.op("sp", lambda e, r0=r0, n=n, ci=ci: e.dma_start(out=dbg_idx[r0:r0 + n, :], in_=idxi[0:n, ci, :]),
                 reads=["idxi"], dma=True)
    P.barrier()

    if STOP == "B5":
        return finish()
    A.release(m_moe)
    RING = cfg.get("RING", 5)
    wt = [A.alloc([128, 16 * 512], BF16, "wt") for _ in range(RING)]
    xT = A.alloc([128, DC, CAP], BF16, "xT")
    actT = A.alloc([128, FC, CAP], BF16, "actT")
    sel_one = A.alloc([128, NCH, CAP], BF16, "sel")
    sel = [sel_one, sel_one]
    gsl = [A.alloc([128, CC], F32, "gsl") for _ in range(2)]
    NT = 2
    hgt = [A.alloc([128, CAP], F32, "hg") for _ in range(NT)]
    sgt2 = [A.alloc([128, CAP], F32, "sg") for _ in range(NT)]
    hut = [A.alloc([128, CAP], F32, "hu") for _ in range(NT)]
    yst = [A.alloc([128, 512], F32, "yst") for _ in range(2)]
    if cfg.get("VERBOSE"):
        print("MoE phase SBUF end", A.cur, "of", A.hi)

    tiles = []
    for e_ in range(E):
        for fb in range(FB):
            tiles.append(("g", e_, fb))
            tiles.append(("u", e_, fb))
        for db in range(DB):
            tiles.append(("d", e_, db))
    tslot = {}
    tnext = [0]

    def tile_view(s, kind):
        if kind == "d":
            return wt[s][:, 0:FC * 512].rearrange("p (k c) -> p k c", k=FC)
        return wt[s][:, 0:DC * 256].rearrange("p (k c) -> p k c", k=DC)

    def ensure_tile(i):
        while tnext[0] < len(tiles) and tnext[0] <= i + RING - 2:
            j = tnext[0]
            kind, e_, blk = tiles[j]
            s = j % RING
            pre = e_ < NPRE
            if kind == "d":
                src = (wdb_d if pre else wd_d)[e_ * DB + blk]
                dst = wt[s][:, 0:FC * 512]
                pk = ("pre", "d", e_ * DB + blk)
            elif kind == "g":
                src = (wgb_d if pre else wg_d)[e_ * FB + blk]
                dst = wt[s][:, 0:DC * 256]
                pk = ("pre", "g", e_ * FB + blk)
            else:
                src = (wub_d if pre else wu_d)[e_ * FB + blk]
                dst = wt[s][:, 0:DC * 256]
                pk = ("pre", "u", e_ * FB + blk)
            P.op("pool", lambda e, dst=dst, src=src: e.dma_start(out=dst, in_=src), reads=([pk] if pre else []),
                 writes=[("wt", s)], dma=True)
            tslot[j] = s
            tnext[0] += 1
        return tslot[i]

    def build_sel(e_):
        sb = e_ % 2
        for ci, (r0, n) in enumerate(tch):
            P.op("dve", lambda e, sb=sb, ci=ci, n=n, e_=e_: e.tensor_scalar(
                out=sel[sb][0:n, ci, :], in0=iotaC[0:n, :], scalar1=pos[0:n, ci, e_:e_ + 1],
                scalar2=maskf[0:n, ci, e_:e_ + 1], op0=ALU.is_equal, op1=ALU.mult),
                reads=["pos", "maskf", "cst"], writes=["sel"])
        bk = bank()
        for cc in range(CC):
            for ci, (r0, n) in enumerate(tch):
                P.op("pe", lambda e, sb=sb, ci=ci, n=n, cc=cc, bk=bk, e_=e_: e.matmul(
                    PS[bk][:, cc * 2:cc * 2 + 2], lhsT=sel[sb][0:n, ci, cc * 128:(cc + 1) * 128],
                    rhs=GHL[0:n, ci, e_, :], start=(ci == 0 and cc == 0), stop=(ci == NCH - 1),
                    skip_group_check=True),
                    reads=["sel", "GHL"], writes=[("ps", bk)])
        for cc in range(CC):
            P.op("dve", lambda e, sb=sb, cc=cc, bk=bk: e.tensor_reduce(
                out=gsl[sb][:, cc:cc + 1], in_=PS[bk][:, cc * 2:cc * 2 + 2], axis=AX.X, op=ALU.add),
                reads=[("ps", bk)], writes=[("gsl", sb)])

    def gather(e_, dcs=None):
        sb = e_ % 2
        for dc in (range(DC) if dcs is None else dcs):
            bk = bank()
            for ci, (r0, n) in enumerate(tch):
                P.op("pe", lambda e, sb=sb, ci=ci, n=n, dc=dc, bk=bk: e.matmul(
                    PS[bk][:, 0:CAP], lhsT=h2[0:n, ci, dc * 128:(dc + 1) * 128], rhs=sel[sb][0:n, ci, :],
                    start=(ci == 0), stop=(ci == NCH - 1)), reads=["h2", "sel"], writes=[("ps", bk)])
            if False:
                pass
            else:
                P.op("dve", lambda e, dc=dc, bk=bk: e.tensor_scalar(out=xT[:, dc, :], in0=PS[bk][:, 0:CAP],
                                                                   scalar1=spc(O_G2 + dc), scalar2=None, op0=ALU.mult),
                     reads=[("ps", bk), "smallp"], writes=[("xT", dc)])

    ti = 0
    build_sel(0)
    gather(0)
    tcount = 0
    ycount = 0
    for e_ in range(E):
        for fb in range(FB):
            sg_ = ensure_tile(ti); ti += 1
            su_ = ensure_tile(ti); ti += 1
            wgv = tile_view(sg_, "g")
            wuv = tile_view(su_, "u")
            for fcl in range(2):
                fc = fb * 2 + fcl
                bg_ = bank()
                for dc in range(DC):
                    P.op("pe", lambda e, bg_=bg_, dc=dc, fcl=fcl, wgv=wgv: e.matmul(
                        PS[bg_][:, 0:CAP], lhsT=wgv[:, dc, fcl * 128:(fcl + 1) * 128], rhs=xT[:, dc, :],
                        start=(dc == 0), stop=(dc == DC - 1)), reads=[("wt", sg_), ("xT", dc)], writes=[("ps", bg_)])
                bu_ = bank()
                for dc in range(DC):
                    P.op("pe", lambda e, bu_=bu_, dc=dc, fcl=fcl, wuv=wuv: e.matmul(
                        PS[bu_][:, 0:CAP], lhsT=wuv[:, dc, fcl * 128:(fcl + 1) * 128], rhs=xT[:, dc, :],
                        start=(dc == 0), stop=(dc == DC - 1)), reads=[("wt", su_), ("xT", dc)], writes=[("ps", bu_)])
                tb = tcount % NT
                tcount += 1
                bgc = spc(O_BG + e_ * FC + fc)
                buc = spc(O_BU + e_ * FC + fc)
                P.op("dve", lambda e, tb=tb, bg_=bg_, bgc=bgc: e.tensor_scalar(
                    out=hgt[tb][:, :], in0=PS[bg_][:, 0:CAP], scalar1=bgc, scalar2=7.0, op0=ALU.add, op1=ALU.min),
                    reads=[("ps", bg_), "smallp"], writes=[("hg", tb)])
                P.op("act", lambda e, tb=tb: e.activation(out=sgt2[tb][:, :], in_=hgt[tb][:, :], func=AF.Sigmoid,
                                                         scale=1.702), reads=[("hg", tb)], writes=[("sg", tb)])
                P.op("dve", lambda e, tb=tb, bu_=bu_, buc=buc: e.tensor_scalar(
                    out=hut[tb][:, :], in0=PS[bu_][:, 0:CAP], scalar1=buc, scalar2=7.0, op0=ALU.add, op1=ALU.min),
                    reads=[("ps", bu_), "smallp"], writes=[("hu", tb)])
                P.op("dve", lambda e, tb=tb: e.tensor_scalar(out=hut[tb][:, :], in0=hut[tb][:, :], scalar1=-7.0,
                                                            scalar2=1.0, op0=ALU.max, op1=ALU.add),
                     reads=[("hu", tb)], writes=[("hu", tb)])
                P.op("dve", lambda e, tb=tb: e.tensor_tensor(out=hgt[tb][:, :], in0=hgt[tb][:, :], in1=sgt2[tb][:, :],
                                                            op=ALU.mult), reads=[("hg", tb), ("sg", tb)],
                     writes=[("hg", tb)])
                P.op("dve", lambda e, tb=tb, fc=fc: e.tensor_tensor(out=actT[:, fc, :], in0=hgt[tb][:, :],
                                                                   in1=hut[tb][:, :], op=ALU.mult),
                     reads=[("hg", tb), ("hu", tb)], writes=["actT"])
        if e_ + 1 < E:
            build_sel(e_ + 1)
        sb = e_ % 2
        for db in range(DB):
            if e_ + 1 < E:
                gather(e_ + 1, range(db * DC // DB, (db + 1) * DC // DB))
            sd_ = ensure_tile(ti); ti += 1
            wdv = tile_view(sd_, "d")
            for cc in range(CC):
                yb = ycount % 2
                ycount += 1
                bk = bank()
                for fc in range(FC):
                    P.op("pe", lambda e, bk=bk, fc=fc, cc=cc, wdv=wdv: e.matmul(
                        PS[bk][:, :], lhsT=actT[:, fc, cc * 128:(cc + 1) * 128], rhs=wdv[:, fc, :],
                        start=(fc == 0), stop=(fc == FC - 1)), reads=["actT", ("wt", sd_)], writes=[("ps", bk)])
                P.op("dve", lambda e, bk=bk, cc=cc, yb=yb, sb=sb: e.tensor_scalar(
                    out=yst[yb][:, :], in0=PS[bk][:, :], scalar1=gsl[sb][:, cc:cc + 1], scalar2=None, op0=ALU.mult),
                    reads=[("ps", bk), ("gsl", sb)], writes=[("yst", yb)])
                dst = Y_d[e_ * CAP + cc * 128:e_ * CAP + (cc + 1) * 128, db * 512:(db + 1) * 512]
                P.op("sp", lambda e, yb=yb, dst=dst: e.dma_start(out=dst, in_=yst[yb][:, :]), reads=[("yst", yb)],
                     writes=["Y_d"], dma=True)
    P.barrier()

    if STOP == "B6":
        return finish()
    A.release(m_tables)
    ga2 = [[A.alloc([128, D], F32, "ga") for _ in range(K)] for _ in range(2)]
    x1f = A.alloc([128, D], F32, "x1f")
    gfb = A.alloc([128, D], F32, "gfb")
    bdn = A.alloc([128, D], F32, "bdn")
    GT = A.alloc([128, 128], F32, "GT")
    s7 = A.alloc([128, 4], F32, "s7")
    P.op("sp", lambda e: e.dma_start(out=gfb[:], in_=gfb_d), writes=["gfb"], dma=True)
    P.op("sp", lambda e: e.dma_start(out=bdn[0:E, :], in_=bd_d), writes=["bdn"], dma=True)
    for ci, (r0, n) in enumerate(tch):
        ga = ga2[ci % 2]
        gp = ci % 2
        P.op("sp", lambda e, r0=r0, n=n: e.dma_start(out=x1f[0:n, :], in_=x1_d[r0:r0 + n, :]), reads=["x1_d"],
             writes=["x1f"], dma=True)
        for kk in range(K):
            P.op("pool", lambda e, kk=kk, n=n, ci=ci, ga=ga: e.indirect_dma_start(
                out=ga[kk][0:n, :], out_offset=None, in_=Y_d,
                in_offset=bass.IndirectOffsetOnAxis(ap=idxi[0:n, ci, kk:kk + 1], axis=0)),
                reads=["Y_d", "idxi"], writes=[("ga", gp, kk)], dma=True)
        bk = bank()
        P.op("pe", lambda e, bk=bk, n=n, ci=ci: e.matmul(PS[bk][0:E, 0:n], lhsT=Gt[0:n, ci, :], rhs=ident[0:n, 0:n],
                                                        start=True, stop=True), reads=["G", "cst"], writes=[("ps", bk)])
        P.op("act", lambda e, bk=bk, n=n: e.activation(out=GT[0:E, 0:n], in_=PS[bk][0:E, 0:n], func=AF.Copy),
             reads=[("ps", bk)], writes=["GT"])
        for db in range(DB):
            bk = bank()
            P.op("pe", lambda e, bk=bk, n=n, db=db: e.matmul(PS[bk][0:n, :], lhsT=GT[0:E, 0:n],
                                                            rhs=bdn[0:E, db * 512:(db + 1) * 512], start=True,
                                                            stop=True), reads=["GT", "bdn"], writes=[("ps", bk)])
            P.op("dve", lambda e, bk=bk, n=n, db=db: e.tensor_tensor(
                out=x1f[0:n, db * 512:(db + 1) * 512], in0=x1f[0:n, db * 512:(db + 1) * 512], in1=PS[bk][0:n, :],
                op=ALU.add), reads=[("ps", bk), "x1f"], writes=["x1f"])
        P.op("pool", lambda e, n=n, ga=ga: e.tensor_tensor(out=ga[0][0:n, :], in0=ga[0][0:n, :], in1=ga[1][0:n, :],
                                                   op=ALU.add), reads=[("ga", gp, 0), ("ga", gp, 1)], writes=[("ga", gp, 0)])
        P.op("dve", lambda e, n=n, ga=ga: e.tensor_tensor(out=ga[2][0:n, :], in0=ga[2][0:n, :], in1=ga[3][0:n, :],
                                                  op=ALU.add), reads=[("ga", gp, 2), ("ga", gp, 3)], writes=[("ga", gp, 2)])
        P.op("pool", lambda e, n=n, ga=ga: e.tensor_tensor(out=ga[0][0:n, :], in0=ga[0][0:n, :], in1=ga[2][0:n, :],
                                                   op=ALU.add), reads=[("ga", gp, 0), ("ga", gp, 2)], writes=[("ga", gp, 0)])
        P.op("dve", lambda e, n=n, ga=ga: e.tensor_tensor(out=x1f[0:n, :], in0=x1f[0:n, :], in1=ga[0][0:n, :], op=ALU.add),
             reads=[("ga", gp, 0), "x1f"], writes=["x1f"])
        P.op("dve", lambda e: e.memset(s7[:, 0:1], 0.0), writes=["s7"])
        jk7 = ga[3]
        P.op("act", lambda e, n=n, jk7=jk7: e.activation(out=jk7[0:n, :], in_=x1f[0:n, :], func=AF.Square,
                                                        accum_out=s7[0:n, 0:1]),
             reads=["x1f", ("ga", gp, 2)], writes=[("ga", gp, 3), "s7"])
        P.op("dve", lambda e, n=n: e.tensor_scalar(out=s7[0:n, 1:2], in0=s7[0:n, 0:1], scalar1=1.0 / D, scalar2=EPS,
                                                 op0=ALU.mult, op1=ALU.add), reads=["s7"], writes=["s7b"])
        P.op("act", lambda e, n=n: e.activation(out=s7[0:n, 3:4], in_=s7[0:n, 1:2], func=AF.Ln), reads=["s7b"],
             writes=["s7d"])
        P.op("act", lambda e, n=n: e.activation(out=s7[0:n, 2:3], in_=s7[0:n, 3:4], func=AF.Exp, scale=-0.5),
             reads=["s7d"], writes=["s7c"])
        P.op("dve", lambda e, n=n, jk7=jk7: e.scalar_tensor_tensor(out=jk7[0:n, :], in0=x1f[0:n, :],
                                                                 scalar=s7[0:n, 2:3], in1=gfb[0:n, :], op0=ALU.mult,
                                                                 op1=ALU.mult),
             reads=["x1f", "s7c", "gfb", ("ga", gp, 3)], writes=[("ga", gp, 3)])
        P.op("sp", lambda e, r0=r0, n=n, jk7=jk7: e.dma_start(out=out_d[r0:r0 + n, :], in_=jk7[0:n, :]),
             reads=[("ga", gp, 3)], dma=True)
    P.barrier()
    P.emit(nc)
    return nc


def _pcol(v):
    v = np.asarray(v, np.float32)
    return np.ascontiguousarray(v.reshape(-1, 128).T)


def make_in_maps(cfg, inp):
    D = cfg["D"]; SEQ = cfg["SEQ"]; B = cfg["B"]; E = cfg["E"]; CAP = cfg["CAP"]; HALO = cfg["HALO"]
    ST = SEQ + cfg["NMETA"]; TH = ST // 2
    LW = D // 2; CW = D // 2; FF = D // 2
    NH = LW // 128; NG = CW // 128; DC = D // 128; FC = FF // 128
    f32 = np.float32
    x = np.asarray(inp["x"], f32)
    meta = np.asarray(inp["meta_tokens"], f32)
    g1 = np.asarray(inp["norm1_g"], f32)[0]
    g2 = np.asarray(inp["norm2_g"], f32)[0]
    gfin = np.asarray(inp["final_norm_g"], f32)
    lcw = np.asarray(inp["lru_conv_w"], f32)[0]
    lcb = np.asarray(inp["lru_conv_b"], f32)[0]
    lwa = np.asarray(inp["lru_w_a"], f32)[0]
    lba = np.asarray(inp["lru_b_a"], f32)[0]
    lwi = np.asarray(inp["lru_w_i"], f32)[0]
    lbi = np.asarray(inp["lru_b_i"], f32)[0]
    lam = np.asarray(inp["lru_lambda"], f32)[0]
    ccw = np.asarray(inp["conf_conv_w"], f32)[0]
    ccb = np.asarray(inp["conf_conv_b"], f32)[0]
    cng = np.asarray(inp["conf_norm_g"], f32)[0]
    cnb = np.asarray(inp["conf_norm_b"], f32)[0]
    bg = np.asarray(inp["b_gate"], f32)[0]
    bu = np.asarray(inp["b_up"], f32)[0]
    brt = np.asarray(inp["b_router"], f32)[0]

    ident = np.eye(128, dtype=f32)
    U = np.triu(np.ones((128, 128), f32), 1)
    onesM = np.full((128, 128), 1.0 / 128, f32)
    ones1 = np.ones((128, 128), f32)
    iotaC = np.tile(np.arange(CAP, dtype=f32)[None, :], (128, 1))
    eoff = np.tile((np.arange(E, dtype=f32) * CAP)[None, :], (128, 1))
    brtb = np.tile(brt[None, :], (128, 1))
    consts = np.ascontiguousarray(np.concatenate([ident, U, onesM, ones1, iotaC, eoff, brtb], axis=1))

    shared = dict(
        consts=consts,
        g1b=np.ascontiguousarray(np.tile(g1[None, :], (128, 1))),
        gfb=np.ascontiguousarray(np.tile(gfin[None, :], (128, 1))),
        w_in=np.ascontiguousarray(np.asarray(inp["w_in"], f32)[0].reshape(DC, 128, 4 * LW // 128, 128)
                                  .transpose(2, 1, 0, 3)).reshape(4 * LW // 128, 128, DC * 128),
        w_out=np.ascontiguousarray(np.asarray(inp["w_out"], f32)[0].reshape(D // 128, 128, D // 512, 512)
                                   .transpose(2, 1, 0, 3)).reshape(D // 512, 128, (D // 128) * 512),
        w_router=np.asarray(inp["w_router"], f32)[0],
        w_gate=np.ascontiguousarray(np.asarray(inp["w_gate"], f32)[0].reshape(E, DC, 128, FF // 256, 256)
                                    .transpose(0, 3, 2, 1, 4)).reshape(E * (FF // 256), 128, DC * 256),
        w_up=np.ascontiguousarray(np.asarray(inp["w_up"], f32)[0].reshape(E, DC, 128, FF // 256, 256)
                                  .transpose(0, 3, 2, 1, 4)).reshape(E * (FF // 256), 128, DC * 256),
        w_down=np.ascontiguousarray(np.asarray(inp["w_down"], f32)[0].reshape(E, FC, 128, D // 512, 512)
                                    .transpose(0, 3, 2, 1, 4)).reshape(E * (D // 512), 128, FC * 512),
        b_down=np.asarray(inp["b_down"], f32)[0],
    )

    def smallp(dirs, rev_taps):
        cols = [_pcol(g1), _pcol(g2)]
        for dr in dirs:
            cw = lcw[dr].reshape(4, NH, 128)
            cols.append(np.ascontiguousarray(cw.transpose(2, 1, 0)).reshape(128, NH * 4))
            cols.append(_pcol(lcb[dr]))
            cols.append(_pcol(lba[dr].reshape(-1)))
            cols.append(_pcol(lbi[dr].reshape(-1)))
            cols.append(_pcol(lam[dr]))
        w = ccw[::-1] if rev_taps else ccw
        cw = w.reshape(31, NG, 128)
        cols.append(np.ascontiguousarray(cw.transpose(2, 1, 0)).reshape(128, NG * 31))
        cols += [_pcol(ccb), _pcol(cng), _pcol(cnb)]
        cols.append(np.ascontiguousarray(bg.reshape(E, FC, 128).transpose(2, 0, 1)).reshape(128, E * FC))
        cols.append(np.ascontiguousarray(bu.reshape(E, FC, 128).transpose(2, 0, 1)).reshape(128, E * FC))
        return np.ascontiguousarray(np.concatenate(cols, axis=1))

    maps = []
    for b in range(B):
        S = np.concatenate([meta, x[b]], axis=0)
        for half in range(2):
            Sl = S if half == 1 else S[::-1]
            dirs = (0, 1) if half == 1 else (1, 0)
            m = dict(shared)
            m["xo"] = np.ascontiguousarray(Sl[:TH])
            m["xn"] = np.ascontiguousarray(Sl[TH - HALO:])
            m["smallp"] = smallp(dirs, rev_taps=(half == 0))
            m["wa_f"] = np.ascontiguousarray(lwa[dirs[0]].reshape(NH * 128, 128))
            m["wa_b"] = np.ascontiguousarray(lwa[dirs[1]].reshape(NH * 128, 128))
            m["wi_f"] = np.ascontiguousarray(lwi[dirs[0]].reshape(NH * 128, 128))
            m["wi_b"] = np.ascontiguousarray(lwi[dirs[1]].reshape(NH * 128, 128))
            maps.append(m)
    return maps


def assemble(cfg, results):
    D = cfg["D"]; SEQ = cfg["SEQ"]; B = cfg["B"]; NM = cfg["NMETA"]
    ST = SEQ + NM; TH = ST // 2
    out = np.empty((B, SEQ, D), np.float32)
    i = 0
    for b in range(B):
        full = np.empty((ST, D), np.float32)
        for half in range(2):
            o = np.asarray(results[i]["out"], np.float32)
            i += 1
            if half == 1:
                full[TH:] = o
            else:
                full[:TH] = o[::-1]
        out[b] = full[NM:]
    return out


_NC_CACHE = {}


def run(cfg, inp, debug=False):
    key = (tuple(sorted(cfg.items())), debug)
    if key not in _NC_CACHE:
        _NC_CACHE[key] = build_program(cfg, debug=debug)
    nc = _NC_CACHE[key]
    maps = make_in_maps(cfg, inp)
    n = len(maps)
    res = run_bass_kernel_spmd(nc, maps, core_ids=list(range(n)))
    return res


def kernel(**inputs):
    cfg = dict(REAL_CFG)
    res = run(cfg, inputs)
    return assemble(cfg, res.results)
```

```python
import numpy as np
import concourse.bass as bass
import concourse.mybir as mybir
from concourse.bass_utils import run_bass_kernel_spmd

F32 = mybir.dt.float32
BF16 = mybir.dt.bfloat16
I32 = mybir.dt.int32
AF = mybir.ActivationFunctionType
ALU = mybir.AluOpType
AX = mybir.AxisListType

REAL_CFG = dict(D=4096, SEQ=2048, B=4, E=32, K=4, CAP=256, HALO=16, NMETA=16)


class _Op:
    __slots__ = ("eng", "fn", "deps", "signal", "sigval", "dma", "dsem", "dtarget", "idx")

    def __init__(self, eng, fn, dma):
        self.eng = eng
        self.fn = fn
        self.deps = []
        self.signal = False
        self.sigval = 0
        self.dma = dma
        self.dsem = None
        self.dtarget = 0


class Prog:
    ENGS = ("pe", "act", "dve", "pool", "sp")
    NDSEM = 8

    def __init__(self):
        self.ops = {e: [] for e in self.ENGS}
        self.last_write = {}
        self.readers = {}
        self.dma_rr = {e: 0 for e in self.ENGS}
        self.dma_last = {}
        self.dma_count = {}
        self.n = 0

    def op(self, eng, fn, reads=(), writes=(), dma=False):
        o = _Op(eng, fn, dma)
        o.idx = self.n
        self.n += 1
        deps = {}
        for k in reads:
            lw = self.last_write.get(k)
            if lw is not None:
                deps[id(lw)] = lw
        for k in writes:
            lw = self.last_write.get(k)
            if lw is not None:
                deps[id(lw)] = lw
            for r in self.readers.get(k, {}).values():
                deps[id(r)] = r
        rkey = eng
        if dma:
            i = self.dma_rr[eng]
            self.dma_rr[eng] = (i + 1) % self.NDSEM
            o.dsem = (eng, i)
            prev = self.dma_last.get(o.dsem)
            if prev is not None:
                deps[id(prev)] = prev
            self.dma_last[o.dsem] = o
            c = self.dma_count.get(o.dsem, 0) + 1
            self.dma_count[o.dsem] = c
            o.dtarget = 16 * c
            rkey = o.dsem
        for d in deps.values():
            if d is o:
                continue
            if (not d.dma) and d.eng == "pe" and eng == "pe" and not dma:
                continue
            o.deps.append(d)
        for k in reads:
            self.readers.setdefault(k, {})[rkey] = o
        for k in writes:
            self.last_write[k] = o
            self.readers[k] = {}
        self.ops[eng].append(o)
        return o

    def barrier(self):
        lasts = []
        for e in self.ENGS:
            for o in reversed(self.ops[e]):
                if not o.dma and o.fn is not None:
                    lasts.append(o)
                    break
        lasts += list(self.dma_last.values())
        for e in self.ENGS:
            o = _Op(e, None, False)
            o.idx = self.n
            self.n += 1
            o.deps = [d for d in lasts if not (d.eng == e and not d.dma and e == "pe")]
            self.ops[e].append(o)

    def emit(self, nc):
        for e in self.ENGS:
            for o in self.ops[e]:
                for d in o.deps:
                    d.signal = True
        for e in self.ENGS:
            c = 0
            for o in self.ops[e]:
                if o.signal and not o.dma:
                    c += 1
                    o.sigval = c
        from contextlib import ExitStack
        with ExitStack() as st:
            esem = {e: st.enter_context(nc.semaphore("es_" + e)) for e in self.ENGS}
            dsem = {}
            for e in self.ENGS:
                if self.dma_count and any(k[0] == e for k in self.dma_count):
                    for i in range(self.NDSEM):
                        dsem[(e, i)] = st.enter_context(nc.semaphore("ds_%s%d" % (e, i)))
            block = st.enter_context(nc.Block())

            def run(eng_name, e):
                waited = {}
                for o in self.ops[eng_name]:
                    for d in o.deps:
                        if d.dma:
                            sem, val, key = dsem[d.dsem], d.dtarget, d.dsem
                        else:
                            sem, val, key = esem[d.eng], d.sigval, d.eng
                        if waited.get(key, 0) < val:
                            e.wait_ge(sem, val)
                            waited[key] = val
                    if o.fn is None:
                        continue
                    inst = o.fn(e)
                    if o.dma:
                        inst.then_inc(dsem[o.dsem], 16)
                    elif o.signal:
                        inst.then_inc(esem[eng_name], 1)

            @block.tensor
            def _(e):
                run("pe", e)

            @block.scalar
            def _(e):
                run("act", e)

            @block.vector
            def _(e):
                run("dve", e)

            @block.gpsimd
            def _(e):
                run("pool", e)

            @block.sync
            def _(e):
                run("sp", e)


class Arena:
    def __init__(self, nc, lo=16512, hi=229344):
        self.nc = nc
        self.lo = lo
        self.hi = hi
        self.cur = lo
        self.cnt = 0

    def alloc(self, shape, dtype, name="t"):
        sz = 4 if dtype in (F32, I32) else 2
        n = 1
        for s in shape[1:]:
            n *= s
        nbytes = (n * sz + 63) // 64 * 64
        off = self.cur
        assert off + nbytes <= self.hi, "SBUF arena overflow: %s %s need %d at %d" % (name, shape, nbytes, off)
        self.cur += nbytes
        self.cnt += 1
        return self.nc.alloc_sbuf_tensor_at("%s_%d" % (name, self.cnt), list(shape), dtype, offset=off)

    def mark(self):
        return self.cur

    def release(self, m):
        self.cur = m


def _chunks(total, size=128):
    out = []
    s = 0
    while s < total:
        n = min(size, total - s)
        out.append((s, n))
        s += n
    return out


def build_program(cfg, debug=False):
    D = cfg["D"]; SEQ = cfg["SEQ"]; E = cfg["E"]; K = cfg["K"]; CAP = cfg["CAP"]; HALO = cfg["HALO"]
    ST = SEQ + cfg["NMETA"]
    TH = ST // 2
    LW = D // 2; CW = D // 2; FF = D // 2
    NH = LW // 128; NG = CW // 128; DC = D // 128; FC = FF // 128; CC = CAP // 128
    CK = 31
    TW = HALO + TH
    DB = D // 512
    FB = FF // 256
    tch = _chunks(TH)
    NCH = len(tch)
    EPS = 1e-5

    nc = bass.Bass("TRN2", target_bir_lowering=False)
    P = Prog()
    A = Arena(nc)

    STOP = cfg.get("STOP", "")
    LVL = cfg.get("LVL", 9)

    def finish():
        P.barrier()
        P.emit(nc)
        return nc

    def din(name, shape, dt=F32):
        return nc.dram_tensor(name, list(shape), dt, kind="ExternalInput").ap()

    xo_d = din("xo", [TH, D])
    xn_d = din("xn", [TW, D])
    NSP = (2 * DC + 2 * (NH * 4 + 4 * NH) + NG * CK + 3 * NG + 2 * E * FC)
    sp_d = din("smallp", [128, NSP])
    NCST = 128 * 4 + CAP + 2 * E
    cst_d = din("consts", [128, NCST])
    g1b_d = din("g1b", [128, D])
    gfb_d = din("gfb", [128, D])
    NU = 4 * LW // 128
    win_d = din("w_in", [NU, 128, DC * 128])
    wa_d = [din("wa_f", [NH * 128, 128]), din("wa_b", [NH * 128, 128])]
    wi_d = [din("wi_f", [NH * 128, 128]), din("wi_b", [NH * 128, 128])]
    wout_d = din("w_out", [D // 512, 128, 2 * NH * 512])
    wr_d = din("w_router", [D, E])
    wg_d = din("w_gate", [E * (FF // 256), 128, (D // 128) * 256])
    wu_d = din("w_up", [E * (FF // 256), 128, (D // 128) * 256])
    wd_d = din("w_down", [E * (D // 512), 128, (FF // 128) * 512])
    bd_d = din("b_down", [E, D])
    out_d = nc.dram_tensor("out", [TH, D], F32, kind="ExternalOutput").ap()
    skind = "ExternalOutput" if debug else "Internal"
    yT_d = nc.dram_tensor("yT_scr", [2 * NH, 128, TH], BF16, kind=skind).ap()
    x1_d = nc.dram_tensor("x1_scr", [TH, D], F32, kind=skind).ap()
    Y_d = nc.dram_tensor("Y_scr", [E * CAP, D], F32, kind=skind).ap()
    NPRE = cfg.get("NPRE", 5)
    wgb_d = nc.dram_tensor("wg_bf", [NPRE * (FF // 256), 128, (D // 128) * 256], BF16, kind="Internal").ap()
    wub_d = nc.dram_tensor("wu_bf", [NPRE * (FF // 256), 128, (D // 128) * 256], BF16, kind="Internal").ap()
    wdb_d = nc.dram_tensor("wd_bf", [NPRE * (D // 512), 128, (FF // 128) * 512], BF16, kind="Internal").ap()
    pre_list = []
    for e_ in range(NPRE):
        for fb in range(FF // 256):
            pre_list.append(("g", e_ * (FF // 256) + fb))
            pre_list.append(("u", e_ * (FF // 256) + fb))
        for db in range(D // 512):
            pre_list.append(("d", e_ * (D // 512) + db))
    pre_pos = [0]

    def pre_issue(n):
        for _ in range(n):
            if pre_pos[0] >= len(pre_list):
                return
            kind, idx = pre_list[pre_pos[0]]
            pre_pos[0] += 1
            src = {"g": wg_d, "u": wu_d, "d": wd_d}[kind][idx]
            dst = {"g": wgb_d, "u": wub_d, "d": wdb_d}[kind][idx]
            P.op("pool", lambda e, src=src, dst=dst: e.dma_start(out=dst, in_=src), writes=[("pre", kind, idx)],
                 dma=True)

    if debug:
        dbg_lg = nc.dram_tensor("dbg_logits", [TH, E], F32, kind="ExternalOutput").ap()
        dbg_G = nc.dram_tensor("dbg_G", [TH, E], F32, kind="ExternalOutput").ap()
        dbg_idx = nc.dram_tensor("dbg_idx", [TH, K], I32, kind="ExternalOutput").ap()
        dbg_st = nc.dram_tensor("dbg_state", [128, NH], F32, kind="ExternalOutput").ap()

    PS = [nc.alloc_psum_tensor("ps%d" % i, [128, 512], F32) for i in range(8)]
    ps_rr = [0]

    def bank():
        i = ps_rr[0]
        ps_rr[0] = (i + 1) % 8
        return i

    smallp = A.alloc([128, NSP], F32, "smallp")
    cst = A.alloc([128, NCST], F32, "consts")
    cbf = A.alloc([128, 3 * 128], BF16, "cbf")
    stA = A.alloc([128, NH], F32, "stateA")
    cA = A.alloc([128, 2, 2 * NH], F32, "cA")
    P.op("sp", lambda e: e.dma_start(out=smallp[:], in_=sp_d), writes=["smallp"], dma=True)
    P.op("sp", lambda e: e.dma_start(out=cst[:], in_=cst_d), writes=["cst"], dma=True)
    ident = cst[:, 0:128]
    Utri = cst[:, 128:256]
    onesM = cst[:, 256:384]
    ones1 = cst[:, 384:512]
    iotaC = cst[:, 512:512 + CAP]
    eoff = cst[:, 512 + CAP:512 + CAP + E]
    brt = cst[:, 512 + CAP + E:512 + CAP + 2 * E]
    P.op("dve", lambda e: e.tensor_copy(out=cbf[:, 0:128], in_=ident), reads=["cst"], writes=["cbf"])
    P.op("dve", lambda e: e.tensor_copy(out=cbf[:, 128:256], in_=Utri), reads=["cst"], writes=["cbf"])
    P.op("dve", lambda e: e.tensor_copy(out=cbf[:, 256:384], in_=ones1), reads=["cst"], writes=["cbf"])
    identb = cbf[:, 0:128]
    Ub = cbf[:, 128:256]
    onesb = cbf[:, 256:384]

    o = 0
    O_G1 = o; o += DC
    O_G2 = o; o += DC
    O_DIR = []
    for _ in range(2):
        d = {}
        d["cw"] = o; o += NH * 4
        d["cb"] = o; o += NH
        d["ba"] = o; o += NH
        d["bi"] = o; o += NH
        d["lam"] = o; o += NH
        O_DIR.append(d)
    O_CW = o; o += NG * CK
    O_CB = o; o += NG
    O_CG = o; o += NG
    O_CBETA = o; o += NG
    O_BG = o; o += E * FC
    O_BU = o; o += E * FC
    assert o == NSP

    def spc(off, n=1):
        return smallp[:, off:off + n]

    for dr in range(2):
        lam = spc(O_DIR[dr]["lam"], NH)
        P.op("act", lambda e, dr=dr, lam=lam: e.activation(out=cA[:, dr, 0:NH], in_=lam, func=AF.Exp, scale=-1.0),
             reads=["smallp"], writes=[("cA", dr)])
        P.op("act", lambda e, dr=dr: e.activation(out=cA[:, dr, 0:NH], in_=cA[:, dr, 0:NH], func=AF.Ln, bias=1.0),
             reads=[("cA", dr)], writes=[("cA", dr)])
        P.op("dve", lambda e, dr=dr: e.tensor_scalar(out=cA[:, dr, NH:2 * NH], in0=cA[:, dr, 0:NH], scalar1=-16.0,
                                                     scalar2=None, op0=ALU.mult),
             reads=[("cA", dr)], writes=[("cA2", dr)])
        P.op("dve", lambda e, dr=dr: e.tensor_scalar(out=cA[:, dr, 0:NH], in0=cA[:, dr, 0:NH], scalar1=-8.0,
                                                     scalar2=None, op0=ALU.mult),
             reads=[("cA", dr), ("cA2", dr)], writes=[("cA", dr)])

    m_persist = A.mark()

    hT = A.alloc([128, DC, TW], BF16, "hT")
    RING_IN = 4
    win_t = [A.alloc([128, DC, 128], BF16, "win") for _ in range(RING_IN)]
    wgt = [[A.alloc([128, NH, 128], BF16, "wa"), A.alloc([128, NH, 128], BF16, "wi")] for _ in range(2)]
    for dr in range(2):
        P.op("pool", lambda e, dr=dr: e.dma_start(out=wgt[dr][0][:], in_=wa_d[dr].rearrange("(h d) c -> d h c", d=128)),
             writes=[("wgt", dr, 0)], dma=True)
        P.op("pool", lambda e, dr=dr: e.dma_start(out=wgt[dr][1][:], in_=wi_d[dr].rearrange("(h d) c -> d h c", d=128)),
             writes=[("wgt", dr, 1)], dma=True)
    m_over = A.mark()

    win_rr = [0]

    def load_win(col0):
        s = win_rr[0]
        win_rr[0] = (s + 1) % RING_IN
        src = win_d[col0 // 128]
        dst = win_t[s][:, :, :].rearrange("p k c -> p (k c)")
        P.op("pool", lambda e: e.dma_start(out=dst, in_=src), writes=[("win", s)], dma=True)
        return s

    def norm_transpose(src_d, rows, col_off, xt, g1b, ssq, rstd, diag, junk):
        for ci, (r0, n) in enumerate(rows):
            b = ci % 2
            P.op("sp", lambda e, b=b, r0=r0, n=n: e.dma_start(out=xt[b][0:n, :], in_=src_d[r0:r0 + n, :]),
                 writes=[("xt", b)], dma=True)
            P.op("pool", lambda e, b=b: e.memset(ssq[b][:, :], 0.0), writes=[("ssq", b)])
            P.op("act", lambda e, b=b, n=n: e.activation(out=junk[0:n, :], in_=xt[b][0:n, :], func=AF.Square,
                                                       accum_out=ssq[b][0:n, :]),
                 reads=[("xt", b)], writes=[("ssq", b), "junk"])
            P.op("dve", lambda e, b=b, n=n: e.tensor_scalar(out=rstd[b][0:n, :], in0=ssq[b][0:n, :], scalar1=1.0 / D,
                                                          scalar2=EPS, op0=ALU.mult, op1=ALU.add),
                 reads=[("ssq", b)], writes=[("rstd", b)])
            P.op("act", lambda e, b=b, n=n: e.activation(out=rstd[b][0:n, :], in_=rstd[b][0:n, :], func=AF.Ln),
                 reads=[("rstd", b)], writes=[("rstd", b)])
            P.op("act", lambda e, b=b, n=n: e.activation(out=rstd[b][0:n, :], in_=rstd[b][0:n, :], func=AF.Exp,
                                                       scale=-0.5),
                 reads=[("rstd", b)], writes=[("rstd", b)])
            P.op("dve", lambda e, b=b, n=n: e.tensor_scalar(out=diag[b][0:n, 0:n], in0=ident[0:n, 0:n],
                                                          scalar1=rstd[b][0:n, :], scalar2=None, op0=ALU.mult),
                 reads=[("rstd", b), "cst"], writes=[("diag", b)])
            P.op("dve", lambda e, b=b, n=n: e.tensor_tensor(out=xt[b][0:n, :], in0=xt[b][0:n, :], in1=g1b[0:n, :],
                                                          op=ALU.mult),
                 reads=[("xt", b), "g1b"], writes=[("xt", b)])
            for q in range(DC // 4):
                bk = bank()
                for j in range(4):
                    dc = q * 4 + j
                    P.op("pe", lambda e, b=b, n=n, dc=dc, bk=bk, j=j: e.matmul(
                        PS[bk][:, j * 128:j * 128 + n], lhsT=xt[b][0:n, dc * 128:(dc + 1) * 128],
                        rhs=diag[b][0:n, 0:n], start=True, stop=True),
                        reads=[("xt", b), ("diag", b)], writes=[("ps", bk)])
                src = PS[bk][:, :].rearrange("p (j t) -> p j t", j=4)[:, :, 0:n]
                dst = hT[:, q * 4:q * 4 + 4, col_off + r0:col_off + r0 + n]
                if q % 2 == 0:
                    P.op("act", lambda e, src=src, dst=dst: e.activation(out=dst, in_=src, func=AF.Copy),
                         reads=[("ps", bk)], writes=[("hT", q)])
                else:
                    P.op("dve", lambda e, src=src, dst=dst: e.tensor_copy(out=dst, in_=src),
                         reads=[("ps", bk)], writes=[("hT", q)])

    def zmm(slot, c0, ncols):
        res = []
        for (t0, n) in _chunks(ncols, 512):
            bk = bank()
            for dc in range(DC):
                P.op("pe", lambda e, bk=bk, dc=dc, t0=t0, n=n: e.matmul(
                    PS[bk][:, 0:n], lhsT=win_t[slot][:, dc, :], rhs=hT[:, dc, c0 + t0:c0 + t0 + n],
                    start=(dc == 0), stop=(dc == DC - 1)),
                    reads=[("win", slot), ("hT", dc // 4)], writes=[("ps", bk)])
            res.append((bk, t0, n))
        return res

    def lru_dir(W, h, dr, xs, base, nT, sign, init, hout, rev, hkey):
        od = O_DIR[dr]
        u, ub, r, ig, a, q = W["u"], W["ub"], W["r"], W["i"], W["a"], W["q"]
        for k in range(4):
            off = base + (k if sign > 0 else -k)
            src = xs[:, off:off + nT]
            wk = spc(od["cw"] + h * 4 + k)
            if k == 0:
                P.op("dve", lambda e, src=src, wk=wk: e.tensor_scalar(
                    out=u[:, 0:nT], in0=src, scalar1=wk, scalar2=spc(od["cb"] + h), op0=ALU.mult, op1=ALU.add),
                    reads=["xs", "smallp"], writes=["u"])
            else:
                P.op("dve", lambda e, src=src, wk=wk: e.scalar_tensor_tensor(
                    out=u[:, 0:nT], in0=src, scalar=wk, in1=u[:, 0:nT], op0=ALU.mult, op1=ALU.add),
                    reads=["xs", "u", "smallp"], writes=["u"])
        P.op("act", lambda e: e.activation(out=ub[:, 0:nT], in_=u[:, 0:nT], func=AF.Copy), reads=["u"], writes=["ub"])
        for gi, (dst, bo) in enumerate(((r, od["ba"]), (ig, od["bi"]))):
            for (t0, n) in _chunks(nT, 512):
                bk = bank()
                P.op("pe", lambda e, bk=bk, t0=t0, n=n, gi=gi: e.matmul(
                    PS[bk][:, 0:n], lhsT=wgt[dr][gi][:, h, :], rhs=ub[:, t0:t0 + n], start=True, stop=True),
                    reads=["ub", ("wgt", dr, gi)], writes=[("ps", bk)])
                P.op("act", lambda e, bk=bk, t0=t0, n=n, dst=dst, bo=bo: e.activation(
                    out=dst[:, t0:t0 + n], in_=PS[bk][:, 0:n], func=AF.Sigmoid, bias=spc(bo + h)),
                    reads=[("ps", bk), "smallp"], writes=["r" if gi == 0 else "i"])
        P.op("act", lambda e: e.activation(out=a[:, 0:nT], in_=r[:, 0:nT], func=AF.Exp, scale=cA[:, dr, h:h + 1]),
             reads=["r", ("cA", dr)], writes=["a"])
        P.op("act", lambda e: e.activation(out=q[:, 0:nT], in_=r[:, 0:nT], func=AF.Exp,
                                           scale=cA[:, dr, NH + h:NH + h + 1]),
             reads=["r", ("cA2", dr)], writes=["q"])
        P.op("act", lambda e: e.activation(out=q[:, 0:nT], in_=q[:, 0:nT], func=AF.Sqrt, scale=-1.0, bias=1.0),
             reads=["q"], writes=["q"])
        P.op("pool", lambda e: e.tensor_tensor(out=ig[:, 0:nT], in0=ig[:, 0:nT], in1=u[:, 0:nT], op=ALU.mult),
             reads=["i", "u"], writes=["i"])
        P.op("dve", lambda e: e.tensor_tensor(out=q[:, 0:nT], in0=q[:, 0:nT], in1=ig[:, 0:nT], op=ALU.mult),
             reads=["q", "i"], writes=["q"])
        if rev:
            P.op("dve", lambda e: e.tensor_tensor_scan(out=hout[:, 0:nT][:, ::-1],
                                                       data0=a[:, 0:nT][:, ::-1], data1=q[:, 0:nT][:, ::-1],
                                                       initial=init, op0=ALU.mult, op1=ALU.add),
                 reads=["a", "q"], writes=[hkey])
        else:
            P.op("dve", lambda e: e.tensor_tensor_scan(out=hout[:, 0:nT], data0=a[:, 0:nT], data1=q[:, 0:nT],
                                                       initial=init, op0=ALU.mult, op1=ALU.add),
                 reads=["a", "q", "stA"], writes=[hkey])

    xt = [A.alloc([128, D], F32, "xt") for _ in range(2)]
    g1b = A.alloc([128, D], F32, "g1b")
    junk = A.alloc([128, D], F32, "junk")
    ssq = [A.alloc([128, 1], F32, "ssq") for _ in range(2)]
    rstd = [A.alloc([128, 1], F32, "rstd") for _ in range(2)]
    diag = [A.alloc([128, 128], F32, "diag") for _ in range(2)]
    P.op("sp", lambda e: e.dma_start(out=g1b[:], in_=g1b_d), writes=["g1b"], dma=True)
    norm_transpose(xo_d, tch, 0, xt, g1b, ssq, rstd, diag, junk)
    A.release(m_over)
    P.barrier()

    WB = {}
    XSW = TW + 4
    for nm in ("xs", "u", "r", "i", "a", "q", "hf", "hb", "g1", "g2"):
        WB[nm] = A.alloc([128, XSW], F32, nm)
    WB["ub"] = A.alloc([128, XSW], BF16, "ub")
    ysp = [A.alloc([128, TH], BF16, "ysp") for _ in range(2)]
    cbuf = A.alloc([128, TW + 16], BF16, "cbuf")
    dgt = A.alloc([128, CK, 128], BF16, "dgt")
    xs = WB["xs"]
    P.op("pool", lambda e: e.memset(xs[:, :], 0.0), writes=["xs"])
    P.op("pool", lambda e: e.memset(cbuf[:, :], 0.0), writes=["cbuf"])

    pending = [load_win(h * 128) for h in range(min(2, NH))]
    for h in range(NH):
        slot = pending.pop(0)
        if h + 2 < NH:
            pending.append(load_win((h + 2) * 128))
        zs = zmm(slot, 0, TH)
        for (bk, t0, n) in zs:
            P.op("act", lambda e, bk=bk, t0=t0, n=n: e.activation(out=xs[:, 3 + t0:3 + t0 + n], in_=PS[bk][:, 0:n],
                                                                  func=AF.Copy),
                 reads=[("ps", bk)], writes=["xs"])
        pre_issue(1)
        lru_dir(WB, h, 0, xs, 0, TH, +1, 0.0, WB["hf"], False, "hf")
        P.op("act", lambda e, h=h: e.activation(out=stA[:, h:h + 1], in_=WB["hf"][:, TH - 1:TH], func=AF.Copy),
             reads=["hf"], writes=["stA"])
    if debug:
        P.op("sp", lambda e: e.dma_start(out=dbg_st, in_=stA[:]), reads=["stA"], dma=True)
    P.barrier()

    if STOP == "A2":
        return finish()
    m_work = A.mark()
    A.release(m_over)
    xt = [A.alloc([128, D], F32, "xt") for _ in range(2)]
    g1b2 = A.alloc([128, D], F32, "g1b")
    junk = A.alloc([128, D], F32, "junk")
    ssq = [A.alloc([128, 1], F32, "ssq") for _ in range(2)]
    rstd = [A.alloc([128, 1], F32, "rstd") for _ in range(2)]
    diag = [A.alloc([128, 128], F32, "diag") for _ in range(2)]
    P.op("sp", lambda e: e.dma_start(out=g1b2[:], in_=g1b_d), writes=["g1b"], dma=True)
    rowsB = [(0, HALO)] + [(HALO + r0, n) for (r0, n) in tch]
    norm_transpose(xn_d, rowsB, 0, xt, g1b2, ssq, rstd, diag, junk)
    P.barrier()
    A.release(m_work)
    P.op("pool", lambda e: e.memset(xs[:, :], 0.0), writes=["xs"])
    P.op("pool", lambda e: e.memset(cbuf[:, :], 0.0), writes=["cbuf"])

    units = []
    for h in range(NH):
        units.append(("x", h, h * 128))
        units.append(("g", h, LW + h * 128))
    for g in range(NG):
        units.append(("a", g, 2 * LW + g * 128))
        units.append(("b", g, 2 * LW + CW + g * 128))
    LOOK = 3
    loaded = {}
    nxt = [0]

    def ensure(i):
        while nxt[0] < len(units) and nxt[0] <= i + LOOK - 1:
            loaded[nxt[0]] = load_win(units[nxt[0]][2])
            nxt[0] += 1
        return loaded[i]

    ui = 0
    for h in range(NH):
        sx = ensure(ui); ui += 1
        zs = zmm(sx, 0, TW)
        for (bk, t0, n) in zs:
            P.op("act", lambda e, bk=bk, t0=t0, n=n: e.activation(out=xs[:, t0:t0 + n], in_=PS[bk][:, 0:n],
                                                                  func=AF.Copy),
                 reads=[("ps", bk)], writes=["xs"])
        pre_issue(cfg.get('PRE_PER', 3))
        lru_dir(WB, h, 0, xs, HALO - 3, TH, +1, stA[:, h:h + 1], WB["hf"], False, "hf")
        lru_dir(WB, h, 1, xs, HALO + 3, TH, -1, 0.0, WB["hb"], True, "hb")
        sg = ensure(ui); ui += 1
        zg = zmm(sg, HALO, TH)
        g1t, g2t = WB["g1"], WB["g2"]
        for (bk, t0, n) in zg:
            P.op("act", lambda e, bk=bk, t0=t0, n=n: e.activation(out=g1t[:, t0:t0 + n], in_=PS[bk][:, 0:n],
                                                                  func=AF.Copy),
                 reads=[("ps", bk)], writes=["g1t"])
        P.op("pool", lambda e: e.tensor_tensor(out=g2t[:, 0:TH], in0=g1t[:, 0:TH], in1=g1t[:, 0:TH], op=ALU.mult),
             reads=["g1t"], writes=["g2t"])
        P.op("dve", lambda e: e.tensor_scalar(out=g2t[:, 0:TH], in0=g2t[:, 0:TH], scalar1=0.044715, scalar2=1.0,
                                              op0=ALU.mult, op1=ALU.add), reads=["g2t"], writes=["g2t"])
        P.op("pool", lambda e: e.tensor_tensor(out=g2t[:, 0:TH], in0=g2t[:, 0:TH], in1=g1t[:, 0:TH], op=ALU.mult),
             reads=["g1t", "g2t"], writes=["g2t"])
        P.op("act", lambda e: e.activation(out=g2t[:, 0:TH], in_=g2t[:, 0:TH], func=AF.Sigmoid, scale=1.5957691216),
             reads=["g2t"], writes=["g2t"])
        P.op("dve", lambda e: e.tensor_tensor(out=g2t[:, 0:TH], in0=g2t[:, 0:TH], in1=g1t[:, 0:TH], op=ALU.mult),
             reads=["g1t", "g2t"], writes=["g2t"])
        hf, hb = WB["hf"], WB["hb"]
        P.op("pool", lambda e: e.tensor_tensor(out=hf[:, 0:TH], in0=hf[:, 0:TH], in1=hb[:, 0:TH], op=ALU.add),
             reads=["hf", "hb"], writes=["hf"])
        yb = h % 2
        P.op("dve", lambda e, yb=yb: e.tensor_tensor(out=ysp[yb][:, :], in0=hf[:, 0:TH], in1=g2t[:, 0:TH], op=ALU.mult),
             reads=["hf", "g2t"], writes=[("ysp", yb)])
        P.op("sp", lambda e, yb=yb, h=h: e.dma_start(out=yT_d[h], in_=ysp[yb][:, :]), reads=[("ysp", yb)], dma=True)

    if STOP == "B2":
        return finish()
    co, xc, sq = WB["u"], WB["r"], WB["i"]
    for g in range(NG):
        sa = ensure(ui); ui += 1
        sb = ensure(ui); ui += 1
        pre_issue(4)
        za = zmm(sa, 0, TW)
        zb = zmm(sb, 0, TW)
        sgt = WB["a"]
        for (bk, t0, n) in zb:
            P.op("act", lambda e, bk=bk, t0=t0, n=n: e.activation(out=sgt[:, t0:t0 + n], in_=PS[bk][:, 0:n],
                                                                  func=AF.Sigmoid),
                 reads=[("ps", bk)], writes=["sgt"])
        for (bk, t0, n) in za:
            P.op("dve", lambda e, bk=bk, t0=t0, n=n: e.tensor_tensor(out=cbuf[:, t0:t0 + n], in0=sgt[:, t0:t0 + n],
                                                                    in1=PS[bk][:, 0:n], op=ALU.mult),
                 reads=[("ps", bk), "sgt"], writes=["cbuf"])
        for k in range(CK):
            P.op("dve", lambda e, k=k, g=g: e.tensor_scalar(out=dgt[:, k, :], in0=ident, scalar1=spc(O_CW + g * CK + k),
                                                           scalar2=None, op0=ALU.mult),
                 reads=["cst", "smallp"], writes=["dgt"])
        cz = []
        for (t0, n) in _chunks(TH, 512):
            bk = bank()
            for k in range(CK):
                P.op("pe", lambda e, bk=bk, k=k, t0=t0, n=n: e.matmul(
                    PS[bk][:, 0:n], lhsT=dgt[:, k, :], rhs=cbuf[:, HALO - 15 + k + t0:HALO - 15 + k + t0 + n],
                    start=(k == 0), stop=(k == CK - 1)), reads=["dgt", "cbuf"], writes=[("ps", bk)])
            cz.append((bk, t0, n))
        for (bk, t0, n) in cz:
            P.op("act", lambda e, bk=bk, t0=t0, n=n, g=g: e.activation(out=co[:, t0:t0 + n], in_=PS[bk][:, 0:n],
                                                                       func=AF.Identity, bias=spc(O_CB + g)),
                 reads=[("ps", bk), "smallp"], writes=["co"])
        for (t0, n) in _chunks(TH, 512):
            bk = bank()
            P.op("pe", lambda e, bk=bk, t0=t0, n=n: e.matmul(PS[bk][:, 0:n], lhsT=onesM, rhs=co[:, t0:t0 + n],
                                                            start=True, stop=True),
                 reads=["co", "cst"], writes=[("ps", bk)])
            P.op("dve", lambda e, bk=bk, t0=t0, n=n: e.tensor_tensor(out=xc[:, t0:t0 + n], in0=co[:, t0:t0 + n],
                                                                    in1=PS[bk][:, 0:n], op=ALU.subtract),
                 reads=[("ps", bk), "co"], writes=["xc"])
            P.op("act", lambda e, t0=t0, n=n: e.activation(out=sq[:, t0:t0 + n], in_=xc[:, t0:t0 + n], func=AF.Square),
                 reads=["xc"], writes=["sq"])
            bk2 = bank()
            P.op("pe", lambda e, bk2=bk2, t0=t0, n=n: e.matmul(PS[bk2][:, 0:n], lhsT=onesM, rhs=sq[:, t0:t0 + n],
                                                              start=True, stop=True),
                 reads=["sq", "cst"], writes=[("ps", bk2)])
            P.op("dve", lambda e, bk2=bk2, t0=t0, n=n: e.tensor_scalar(out=sq[:, t0:t0 + n], in0=PS[bk2][:, 0:n],
                                                                      scalar1=EPS, scalar2=None, op0=ALU.add),
                 reads=[("ps", bk2)], writes=["sq"])
            P.op("act", lambda e, t0=t0, n=n: e.activation(out=sq[:, t0:t0 + n], in_=sq[:, t0:t0 + n], func=AF.Ln),
                 reads=["sq"], writes=["sq"])
            P.op("act", lambda e, t0=t0, n=n: e.activation(out=sq[:, t0:t0 + n], in_=sq[:, t0:t0 + n], func=AF.Exp,
                                                           scale=-0.5), reads=["sq"], writes=["sq"])
            P.op("pool", lambda e, t0=t0, n=n: e.tensor_tensor(out=xc[:, t0:t0 + n], in0=xc[:, t0:t0 + n],
                                                              in1=sq[:, t0:t0 + n], op=ALU.mult),
                 reads=["xc", "sq"], writes=["xc"])
        yb = g % 2
        P.op("act", lambda e, yb=yb, g=g: e.activation(out=ysp[yb][:, :], in_=xc[:, 0:TH], func=AF.Silu,
                                                       scale=spc(O_CG + g), bias=spc(O_CBETA + g)),
             reads=["xc", "smallp"], writes=[("ysp", yb)])
        P.op("sp", lambda e, yb=yb, g=g: e.dma_start(out=yT_d[NH + g], in_=ysp[yb][:, :]), reads=[("ysp", yb)],
             dma=True)
    P.barrier()

    if STOP == "B3":
        return finish()
    A.release(m_persist)
    yT = A.alloc([128, 2 * NH, TH], BF16, "yT")
    wo_t = [A.alloc([128, 2 * NH, 512], BF16, "wo") for _ in range(2)]
    xblk = [A.alloc([128, 512], F32, "xblk") for _ in range(3)]
    x1blk = [A.alloc([128, 512], F32, "x1blk") for _ in range(3)]
    jk5 = A.alloc([128, 512], F32, "jk5")
    ssq2 = A.alloc([128, NCH, DB], F32, "ssq2")
    rstd2 = A.alloc([128, NCH], F32, "rstd2")
    m_b4 = A.mark()
    for u_ in range(2 * NH):
        P.op("sp", lambda e, u_=u_: e.dma_start(out=yT[:, u_, :], in_=yT_d[u_]), writes=["yT"], dma=True)
    P.op("dve", lambda e: e.memset(ssq2[:], 0.0), writes=["ssq2"])

    def load_wo(db):
        s = db % 2
        src = wout_d[db]
        dst = wo_t[s][:, :, :].rearrange("p k c -> p (k c)")
        P.op("pool", lambda e: e.dma_start(out=dst, in_=src), writes=[("wo", s)], dma=True)

    load_wo(0)
    it = 0
    for db in range(DB):
        pre_issue((len(pre_list) - pre_pos[0] + DB - db - 1) // (DB - db))
        if db + 1 < DB:
            load_wo(db + 1)
        s = db % 2
        for ci, (r0, n) in enumerate(tch):
            b3 = it % 3
            it += 1
            P.op("sp", lambda e, b3=b3, r0=r0, n=n, db=db: e.dma_start(
                out=xblk[b3][0:n, :], in_=xn_d[HALO + r0:HALO + r0 + n, db * 512:(db + 1) * 512]),
                writes=[("xblk", b3)], dma=True)
            bk = bank()
            for cc in range(2 * NH):
                P.op("pe", lambda e, bk=bk, cc=cc, r0=r0, n=n, s=s: e.matmul(
                    PS[bk][0:n, :], lhsT=yT[:, cc, r0:r0 + n], rhs=wo_t[s][:, cc, :],
                    start=(cc == 0), stop=(cc == 2 * NH - 1)), reads=["yT", ("wo", s)], writes=[("ps", bk)])
            P.op("dve", lambda e, bk=bk, b3=b3, n=n: e.tensor_tensor(out=x1blk[b3][0:n, :], in0=xblk[b3][0:n, :],
                                                                    in1=PS[bk][0:n, :], op=ALU.add),
                 reads=[("ps", bk), ("xblk", b3)], writes=[("x1blk", b3)])
            P.op("act", lambda e, b3=b3, n=n, ci=ci, db=db: e.activation(
                out=jk5[0:n, :], in_=x1blk[b3][0:n, :], func=AF.Square, accum_out=ssq2[0:n, ci, db:db + 1]),
                reads=[("x1blk", b3)], writes=["jk5", "ssq2"])
            P.op("sp", lambda e, b3=b3, r0=r0, n=n, db=db: e.dma_start(
                out=x1_d[r0:r0 + n, db * 512:(db + 1) * 512], in_=x1blk[b3][0:n, :]),
                reads=[("x1blk", b3)], writes=["x1_d"], dma=True)
    P.op("dve", lambda e: e.tensor_reduce(out=rstd2[:, :], in_=ssq2[:, :, :], axis=AX.X, op=ALU.add),
         reads=["ssq2"], writes=["rstd2"])
    P.op("dve", lambda e: e.tensor_scalar(out=rstd2[:, :], in0=rstd2[:, :], scalar1=1.0 / D, scalar2=EPS,
                                          op0=ALU.mult, op1=ALU.add), reads=["rstd2"], writes=["rstd2"])
    P.op("act", lambda e: e.activation(out=rstd2[:, :], in_=rstd2[:, :], func=AF.Ln), reads=["rstd2"], writes=["rstd2"])
    P.op("act", lambda e: e.activation(out=rstd2[:, :], in_=rstd2[:, :], func=AF.Exp, scale=-0.5), reads=["rstd2"],
         writes=["rstd2"])
    P.barrier()

    if STOP == "B4":
        return finish()
    A.release(m_persist)
    rstd2b = A.alloc([128, NCH], F32, "rstd2b")
    P.op("dve", lambda e: e.tensor_copy(out=rstd2b[:, :], in_=rstd2[:, :]), reads=["rstd2"], writes=["rstd2b"])
    P.barrier()
    Gt = A.alloc([128, NCH, E], F32, "G")
    GHL = A.alloc([128, NCH, E, 2], BF16, "GHL")
    maskf = A.alloc([128, NCH, E], F32, "maskf")
    maskb = A.alloc([128, NCH, E], BF16, "maskb")
    pos = A.alloc([128, NCH, E], F32, "pos")
    idxi = A.alloc([128, NCH, K], I32, "idxi")
    m_tables = A.mark()
    h2 = A.alloc([128, NCH, D], BF16, "h2")
    m_moe = A.mark()
    x1c = [A.alloc([128, D], F32, "x1c") for _ in range(2)]
    hT2 = A.alloc([128, DC, 128], F32, "hT2")
    wr_t = A.alloc([128, DC, E], F32, "wr")
    diag2 = A.alloc([128, 128], F32, "diag2")
    lg = A.alloc([128, E], F32, "lg")
    wk = A.alloc([128, E], F32, "wk")
    eq = A.alloc([128, E], F32, "eq")
    mx = A.alloc([128, 8], F32, "mx")
    rk = A.alloc([128, E], F32, "rk")
    sl = A.alloc([128, E], F32, "sl")
    idxf = A.alloc([128, K], F32, "idxf")
    P.op("sp", lambda e: e.dma_start(out=wr_t[:], in_=wr_d.rearrange("(k p) c -> p k c", p=128)), writes=["wr"],
         dma=True)
    for ci, (r0, n) in enumerate(tch):
        b = ci % 2
        P.op("sp", lambda e, b=b, r0=r0, n=n: e.dma_start(out=x1c[b][0:n, :], in_=x1_d[r0:r0 + n, :]),
             reads=["x1_d"], writes=[("x1c", b)], dma=True)
        if LVL < -2:
            continue
        P.op("act", lambda e, b=b, n=n, ci=ci: e.activation(out=h2[0:n, ci, :], in_=x1c[b][0:n, :], func=AF.Identity,
                                                          scale=rstd2b[0:n, ci:ci + 1]),
             reads=[("x1c", b), "rstd2b"], writes=["h2"])
        if LVL < -1:
            continue
        P.op("dve", lambda e, n=n, ci=ci: e.tensor_scalar(out=diag2[0:n, 0:n], in0=ident[0:n, 0:n],
                                                        scalar1=rstd2b[0:n, ci:ci + 1], scalar2=None, op0=ALU.mult),
             reads=["rstd2b", "cst"], writes=["diag2"])
        for q in range(DC // 4):
            bk = bank()
            for j in range(4):
                dc = q * 4 + j
                P.op("pe", lambda e, b=b, n=n, dc=dc, bk=bk, j=j: e.matmul(
                    PS[bk][:, j * 128:j * 128 + n], lhsT=x1c[b][0:n, dc * 128:(dc + 1) * 128],
                    rhs=diag2[0:n, 0:n], start=True, stop=True),
                    reads=[("x1c", b), "diag2"], writes=[("ps", bk)])
            for j in range(4):
                dc = q * 4 + j
                eng = "act" if (j % 2 == 0 and cfg.get("EVAC", "dve") == "mix") else "dve"
                if eng == "act":
                    P.op("act", lambda e, bk=bk, j=j, dc=dc, n=n: e.activation(
                        out=hT2[:, dc, 0:n], in_=PS[bk][:, j * 128:j * 128 + n], func=AF.Identity, scale=spc(O_G2 + dc)),
                        reads=[("ps", bk), "smallp"], writes=[("hT2", dc)])
                else:
                    P.op("dve", lambda e, bk=bk, j=j, dc=dc, n=n: e.tensor_scalar(
                        out=hT2[:, dc, 0:n], in0=PS[bk][:, j * 128:j * 128 + n], scalar1=spc(O_G2 + dc), scalar2=None,
                        op0=ALU.mult), reads=[("ps", bk), "smallp"], writes=[("hT2", dc)])
        if LVL < 0:
            continue
        bk = bank()
        for dc in range(DC):
            P.op("pe", lambda e, bk=bk, dc=dc, n=n: e.matmul(PS[bk][0:n, 0:E], lhsT=hT2[:, dc, 0:n], rhs=wr_t[:, dc, :],
                                                            start=(dc == 0), stop=(dc == DC - 1)),
                 reads=[("hT2", dc), "wr"], writes=[("ps", bk)])
        P.op("dve", lambda e, bk=bk, n=n: e.tensor_tensor(out=lg[0:n, :], in0=PS[bk][0:n, 0:E], in1=brt[0:n, :],
                                                         op=ALU.add), reads=[("ps", bk), "cst"], writes=["lg"])
        if debug:
            P.op("sp", lambda e, r0=r0, n=n: e.dma_start(out=dbg_lg[r0:r0 + n, :], in_=lg[0:n, :]), reads=["lg"],
                 dma=True)
        if LVL < 1:
            continue
        P.op("dve", lambda e, n=n: e.tensor_copy(out=wk[0:n, :], in_=lg[0:n, :]), reads=["lg"], writes=["wk"])
        for kk in range(K):
            P.op("dve", lambda e, n=n, kk=kk: e.tensor_reduce(out=mx[0:n, kk:kk + 1], in_=wk[0:n, :], axis=AX.X,
                                                            op=ALU.max), reads=["wk"], writes=["mx"])
            if kk < K - 1:
                P.op("dve", lambda e, n=n, kk=kk: e.tensor_scalar(out=eq[0:n, :], in0=wk[0:n, :],
                                                                scalar1=mx[0:n, kk:kk + 1], scalar2=-1e30,
                                                                op0=ALU.is_equal, op1=ALU.mult),
                     reads=["wk", "mx"], writes=["eq"])
                P.op("dve", lambda e, n=n: e.tensor_tensor(out=wk[0:n, :], in0=wk[0:n, :], in1=eq[0:n, :], op=ALU.add),
                     reads=["wk", "eq"], writes=["wk"])
        P.op("dve", lambda e, n=n, ci=ci: e.tensor_scalar(out=maskf[0:n, ci, :], in0=lg[0:n, :],
                                                        scalar1=mx[0:n, K - 1:K], scalar2=None, op0=ALU.is_ge),
             reads=["lg", "mx"], writes=["maskf"])
        P.op("dve", lambda e, n=n, ci=ci: e.tensor_copy(out=maskb[0:n, ci, :], in_=maskf[0:n, ci, :]),
             reads=["maskf"], writes=["maskb"])
        P.op("dve", lambda e, n=n: e.tensor_scalar(out=mx[0:n, 4:5], in0=mx[0:n, 0:1], scalar1=-1.0, scalar2=None,
                                                 op0=ALU.mult), reads=["mx"], writes=["mx"])
        P.op("act", lambda e, n=n: e.activation(out=wk[0:n, :], in_=lg[0:n, :], func=AF.Exp, bias=mx[0:n, 4:5]),
             reads=["lg", "mx"], writes=["wk"])
        P.op("dve", lambda e, n=n, ci=ci: e.tensor_tensor(out=wk[0:n, :], in0=wk[0:n, :], in1=maskf[0:n, ci, :],
                                                        op=ALU.mult), reads=["wk", "maskf"], writes=["wk"])
        P.op("dve", lambda e, n=n: e.tensor_reduce(out=mx[0:n, 5:6], in_=wk[0:n, :], axis=AX.X, op=ALU.add),
             reads=["wk"], writes=["mx"])
        P.op("dve", lambda e, n=n: e.reciprocal(out=mx[0:n, 6:7], in_=mx[0:n, 5:6]), reads=["mx"], writes=["mx"])
        P.op("dve", lambda e, n=n, ci=ci: e.tensor_scalar(out=Gt[0:n, ci, :], in0=wk[0:n, :], scalar1=mx[0:n, 6:7],
                                                        scalar2=None, op0=ALU.mult),
             reads=["wk", "mx"], writes=["G"])
        if LVL < 2:
            continue
        P.op("dve", lambda e, n=n, ci=ci: e.tensor_copy(out=GHL[0:n, ci, :, 0], in_=Gt[0:n, ci, :]),
             reads=["G"], writes=["GHL"])
        P.op("dve", lambda e, n=n, ci=ci: e.tensor_tensor(out=eq[0:n, :], in0=Gt[0:n, ci, :], in1=GHL[0:n, ci, :, 0],
                                                        op=ALU.subtract), reads=["G", "GHL"], writes=["eq"])
        P.op("dve", lambda e, n=n, ci=ci: e.tensor_copy(out=GHL[0:n, ci, :, 1], in_=eq[0:n, :]),
             reads=["eq"], writes=["GHL"])
        if debug:
            P.op("sp", lambda e, r0=r0, n=n, ci=ci: e.dma_start(out=dbg_G[r0:r0 + n, :], in_=Gt[0:n, ci, :]),
                 reads=["G"], dma=True)
        if LVL < 3:
            continue
        bk = bank()
        for kc in range(ci + 1):
            m = tch[kc][1]
            lhs = onesb[0:m, 0:n] if kc < ci else Ub[0:m, 0:n]
            P.op("pe", lambda e, bk=bk, kc=kc, m=m, n=n, lhs=lhs, ci=ci: e.matmul(
                PS[bk][0:n, 0:E], lhsT=lhs, rhs=maskb[0:m, kc, :], start=(kc == 0), stop=(kc == ci)),
                reads=["maskb", "cbf"], writes=[("ps", bk)])
        P.op("act", lambda e, bk=bk, n=n, ci=ci: e.activation(out=pos[0:n, ci, :], in_=PS[bk][0:n, 0:E], func=AF.Copy),
             reads=[("ps", bk)], writes=["pos"])
        if LVL < 4:
            continue
        P.op("dve", lambda e, n=n, ci=ci: e.tensor_tensor(out=sl[0:n, :], in0=pos[0:n, ci, :], in1=eoff[0:n, :],
                                                        op=ALU.add), reads=["pos", "cst"], writes=["sl"])
        P.op("dve", lambda e, n=n, ci=ci: e.tensor_tensor_scan(out=rk[0:n, :], data0=ones1[0:n, 0:E],
                                                             data1=maskf[0:n, ci, :], initial=0.0, op0=ALU.mult,
                                                             op1=ALU.add), reads=["maskf", "cst"], writes=["rk"])
        for kk in range(K):
            P.op("dve", lambda e, n=n, ci=ci, kk=kk: e.scalar_tensor_tensor(
                out=eq[0:n, :], in0=rk[0:n, :], scalar=float(kk + 1), in1=maskf[0:n, ci, :], op0=ALU.is_equal,
                op1=ALU.mult), reads=["rk", "maskf"], writes=["eq"])
            P.op("dve", lambda e, n=n: e.tensor_tensor(out=eq[0:n, :], in0=eq[0:n, :], in1=sl[0:n, :], op=ALU.mult),
                 reads=["eq", "sl"], writes=["eq"])
            P.op("dve", lambda e, n=n, kk=kk: e.tensor_reduce(out=idxf[0:n, kk:kk + 1], in_=eq[0:n, :], axis=AX.X,
                                                            op=ALU.add), reads=["eq"], writes=["idxf"])
        P.op("dve", lambda e, n=n, ci=ci: e.tensor_copy(out=idxi[0:n, ci, :], in_=idxf[0:n, :]),
             reads=["idxf"], writes=["idxi"])
        if debug:
            P.op("sp", lambda e, r0=r0, n=n, ci=ci: e.dma_start(out=dbg_idx[r0:r0 + n, :], in_=idxi[0:n, ci, :]),
                 reads=["idxi"], dma=True)
    P.barrier()

    if STOP == "B5":
        return finish()
    A.release(m_moe)
    RING = cfg.get("RING", 5)
    wt = [A.alloc([128, 16 * 512], BF16, "wt") for _ in range(RING)]
    xT = A.alloc([128, DC, CAP], BF16, "xT")
    actT = A.alloc([128, FC, CAP], BF16, "actT")
    sel_one = A.alloc([128, NCH, CAP], BF16, "sel")
    sel = [sel_one, sel_one]
    gsl = [A.alloc([128, CC], F32, "gsl") for _ in range(2)]
    NT = 2
    hgt = [A.alloc([128, CAP], F32, "hg") for _ in range(NT)]
    sgt2 = [A.alloc([128, CAP], F32, "sg") for _ in range(NT)]
    hut = [A.alloc([128, CAP], F32, "hu") for _ in range(NT)]
    yst = [A.alloc([128, 512], F32, "yst") for _ in range(2)]
    if cfg.get("VERBOSE"):
        print("MoE phase SBUF end", A.cur, "of", A.hi)

    tiles = []
    for e_ in range(E):
        for fb in range(FB):
            tiles.append(("g", e_, fb))
            tiles.append(("u", e_, fb))
        for db in range(DB):
            tiles.append(("d", e_, db))
    tslot = {}
    tnext = [0]

    def tile_view(s, kind):
        if kind == "d":
            return wt[s][:, 0:FC * 512].rearrange("p (k c) -> p k c", k=FC)
        return wt[s][:, 0:DC * 256].rearrange("p (k c) -> p k c", k=DC)

    def ensure_tile(i):
        while tnext[0] < len(tiles) and tnext[0] <= i + RING - 2:
            j = tnext[0]
            kind, e_, blk = tiles[j]
            s = j % RING
            pre = e_ < NPRE
            if kind == "d":
                src = (wdb_d if pre else wd_d)[e_ * DB + blk]
                dst = wt[s][:, 0:FC * 512]
                pk = ("pre", "d", e_ * DB + blk)
            elif kind == "g":
                src = (wgb_d if pre else wg_d)[e_ * FB + blk]
                dst = wt[s][:, 0:DC * 256]
                pk = ("pre", "g", e_ * FB + blk)
            else:
                src = (wub_d if pre else wu_d)[e_ * FB + blk]
                dst = wt[s][:, 0:DC * 256]
                pk = ("pre", "u", e_ * FB + blk)
            P.op("pool", lambda e, dst=dst, src=src: e.dma_start(out=dst, in_=src), reads=([pk] if pre else []),
                 writes=[("wt", s)], dma=True)
            tslot[j] = s
            tnext[0] += 1
        return tslot[i]

    def build_sel(e_):
        sb = e_ % 2
        for ci, (r0, n) in enumerate(tch):
            P.op("dve", lambda e, sb=sb, ci=ci, n=n, e_=e_: e.tensor_scalar(
                out=sel[sb][0:n, ci, :], in0=iotaC[0:n, :], scalar1=pos[0:n, ci, e_:e_ + 1],
                scalar2=maskf[0:n, ci, e_:e_ + 1], op0=ALU.is_equal, op1=ALU.mult),
                reads=["pos", "maskf", "cst"], writes=["sel"])
        bk = bank()
        for cc in range(CC):
            for ci, (r0, n) in enumerate(tch):
                P.op("pe", lambda e, sb=sb, ci=ci, n=n, cc=cc, bk=bk, e_=e_: e.matmul(
                    PS[bk][:, cc * 2:cc * 2 + 2], lhsT=sel[sb][0:n, ci, cc * 128:(cc + 1) * 128],
                    rhs=GHL[0:n, ci, e_, :], start=(ci == 0 and cc == 0), stop=(ci == NCH - 1),
                    skip_group_check=True),
                    reads=["sel", "GHL"], writes=[("ps", bk)])
        for cc in range(CC):
            P.op("dve", lambda e, sb=sb, cc=cc, bk=bk: e.tensor_reduce(
                out=gsl[sb][:, cc:cc + 1], in_=PS[bk][:, cc * 2:cc * 2 + 2], axis=AX.X, op=ALU.add),
                reads=[("ps", bk)], writes=[("gsl", sb)])

    def gather(e_, dcs=None):
        sb = e_ % 2
        for dc in (range(DC) if dcs is None else dcs):
            bk = bank()
            for ci, (r0, n) in enumerate(tch):
                P.op("pe", lambda e, sb=sb, ci=ci, n=n, dc=dc, bk=bk: e.matmul(
                    PS[bk][:, 0:CAP], lhsT=h2[0:n, ci, dc * 128:(dc + 1) * 128], rhs=sel[sb][0:n, ci, :],
                    start=(ci == 0), stop=(ci == NCH - 1)), reads=["h2", "sel"], writes=[("ps", bk)])
            if False:
                pass
            else:
                P.op("dve", lambda e, dc=dc, bk=bk: e.tensor_scalar(out=xT[:, dc, :], in0=PS[bk][:, 0:CAP],
                                                                   scalar1=spc(O_G2 + dc), scalar2=None, op0=ALU.mult),
                     reads=[("ps", bk), "smallp"], writes=[("xT", dc)])

    ti = 0
    build_sel(0)
    gather(0)
    tcount = 0
    ycount = 0
    for e_ in range(E):
        for fb in range(FB):
            sg_ = ensure_tile(ti); ti += 1
            su_ = ensure_tile(ti); ti += 1
            wgv = tile_view(sg_, "g")
            wuv = tile_view(su_, "u")
            for fcl in range(2):
                fc = fb * 2 + fcl
                bg_ = bank()
                for dc in range(DC):
                    P.op("pe", lambda e, bg_=bg_, dc=dc, fcl=fcl, wgv=wgv: e.matmul(
                        PS[bg_][:, 0:CAP], lhsT=wgv[:, dc, fcl * 128:(fcl + 1) * 128], rhs=xT[:, dc, :],
                        start=(dc == 0), stop=(dc == DC - 1)), reads=[("wt", sg_), ("xT", dc)], writes=[("ps", bg_)])
                bu_ = bank()
                for dc in range(DC):
                    P.op("pe", lambda e, bu_=bu_, dc=dc, fcl=fcl, wuv=wuv: e.matmul(
                        PS[bu_][:, 0:CAP], lhsT=wuv[:, dc, fcl * 128:(fcl + 1) * 128], rhs=xT[:, dc, :],
                        start=(dc == 0), stop=(dc == DC - 1)), reads=[("wt", su_), ("xT", dc)], writes=[("ps", bu_)])
                tb = tcount % NT
                tcount += 1
                bgc = spc(O_BG + e_ * FC + fc)
                buc = spc(O_BU + e_ * FC + fc)
                P.op("dve", lambda e, tb=tb, bg_=bg_, bgc=bgc: e.tensor_scalar(
                    out=hgt[tb][:, :], in0=PS[bg_][:, 0:CAP], scalar1=bgc, scalar2=7.0, op0=ALU.add, op1=ALU.min),
                    reads=[("ps", bg_), "smallp"], writes=[("hg", tb)])
                P.op("act", lambda e, tb=tb: e.activation(out=sgt2[tb][:, :], in_=hgt[tb][:, :], func=AF.Sigmoid,
                                                         scale=1.702), reads=[("hg", tb)], writes=[("sg", tb)])
                P.op("dve", lambda e, tb=tb, bu_=bu_, buc=buc: e.tensor_scalar(
                    out=hut[tb][:, :], in0=PS[bu_][:, 0:CAP], scalar1=buc, scalar2=7.0, op0=ALU.add, op1=ALU.min),
                    reads=[("ps", bu_), "smallp"], writes=[("hu", tb)])
                P.op("dve", lambda e, tb=tb: e.tensor_scalar(out=hut[tb][:, :], in0=hut[tb][:, :], scalar1=-7.0,
                                                            scalar2=1.0, op0=ALU.max, op1=ALU.add),
                     reads=[("hu", tb)], writes=[("hu", tb)])
                P.op("dve", lambda e, tb=tb: e.tensor_tensor(out=hgt[tb][:, :], in0=hgt[tb][:, :], in1=sgt2[tb][:, :],
                                                            op=ALU.mult), reads=[("hg", tb), ("sg", tb)],
                     writes=[("hg", tb)])
                P.op("dve", lambda e, tb=tb, fc=fc: e.tensor_tensor(out=actT[:, fc, :], in0=hgt[tb][:, :],
                                                                   in1=hut[tb][:, :], op=ALU.mult),
                     reads=[("hg", tb), ("hu", tb)], writes=["actT"])
        if e_ + 1 < E:
            build_sel(e_ + 1)
        sb = e_ % 2
        for db in range(DB):
            if e_ + 1 < E:
                gather(e_ + 1, range(db * DC // DB, (db + 1) * DC // DB))
            sd_ = ensure_tile(ti); ti += 1
            wdv = tile_view(sd_, "d")
            for cc in range(CC):
                yb = ycount % 2
                ycount += 1
                bk = bank()
                for fc in range(FC):
                    P.op("pe", lambda e, bk=bk, fc=fc, cc=cc, wdv=wdv: e.matmul(
                        PS[bk][:, :], lhsT=actT[:, fc, cc * 128:(cc + 1) * 128], rhs=wdv[:, fc, :],
                        start=(fc == 0), stop=(fc == FC - 1)), reads=["actT", ("wt", sd_)], writes=[("ps", bk)])
                P.op("dve", lambda e, bk=bk, cc=cc, yb=yb, sb=sb: e.tensor_scalar(
                    out=yst[yb][:, :], in0=PS[bk][:, :], scalar1=gsl[sb][:, cc:cc + 1], scalar2=None, op0=ALU.mult),
                    reads=[("ps", bk), ("gsl", sb)], writes=[("yst", yb)])
                dst = Y_d[e_ * CAP + cc * 128:e_ * CAP + (cc + 1) * 128, db * 512:(db + 1) * 512]
                P.op("sp", lambda e, yb=yb, dst=dst: e.dma_start(out=dst, in_=yst[yb][:, :]), reads=[("yst", yb)],
                     writes=["Y_d"], dma=True)
    P.barrier()

    if STOP == "B6":
        return finish()
    A.release(m_tables)
    ga2 = [[A.alloc([128, D], F32, "ga") for _ in range(K)] for _ in range(2)]
    x1f = A.alloc([128, D], F32, "x1f")
    gfb = A.alloc([128, D], F32, "gfb")
    bdn = A.alloc([128, D], F32, "bdn")
    GT = A.alloc([128, 128], F32, "GT")
    s7 = A.alloc([128, 4], F32, "s7")
    P.op("sp", lambda e: e.dma_start(out=gfb[:], in_=gfb_d), writes=["gfb"], dma=True)
    P.op("sp", lambda e: e.dma_start(out=bdn[0:E, :], in_=bd_d), writes=["bdn"], dma=True)
    for ci, (r0, n) in enumerate(tch):
        ga = ga2[ci % 2]
        gp = ci % 2
        P.op("sp", lambda e, r0=r0, n=n: e.dma_start(out=x1f[0:n, :], in_=x1_d[r0:r0 + n, :]), reads=["x1_d"],
             writes=["x1f"], dma=True)
        for kk in range(K):
            P.op("pool", lambda e, kk=kk, n=n, ci=ci, ga=ga: e.indirect_dma_start(
                out=ga[kk][0:n, :], out_offset=None, in_=Y_d,
                in_offset=bass.IndirectOffsetOnAxis(ap=idxi[0:n, ci, kk:kk + 1], axis=0)),
                reads=["Y_d", "idxi"], writes=[("ga", gp, kk)], dma=True)
        bk = bank()
        P.op("pe", lambda e, bk=bk, n=n, ci=ci: e.matmul(PS[bk][0:E, 0:n], lhsT=Gt[0:n, ci, :], rhs=ident[0:n, 0:n],
                                                        start=True, stop=True), reads=["G", "cst"], writes=[("ps", bk)])
        P.op("act", lambda e, bk=bk, n=n: e.activation(out=GT[0:E, 0:n], in_=PS[bk][0:E, 0:n], func=AF.Copy),
             reads=[("ps", bk)], writes=["GT"])
        for db in range(DB):
            bk = bank()
            P.op("pe", lambda e, bk=bk, n=n, db=db: e.matmul(PS[bk][0:n, :], lhsT=GT[0:E, 0:n],
                                                            rhs=bdn[0:E, db * 512:(db + 1) * 512], start=True,
                                                            stop=True), reads=["GT", "bdn"], writes=[("ps", bk)])
            P.op("dve", lambda e, bk=bk, n=n, db=db: e.tensor_tensor(
                out=x1f[0:n, db * 512:(db + 1) * 512], in0=x1f[0:n, db * 512:(db + 1) * 512], in1=PS[bk][0:n, :],
                op=ALU.add), reads=[("ps", bk), "x1f"], writes=["x1f"])
        P.op("pool", lambda e, n=n, ga=ga: e.tensor_tensor(out=ga[0][0:n, :], in0=ga[0][0:n, :], in1=ga[1][0:n, :],
                                                   op=ALU.add), reads=[("ga", gp, 0), ("ga", gp, 1)], writes=[("ga", gp, 0)])
        P.op("dve", lambda e, n=n, ga=ga: e.tensor_tensor(out=ga[2][0:n, :], in0=ga[2][0:n, :], in1=ga[3][0:n, :],
                                                  op=ALU.add), reads=[("ga", gp, 2), ("ga", gp, 3)], writes=[("ga", gp, 2)])
        P.op("pool", lambda e, n=n, ga=ga: e.tensor_tensor(out=ga[0][0:n, :], in0=ga[0][0:n, :], in1=ga[2][0:n, :],
                                                   op=ALU.add), reads=[("ga", gp, 0), ("ga", gp, 2)], writes=[("ga", gp, 0)])
        P.op("dve", lambda e, n=n, ga=ga: e.tensor_tensor(out=x1f[0:n, :], in0=x1f[0:n, :], in1=ga[0][0:n, :], op=ALU.add),
             reads=[("ga", gp, 0), "x1f"], writes=["x1f"])
        P.op("dve", lambda e: e.memset(s7[:, 0:1], 0.0), writes=["s7"])
        jk7 = ga[3]
        P.op("act", lambda e, n=n, jk7=jk7: e.activation(out=jk7[0:n, :], in_=x1f[0:n, :], func=AF.Square,
                                                        accum_out=s7[0:n, 0:1]),
             reads=["x1f", ("ga", gp, 2)], writes=[("ga", gp, 3), "s7"])
        P.op("dve", lambda e, n=n: e.tensor_scalar(out=s7[0:n, 1:2], in0=s7[0:n, 0:1], scalar1=1.0 / D, scalar2=EPS,
                                                 op0=ALU.mult, op1=ALU.add), reads=["s7"], writes=["s7b"])
        P.op("act", lambda e, n=n: e.activation(out=s7[0:n, 3:4], in_=s7[0:n, 1:2], func=AF.Ln), reads=["s7b"],
             writes=["s7d"])
        P.op("act", lambda e, n=n: e.activation(out=s7[0:n, 2:3], in_=s7[0:n, 3:4], func=AF.Exp, scale=-0.5),
             reads=["s7d"], writes=["s7c"])
        P.op("dve", lambda e, n=n, jk7=jk7: e.scalar_tensor_tensor(out=jk7[0:n, :], in0=x1f[0:n, :],
                                                                 scalar=s7[0:n, 2:3], in1=gfb[0:n, :], op0=ALU.mult,
                                                                 op1=ALU.mult),
             reads=["x1f", "s7c", "gfb", ("ga", gp, 3)], writes=[("ga", gp, 3)])
        P.op("sp", lambda e, r0=r0, n=n, jk7=jk7: e.dma_start(out=out_d[r0:r0 + n, :], in_=jk7[0:n, :]),
             reads=[("ga", gp, 3)], dma=True)
    P.barrier()
    P.emit(nc)
    return nc


def _pcol(v):
    v = np.asarray(v, np.float32)
    return np.ascontiguousarray(v.reshape(-1, 128).T)


def make_in_maps(cfg, inp):
    D = cfg["D"]; SEQ = cfg["SEQ"]; B = cfg["B"]; E = cfg["E"]; CAP = cfg["CAP"]; HALO = cfg["HALO"]
    ST = SEQ + cfg["NMETA"]; TH = ST // 2
    LW = D // 2; CW = D // 2; FF = D // 2
    NH = LW // 128; NG = CW // 128; DC = D // 128; FC = FF // 128
    f32 = np.float32
    x = np.asarray(inp["x"], f32)
    meta = np.asarray(inp["meta_tokens"], f32)
    g1 = np.asarray(inp["norm1_g"], f32)[0]
    g2 = np.asarray(inp["norm2_g"], f32)[0]
    gfin = np.asarray(inp["final_norm_g"], f32)
    lcw = np.asarray(inp["lru_conv_w"], f32)[0]
    lcb = np.asarray(inp["lru_conv_b"], f32)[0]
    lwa = np.asarray(inp["lru_w_a"], f32)[0]
    lba = np.asarray(inp["lru_b_a"], f32)[0]
    lwi = np.asarray(inp["lru_w_i"], f32)[0]
    lbi = np.asarray(inp["lru_b_i"], f32)[0]
    lam = np.asarray(inp["lru_lambda"], f32)[0]
    ccw = np.asarray(inp["conf_conv_w"], f32)[0]
    ccb = np.asarray(inp["conf_conv_b"], f32)[0]
    cng = np.asarray(inp["conf_norm_g"], f32)[0]
    cnb = np.asarray(inp["conf_norm_b"], f32)[0]
    bg = np.asarray(inp["b_gate"], f32)[0]
    bu = np.asarray(inp["b_up"], f32)[0]
    brt = np.asarray(inp["b_router"], f32)[0]

    ident = np.eye(128, dtype=f32)
    U = np.triu(np.ones((128, 128), f32), 1)
    onesM = np.full((128, 128), 1.0 / 128, f32)
    ones1 = np.ones((128, 128), f32)
    iotaC = np.tile(np.arange(CAP, dtype=f32)[None, :], (128, 1))
    eoff = np.tile((np.arange(E, dtype=f32) * CAP)[None, :], (128, 1))
    brtb = np.tile(brt[None, :], (128, 1))
    consts = np.ascontiguousarray(np.concatenate([ident, U, onesM, ones1, iotaC, eoff, brtb], axis=1))

    shared = dict(
        consts=consts,
        g1b=np.ascontiguousarray(np.tile(g1[None, :], (128, 1))),
        gfb=np.ascontiguousarray(np.tile(gfin[None, :], (128, 1))),
        w_in=np.ascontiguousarray(np.asarray(inp["w_in"], f32)[0].reshape(DC, 128, 4 * LW // 128, 128)
                                  .transpose(2, 1, 0, 3)).reshape(4 * LW // 128, 128, DC * 128),
        w_out=np.ascontiguousarray(np.asarray(inp["w_out"], f32)[0].reshape(D // 128, 128, D // 512, 512)
                                   .transpose(2, 1, 0, 3)).reshape(D // 512, 128, (D // 128) * 512),
        w_router=np.asarray(inp["w_router"], f32)[0],
        w_gate=np.ascontiguousarray(np.asarray(inp["w_gate"], f32)[0].reshape(E, DC, 128, FF // 256, 256)
                                    .transpose(0, 3, 2, 1, 4)).reshape(E * (FF // 256), 128, DC * 256),
        w_up=np.ascontiguousarray(np.asarray(inp["w_up"], f32)[0].reshape(E, DC, 128, FF // 256, 256)
                                  .transpose(0, 3, 2, 1, 4)).reshape(E * (FF // 256), 128, DC * 256),
        w_down=np.ascontiguousarray(np.asarray(inp["w_down"], f32)[0].reshape(E, FC, 128, D // 512, 512)
                                    .transpose(0, 3, 2, 1, 4)).reshape(E * (D // 512), 128, FC * 512),
        b_down=np.asarray(inp["b_down"], f32)[0],
    )

    def smallp(dirs, rev_taps):
        cols = [_pcol(g1), _pcol(g2)]
        for dr in dirs:
            cw = lcw[dr].reshape(4, NH, 128)
            cols.append(np.ascontiguousarray(cw.transpose(2, 1, 0)).reshape(128, NH * 4))
            cols.append(_pcol(lcb[dr]))
            cols.append(_pcol(lba[dr].reshape(-1)))
            cols.append(_pcol(lbi[dr].reshape(-1)))
            cols.append(_pcol(lam[dr]))
        w = ccw[::-1] if rev_taps else ccw
        cw = w.reshape(31, NG, 128)
        cols.append(np.ascontiguousarray(cw.transpose(2, 1, 0)).reshape(128, NG * 31))
        cols += [_pcol(ccb), _pcol(cng), _pcol(cnb)]
        cols.append(np.ascontiguousarray(bg.reshape(E, FC, 128).transpose(2, 0, 1)).reshape(128, E * FC))
        cols.append(np.ascontiguousarray(bu.reshape(E, FC, 128).transpose(2, 0, 1)).reshape(128, E * FC))
        return np.ascontiguousarray(np.concatenate(cols, axis=1))

    maps = []
    for b in range(B):
        S = np.concatenate([meta, x[b]], axis=0)
        for half in range(2):
            Sl = S if half == 1 else S[::-1]
            dirs = (0, 1) if half == 1 else (1, 0)
            m = dict(shared)
            m["xo"] = np.ascontiguousarray(Sl[:TH])
            m["xn"] = np.ascontiguousarray(Sl[TH - HALO:])
            m["smallp"] = smallp(dirs, rev_taps=(half == 0))
            m["wa_f"] = np.ascontiguousarray(lwa[dirs[0]].reshape(NH * 128, 128))
            m["wa_b"] = np.ascontiguousarray(lwa[dirs[1]].reshape(NH * 128, 128))
            m["wi_f"] = np.ascontiguousarray(lwi[dirs[0]].reshape(NH * 128, 128))
            m["wi_b"] = np.ascontiguousarray(lwi[dirs[1]].reshape(NH * 128, 128))
            maps.append(m)
    return maps


def assemble(cfg, results):
    D = cfg["D"]; SEQ = cfg["SEQ"]; B = cfg["B"]; NM = cfg["NMETA"]
    ST = SEQ + NM; TH = ST // 2
    out = np.empty((B, SEQ, D), np.float32)
    i = 0
    for b in range(B):
        full = np.empty((ST, D), np.float32)
        for half in range(2):
            o = np.asarray(results[i]["out"], np.float32)
            i += 1
            if half == 1:
                full[TH:] = o
            else:
                full[:TH] = o[::-1]
        out[b] = full[NM:]
    return out


_NC_CACHE = {}


def run(cfg, inp, debug=False):
    key = (tuple(sorted(cfg.items())), debug)
    if key not in _NC_CACHE:
        _NC_CACHE[key] = build_program(cfg, debug=debug)
    nc = _NC_CACHE[key]
    maps = make_in_maps(cfg, inp)
    n = len(maps)
    res = run_bass_kernel_spmd(nc, maps, core_ids=list(range(n)))
    return res


def kernel(**inputs):
    cfg = dict(REAL_CFG)
    res = run(cfg, inputs)
    return assemble(cfg, res.results)
```

```python
import numpy as np
import concourse.bass as bass
import concourse.mybir as mybir
from concourse.bass_utils import run_bass_kernel_spmd

F32 = mybir.dt.float32
BF16 = mybir.dt.bfloat16
I32 = mybir.dt.int32
AF = mybir.ActivationFunctionType
ALU = mybir.AluOpType
AX = mybir.AxisListType

REAL_CFG = dict(D=4096, SEQ=2048, B=4, E=32, K=4, CAP=256, HALO=16, NMETA=16)


class _Op:
    __slots__ = ("eng", "fn", "deps", "signal", "sigval", "dma", "dsem", "dtarget", "idx")

    def __init__(self, eng, fn, dma):
        self.eng = eng
        self.fn = fn
        self.deps = []
        self.signal = False
        self.sigval = 0
        self.dma = dma
        self.dsem = None
        self.dtarget = 0


class Prog:
    ENGS = ("pe", "act", "dve", "pool", "sp")
    NDSEM = 8

    def __init__(self):
        self.ops = {e: [] for e in self.ENGS}
        self.last_write = {}
        self.readers = {}
        self.dma_rr = {e: 0 for e in self.ENGS}
        self.dma_last = {}
        self.dma_count = {}
        self.n = 0

    def op(self, eng, fn, reads=(), writes=(), dma=False):
        o = _Op(eng, fn, dma)
        o.idx = self.n
        self.n += 1
        deps = {}
        for k in reads:
            lw = self.last_write.get(k)
            if lw is not None:
                deps[id(lw)] = lw
        for k in writes:
            lw = self.last_write.get(k)
            if lw is not None:
                deps[id(lw)] = lw
            for r in self.readers.get(k, {}).values():
                deps[id(r)] = r
        rkey = eng
        if dma:
            i = self.dma_rr[eng]
            self.dma_rr[eng] = (i + 1) % self.NDSEM
            o.dsem = (eng, i)
            prev = self.dma_last.get(o.dsem)
            if prev is not None:
                deps[id(prev)] = prev
            self.dma_last[o.dsem] = o
            c = self.dma_count.get(o.dsem, 0) + 1
            self.dma_count[o.dsem] = c
            o.dtarget = 16 * c
            rkey = o.dsem
        for d in deps.values():
            if d is o:
                continue
            if (not d.dma) and d.eng == "pe" and eng == "pe" and not dma:
                continue
            o.deps.append(d)
        for k in reads:
            self.readers.setdefault(k, {})[rkey] = o
        for k in writes:
            self.last_write[k] = o
            self.readers[k] = {}
        self.ops[eng].append(o)
        return o

    def barrier(self):
        lasts = []
        for e in self.ENGS:
            for o in reversed(self.ops[e]):
                if not o.dma and o.fn is not None:
                    lasts.append(o)
                    break
        lasts += list(self.dma_last.values())
        for e in self.ENGS:
            o = _Op(e, None, False)
            o.idx = self.n
            self.n += 1
            o.deps = [d for d in lasts if not (d.eng == e and not d.dma and e == "pe")]
            self.ops[e].append(o)

    def emit(self, nc):
        for e in self.ENGS:
            for o in self.ops[e]:
                for d in o.deps:
                    d.signal = True
        for e in self.ENGS:
            c = 0
            for o in self.ops[e]:
                if o.signal and not o.dma:
                    c += 1
                    o.sigval = c
        from contextlib import ExitStack
        with ExitStack() as st:
            esem = {e: st.enter_context(nc.semaphore("es_" + e)) for e in self.ENGS}
            dsem = {}
            for e in self.ENGS:
                if self.dma_count and any(k[0] == e for k in self.dma_count):
                    for i in range(self.NDSEM):
                        dsem[(e, i)] = st.enter_context(nc.semaphore("ds_%s%d" % (e, i)))
            block = st.enter_context(nc.Block())

            def run(eng_name, e):
                waited = {}
                for o in self.ops[eng_name]:
                    for d in o.deps:
                        if d.dma:
                            sem, val, key = dsem[d.dsem], d.dtarget, d.dsem
                        else:
                            sem, val, key = esem[d.eng], d.sigval, d.eng
                        if waited.get(key, 0) < val:
                            e.wait_ge(sem, val)
                            waited[key] = val
                    if o.fn is None:
                        continue
                    inst = o.fn(e)
                    if o.dma:
                        inst.then_inc(dsem[o.dsem], 16)
                    elif o.signal:
                        inst.then_inc(esem[eng_name], 1)

            @block.tensor
            def _(e):
                run("pe", e)

            @block.scalar
            def _(e):
                run("act", e)

            @block.vector
            def _(e):
                run("dve", e)

            @block.gpsimd
            def _(e):
                run("pool", e)

            @block.sync
            def _(e):
                run("sp", e)


class Arena:
    def __init__(self, nc, lo=16512, hi=229344):
        self.nc = nc
        self.lo = lo
        self.hi = hi
        self.cur = lo
        self.cnt = 0

    def alloc(self, shape, dtype, name="t"):
        sz = 4 if dtype in (F32, I32) else 2
        n = 1
        for s in shape[1:]:
            n *= s
        nbytes = (n * sz + 63) // 64 * 64
        off = self.cur
        assert off + nbytes <= self.hi, "SBUF arena overflow: %s %s need %d at %d" % (name, shape, nbytes, off)
        self.cur += nbytes
        self.cnt += 1
        return self.nc.alloc_sbuf_tensor_at("%s_%d" % (name, self.cnt), list(shape), dtype, offset=off)

    def mark(self):
        return self.cur

    def release(self, m):
        self.cur = m


def _chunks(total, size=128):
    if size == 512 and total > 512:
        nb = -(-total // 512)
        size = -(-total // nb)
        size += size % 2
    out = []
    s = 0
    while s < total:
        n = min(size, total - s)
        out.append((s, n))
        s += n
    return out


def build_program(cfg, debug=False):
    D = cfg["D"]; SEQ = cfg["SEQ"]; E = cfg["E"]; K = cfg["K"]; CAP = cfg["CAP"]; HALO = cfg["HALO"]
    ST = SEQ + cfg["NMETA"]
    TH = ST // 2
    LW = D // 2; CW = D // 2; FF = D // 2
    NH = LW // 128; NG = CW // 128; DC = D // 128; FC = FF // 128; CC = CAP // 128
    CK = 31
    TW = HALO + TH
    DB = D // 512
    FB = FF // 256
    tch = _chunks(TH)
    NCH = len(tch)
    EPS = 1e-5

    nc = bass.Bass("TRN2", target_bir_lowering=False)
    P = Prog()
    A = Arena(nc)

    STOP = cfg.get("STOP", "")
    LVL = cfg.get("LVL", 9)

    def finish():
        P.barrier()
        P.emit(nc)
        return nc

    def din(name, shape, dt=F32):
        return nc.dram_tensor(name, list(shape), dt, kind="ExternalInput").ap()

    xo_d = din("xo", [TH, D])
    xn_d = din("xn", [TW, D])
    NSP = (2 * DC + 2 * (NH * 4 + 4 * NH) + NG * CK + 3 * NG + 2 * E * FC)
    sp_d = din("smallp", [128, NSP])
    NCST = 128 * 4 + CAP + 2 * E
    cst_d = din("consts", [128, NCST])
    g1b_d = din("g1b", [128, D])
    gfb_d = din("gfb", [128, D])
    NU = 4 * LW // 128
    win_d = din("w_in", [NU, 128, DC * 128])
    wa_d = [din("wa_f", [NH * 128, 128]), din("wa_b", [NH * 128, 128])]
    wi_d = [din("wi_f", [NH * 128, 128]), din("wi_b", [NH * 128, 128])]
    wout_d = din("w_out", [D // 512, 128, 2 * NH * 512])
    wr_d = din("w_router", [D, E])
    wg_d = din("w_gate", [E * (FF // 256), 128, (D // 128) * 256])
    wu_d = din("w_up", [E * (FF // 256), 128, (D // 128) * 256])
    wd_d = din("w_down", [E * (D // 512), 128, (FF // 128) * 512])
    bd_d = din("b_down", [E, D])
    out_d = nc.dram_tensor("out", [TH, D], F32, kind="ExternalOutput").ap()
    skind = "ExternalOutput" if debug else "Internal"
    yT_d = nc.dram_tensor("yT_scr", [2 * NH, 128, TH], BF16, kind=skind).ap()
    x1_d = nc.dram_tensor("x1_scr", [TH, D], F32, kind=skind).ap()
    Y_d = nc.dram_tensor("Y_scr", [E * CAP, D], F32, kind=skind).ap()
    NPRE = cfg.get("NPRE", 5)
    wgb_d = nc.dram_tensor("wg_bf", [NPRE * (FF // 256), 128, (D // 128) * 256], BF16, kind="Internal").ap()
    wub_d = nc.dram_tensor("wu_bf", [NPRE * (FF // 256), 128, (D // 128) * 256], BF16, kind="Internal").ap()
    wdb_d = nc.dram_tensor("wd_bf", [NPRE * (D // 512), 128, (FF // 128) * 512], BF16, kind="Internal").ap()
    pre_list = []
    for e_ in range(NPRE):
        for fb in range(FF // 256):
            pre_list.append(("g", e_ * (FF // 256) + fb))
            pre_list.append(("u", e_ * (FF // 256) + fb))
        for db in range(D // 512):
            pre_list.append(("d", e_ * (D // 512) + db))
    pre_pos = [0]

    def pre_issue(n):
        for _ in range(n):
            if pre_pos[0] >= len(pre_list):
                return
            kind, idx = pre_list[pre_pos[0]]
            pre_pos[0] += 1
            src = {"g": wg_d, "u": wu_d, "d": wd_d}[kind][idx]
            dst = {"g": wgb_d, "u": wub_d, "d": wdb_d}[kind][idx]
            P.op("pool", lambda e, src=src, dst=dst: e.dma_start(out=dst, in_=src), writes=[("pre", kind, idx)],
                 dma=True)

    if debug:
        dbg_lg = nc.dram_tensor("dbg_logits", [TH, E], F32, kind="ExternalOutput").ap()
        dbg_G = nc.dram_tensor("dbg_G", [TH, E], F32, kind="ExternalOutput").ap()
        dbg_idx = nc.dram_tensor("dbg_idx", [TH, K], I32, kind="ExternalOutput").ap()
        dbg_st = nc.dram_tensor("dbg_state", [128, NH], F32, kind="ExternalOutput").ap()

    PS = [nc.alloc_psum_tensor("ps%d" % i, [128, 512], F32) for i in range(8)]
    ps_rr = [0]

    def bank():
        i = ps_rr[0]
        ps_rr[0] = (i + 1) % 8
        return i

    smallp = A.alloc([128, NSP], F32, "smallp")
    cst = A.alloc([128, NCST], F32, "consts")
    cbf = A.alloc([128, 3 * 128], BF16, "cbf")
    stA = A.alloc([128, NH], F32, "stateA")
    cA = A.alloc([128, 2, 2 * NH], F32, "cA")
    P.op("sp", lambda e: e.dma_start(out=smallp[:], in_=sp_d), writes=["smallp"], dma=True)
    P.op("sp", lambda e: e.dma_start(out=cst[:], in_=cst_d), writes=["cst"], dma=True)
    ident = cst[:, 0:128]
    Utri = cst[:, 128:256]
    onesM = cst[:, 256:384]
    ones1 = cst[:, 384:512]
    iotaC = cst[:, 512:512 + CAP]
    eoff = cst[:, 512 + CAP:512 + CAP + E]
    brt = cst[:, 512 + CAP + E:512 + CAP + 2 * E]
    P.op("dve", lambda e: e.tensor_copy(out=cbf[:, 0:128], in_=ident), reads=["cst"], writes=["cbf"])
    P.op("dve", lambda e: e.tensor_copy(out=cbf[:, 128:256], in_=Utri), reads=["cst"], writes=["cbf"])
    P.op("dve", lambda e: e.tensor_copy(out=cbf[:, 256:384], in_=ones1), reads=["cst"], writes=["cbf"])
    identb = cbf[:, 0:128]
    Ub = cbf[:, 128:256]
    onesb = cbf[:, 256:384]

    o = 0
    O_G1 = o; o += DC
    O_G2 = o; o += DC
    O_DIR = []
    for _ in range(2):
        d = {}
        d["cw"] = o; o += NH * 4
        d["cb"] = o; o += NH
        d["ba"] = o; o += NH
        d["bi"] = o; o += NH
        d["lam"] = o; o += NH
        O_DIR.append(d)
    O_CW = o; o += NG * CK
    O_CB = o; o += NG
    O_CG = o; o += NG
    O_CBETA = o; o += NG
    O_BG = o; o += E * FC
    O_BU = o; o += E * FC
    assert o == NSP

    def spc(off, n=1):
        return smallp[:, off:off + n]

    for dr in range(2):
        lam = spc(O_DIR[dr]["lam"], NH)
        P.op("act", lambda e, dr=dr, lam=lam: e.activation(out=cA[:, dr, 0:NH], in_=lam, func=AF.Exp, scale=-1.0),
             reads=["smallp"], writes=[("cA", dr)])
        P.op("act", lambda e, dr=dr: e.activation(out=cA[:, dr, 0:NH], in_=cA[:, dr, 0:NH], func=AF.Ln, bias=1.0),
             reads=[("cA", dr)], writes=[("cA", dr)])
        P.op("dve", lambda e, dr=dr: e.tensor_scalar(out=cA[:, dr, NH:2 * NH], in0=cA[:, dr, 0:NH], scalar1=-16.0,
                                                     scalar2=None, op0=ALU.mult),
             reads=[("cA", dr)], writes=[("cA2", dr)])
        P.op("dve", lambda e, dr=dr: e.tensor_scalar(out=cA[:, dr, 0:NH], in0=cA[:, dr, 0:NH], scalar1=-8.0,
                                                     scalar2=None, op0=ALU.mult),
             reads=[("cA", dr), ("cA2", dr)], writes=[("cA", dr)])

    m_persist = A.mark()

    hT = A.alloc([128, DC, TW], BF16, "hT")
    RING_IN = 4
    win_t = [A.alloc([128, DC, 128], BF16, "win") for _ in range(RING_IN)]
    wgt = [[A.alloc([128, NH, 128], BF16, "wa"), A.alloc([128, NH, 128], BF16, "wi")] for _ in range(2)]
    for dr in range(2):
        P.op("pool", lambda e, dr=dr: e.dma_start(out=wgt[dr][0][:], in_=wa_d[dr].rearrange("(h d) c -> d h c", d=128)),
             writes=[("wgt", dr, 0)], dma=True)
        P.op("pool", lambda e, dr=dr: e.dma_start(out=wgt[dr][1][:], in_=wi_d[dr].rearrange("(h d) c -> d h c", d=128)),
             writes=[("wgt", dr, 1)], dma=True)
    m_over = A.mark()

    win_rr = [0]

    def load_win(col0):
        s = win_rr[0]
        win_rr[0] = (s + 1) % RING_IN
        src = win_d[col0 // 128]
        dst = win_t[s][:, :, :].rearrange("p k c -> p (k c)")
        P.op("pool", lambda e: e.dma_start(out=dst, in_=src), writes=[("win", s)], dma=True)
        return s

    def norm_transpose(src_d, rows, col_off, xt, g1b, ssq, rstd, diag, junk):
        for ci, (r0, n) in enumerate(rows):
            b = ci % 2
            P.op("sp", lambda e, b=b, r0=r0, n=n: e.dma_start(out=xt[b][0:n, :], in_=src_d[r0:r0 + n, :]),
                 writes=[("xt", b)], dma=True)
            P.op("pool", lambda e, b=b: e.memset(ssq[b][:, :], 0.0), writes=[("ssq", b)])
            P.op("act", lambda e, b=b, n=n: e.activation(out=junk[0:n, :], in_=xt[b][0:n, :], func=AF.Square,
                                                       accum_out=ssq[b][0:n, :]),
                 reads=[("xt", b)], writes=[("ssq", b), "junk"])
            P.op("dve", lambda e, b=b, n=n: e.tensor_scalar(out=rstd[b][0:n, :], in0=ssq[b][0:n, :], scalar1=1.0 / D,
                                                          scalar2=EPS, op0=ALU.mult, op1=ALU.add),
                 reads=[("ssq", b)], writes=[("rstd", b)])
            P.op("act", lambda e, b=b, n=n: e.activation(out=rstd[b][0:n, :], in_=rstd[b][0:n, :], func=AF.Ln),
                 reads=[("rstd", b)], writes=[("rstd", b)])
            P.op("act", lambda e, b=b, n=n: e.activation(out=rstd[b][0:n, :], in_=rstd[b][0:n, :], func=AF.Exp,
                                                       scale=-0.5),
                 reads=[("rstd", b)], writes=[("rstd", b)])
            P.op("dve", lambda e, b=b, n=n: e.tensor_scalar(out=diag[b][0:n, 0:n], in0=ident[0:n, 0:n],
                                                          scalar1=rstd[b][0:n, :], scalar2=None, op0=ALU.mult),
                 reads=[("rstd", b), "cst"], writes=[("diag", b)])
            P.op("dve", lambda e, b=b, n=n: e.tensor_tensor(out=xt[b][0:n, :], in0=xt[b][0:n, :], in1=g1b[0:n, :],
                                                          op=ALU.mult),
                 reads=[("xt", b), "g1b"], writes=[("xt", b)])
            for q in range(DC // 4):
                bk = bank()
                for j in range(4):
                    dc = q * 4 + j
                    P.op("pe", lambda e, b=b, n=n, dc=dc, bk=bk, j=j: e.matmul(
                        PS[bk][:, j * 128:j * 128 + n], lhsT=xt[b][0:n, dc * 128:(dc + 1) * 128],
                        rhs=diag[b][0:n, 0:n], start=True, stop=True),
                        reads=[("xt", b), ("diag", b)], writes=[("ps", bk)])
                src = PS[bk][:, :].rearrange("p (j t) -> p j t", j=4)[:, :, 0:n]
                dst = hT[:, q * 4:q * 4 + 4, col_off + r0:col_off + r0 + n]
                if q % 2 == 0:
                    P.op("act", lambda e, src=src, dst=dst: e.activation(out=dst, in_=src, func=AF.Copy),
                         reads=[("ps", bk)], writes=[("hT", q)])
                else:
                    P.op("dve", lambda e, src=src, dst=dst: e.tensor_copy(out=dst, in_=src),
                         reads=[("ps", bk)], writes=[("hT", q)])

    def zmm(slot, c0, ncols):
        res = []
        for (t0, n) in _chunks(ncols, 512):
            bk = bank()
            for dc in range(DC):
                P.op("pe", lambda e, bk=bk, dc=dc, t0=t0, n=n: e.matmul(
                    PS[bk][:, 0:n], lhsT=win_t[slot][:, dc, :], rhs=hT[:, dc, c0 + t0:c0 + t0 + n],
                    start=(dc == 0), stop=(dc == DC - 1)),
                    reads=[("win", slot), ("hT", dc // 4)], writes=[("ps", bk)])
            res.append((bk, t0, n))
        return res

    def lru_dir(W, h, dr, xs, base, nT, sign, init, hout, rev, hkey):
        od = O_DIR[dr]
        u, ub, r, ig, a, q = W["u"], W["ub"], W["r"], W["i"], W["a"], W["q"]
        for k in range(4):
            off = base + (k if sign > 0 else -k)
            src = xs[:, off:off + nT]
            wk = spc(od["cw"] + h * 4 + k)
            if k == 0:
                P.op("dve", lambda e, src=src, wk=wk: e.tensor_scalar(
                    out=u[:, 0:nT], in0=src, scalar1=wk, scalar2=spc(od["cb"] + h), op0=ALU.mult, op1=ALU.add),
                    reads=["xs", "smallp"], writes=["u"])
            else:
                P.op("dve", lambda e, src=src, wk=wk: e.scalar_tensor_tensor(
                    out=u[:, 0:nT], in0=src, scalar=wk, in1=u[:, 0:nT], op0=ALU.mult, op1=ALU.add),
                    reads=["xs", "u", "smallp"], writes=["u"])
        P.op("act", lambda e: e.activation(out=ub[:, 0:nT], in_=u[:, 0:nT], func=AF.Copy), reads=["u"], writes=["ub"])
        for gi, (dst, bo) in enumerate(((r, od["ba"]), (ig, od["bi"]))):
            for (t0, n) in _chunks(nT, 512):
                bk = bank()
                P.op("pe", lambda e, bk=bk, t0=t0, n=n, gi=gi: e.matmul(
                    PS[bk][:, 0:n], lhsT=wgt[dr][gi][:, h, :], rhs=ub[:, t0:t0 + n], start=True, stop=True),
                    reads=["ub", ("wgt", dr, gi)], writes=[("ps", bk)])
                P.op("act", lambda e, bk=bk, t0=t0, n=n, dst=dst, bo=bo: e.activation(
                    out=dst[:, t0:t0 + n], in_=PS[bk][:, 0:n], func=AF.Sigmoid, bias=spc(bo + h)),
                    reads=[("ps", bk), "smallp"], writes=["r" if gi == 0 else "i"])
        P.op("act", lambda e: e.activation(out=a[:, 0:nT], in_=r[:, 0:nT], func=AF.Exp, scale=cA[:, dr, h:h + 1]),
             reads=["r", ("cA", dr)], writes=["a"])
        P.op("act", lambda e: e.activation(out=q[:, 0:nT], in_=r[:, 0:nT], func=AF.Exp,
                                           scale=cA[:, dr, NH + h:NH + h + 1]),
             reads=["r", ("cA2", dr)], writes=["q"])
        P.op("act", lambda e: e.activation(out=q[:, 0:nT], in_=q[:, 0:nT], func=AF.Sqrt, scale=-1.0, bias=1.0),
             reads=["q"], writes=["q"])
        P.op("pool", lambda e: e.tensor_tensor(out=ig[:, 0:nT], in0=ig[:, 0:nT], in1=u[:, 0:nT], op=ALU.mult),
             reads=["i", "u"], writes=["i"])
        P.op("dve", lambda e: e.tensor_tensor(out=q[:, 0:nT], in0=q[:, 0:nT], in1=ig[:, 0:nT], op=ALU.mult),
             reads=["q", "i"], writes=["q"])
        if rev:
            P.op("dve", lambda e: e.tensor_tensor_scan(out=hout[:, 0:nT][:, ::-1],
                                                       data0=a[:, 0:nT][:, ::-1], data1=q[:, 0:nT][:, ::-1],
                                                       initial=init, op0=ALU.mult, op1=ALU.add),
                 reads=["a", "q"], writes=[hkey])
        else:
            P.op("dve", lambda e: e.tensor_tensor_scan(out=hout[:, 0:nT], data0=a[:, 0:nT], data1=q[:, 0:nT],
                                                       initial=init, op0=ALU.mult, op1=ALU.add),
                 reads=["a", "q", "stA"], writes=[hkey])

    xt = [A.alloc([128, D], F32, "xt") for _ in range(2)]
    g1b = A.alloc([128, D], F32, "g1b")
    junk = A.alloc([128, D], F32, "junk")
    ssq = [A.alloc([128, 1], F32, "ssq") for _ in range(2)]
    rstd = [A.alloc([128, 1], F32, "rstd") for _ in range(2)]
    diag = [A.alloc([128, 128], F32, "diag") for _ in range(2)]
    P.op("sp", lambda e: e.dma_start(out=g1b[:], in_=g1b_d), writes=["g1b"], dma=True)
    norm_transpose(xo_d, tch, 0, xt, g1b, ssq, rstd, diag, junk)
    A.release(m_over)
    P.barrier()

    WB = {}
    XSW = TW + 4
    for nm in ("xs", "u", "r", "i", "a", "q", "hf", "hb", "g1", "g2"):
        WB[nm] = A.alloc([128, XSW], F32, nm)
    WB["ub"] = A.alloc([128, XSW], BF16, "ub")
    ysp = [A.alloc([128, TH], BF16, "ysp") for _ in range(2)]
    cbuf = A.alloc([128, TW + 16], BF16, "cbuf")
    dgt = A.alloc([128, CK, 128], BF16, "dgt")
    xs = WB["xs"]
    P.op("pool", lambda e: e.memset(xs[:, :], 0.0), writes=["xs"])
    P.op("pool", lambda e: e.memset(cbuf[:, :], 0.0), writes=["cbuf"])

    pending = [load_win(h * 128) for h in range(min(2, NH))]
    for h in range(NH):
        slot = pending.pop(0)
        if h + 2 < NH:
            pending.append(load_win((h + 2) * 128))
        zs = zmm(slot, 0, TH)
        for (bk, t0, n) in zs:
            P.op("act", lambda e, bk=bk, t0=t0, n=n: e.activation(out=xs[:, 3 + t0:3 + t0 + n], in_=PS[bk][:, 0:n],
                                                                  func=AF.Copy),
                 reads=[("ps", bk)], writes=["xs"])
        pre_issue(1)
        lru_dir(WB, h, 0, xs, 0, TH, +1, 0.0, WB["hf"], False, "hf")
        P.op("act", lambda e, h=h: e.activation(out=stA[:, h:h + 1], in_=WB["hf"][:, TH - 1:TH], func=AF.Copy),
             reads=["hf"], writes=["stA"])
    if debug:
        P.op("sp", lambda e: e.dma_start(out=dbg_st, in_=stA[:]), reads=["stA"], dma=True)
    P.barrier()

    if STOP == "A2":
        return finish()
    m_work = A.mark()
    A.release(m_over)
    xt = [A.alloc([128, D], F32, "xt") for _ in range(2)]
    g1b2 = A.alloc([128, D], F32, "g1b")
    junk = A.alloc([128, D], F32, "junk")
    ssq = [A.alloc([128, 1], F32, "ssq") for _ in range(2)]
    rstd = [A.alloc([128, 1], F32, "rstd") for _ in range(2)]
    diag = [A.alloc([128, 128], F32, "diag") for _ in range(2)]
    P.op("sp", lambda e: e.dma_start(out=g1b2[:], in_=g1b_d), writes=["g1b"], dma=True)
    rowsB = [(0, HALO)] + [(HALO + r0, n) for (r0, n) in tch]
    norm_transpose(xn_d, rowsB, 0, xt, g1b2, ssq, rstd, diag, junk)
    P.barrier()
    A.release(m_work)
    P.op("pool", lambda e: e.memset(xs[:, :], 0.0), writes=["xs"])
    P.op("pool", lambda e: e.memset(cbuf[:, :], 0.0), writes=["cbuf"])

    units = []
    for h in range(NH):
        units.append(("x", h, h * 128))
        units.append(("g", h, LW + h * 128))
    for g in range(NG):
        units.append(("a", g, 2 * LW + g * 128))
        units.append(("b", g, 2 * LW + CW + g * 128))
    LOOK = 3
    loaded = {}
    nxt = [0]

    def ensure(i):
        while nxt[0] < len(units) and nxt[0] <= i + LOOK - 1:
            loaded[nxt[0]] = load_win(units[nxt[0]][2])
            nxt[0] += 1
        return loaded[i]

    ui = 0
    for h in range(NH):
        sx = ensure(ui); ui += 1
        zs = zmm(sx, 0, TW)
        for (bk, t0, n) in zs:
            P.op("act", lambda e, bk=bk, t0=t0, n=n: e.activation(out=xs[:, t0:t0 + n], in_=PS[bk][:, 0:n],
                                                                  func=AF.Copy),
                 reads=[("ps", bk)], writes=["xs"])
        pre_issue(cfg.get('PRE_PER', 3))
        lru_dir(WB, h, 0, xs, HALO - 3, TH, +1, stA[:, h:h + 1], WB["hf"], False, "hf")
        lru_dir(WB, h, 1, xs, HALO + 3, TH, -1, 0.0, WB["hb"], True, "hb")
        sg = ensure(ui); ui += 1
        zg = zmm(sg, HALO, TH)
        g1t, g2t = WB["g1"], WB["g2"]
        for (bk, t0, n) in zg:
            P.op("act", lambda e, bk=bk, t0=t0, n=n: e.activation(out=g1t[:, t0:t0 + n], in_=PS[bk][:, 0:n],
                                                                  func=AF.Copy),
                 reads=[("ps", bk)], writes=["g1t"])
        P.op("pool", lambda e: e.tensor_tensor(out=g2t[:, 0:TH], in0=g1t[:, 0:TH], in1=g1t[:, 0:TH], op=ALU.mult),
             reads=["g1t"], writes=["g2t"])
        P.op("dve", lambda e: e.tensor_scalar(out=g2t[:, 0:TH], in0=g2t[:, 0:TH], scalar1=0.044715, scalar2=1.0,
                                              op0=ALU.mult, op1=ALU.add), reads=["g2t"], writes=["g2t"])
        P.op("pool", lambda e: e.tensor_tensor(out=g2t[:, 0:TH], in0=g2t[:, 0:TH], in1=g1t[:, 0:TH], op=ALU.mult),
             reads=["g1t", "g2t"], writes=["g2t"])
        P.op("act", lambda e: e.activation(out=g2t[:, 0:TH], in_=g2t[:, 0:TH], func=AF.Sigmoid, scale=1.5957691216),
             reads=["g2t"], writes=["g2t"])
        P.op("dve", lambda e: e.tensor_tensor(out=g2t[:, 0:TH], in0=g2t[:, 0:TH], in1=g1t[:, 0:TH], op=ALU.mult),
             reads=["g1t", "g2t"], writes=["g2t"])
        hf, hb = WB["hf"], WB["hb"]
        P.op("pool", lambda e: e.tensor_tensor(out=hf[:, 0:TH], in0=hf[:, 0:TH], in1=hb[:, 0:TH], op=ALU.add),
             reads=["hf", "hb"], writes=["hf"])
        yb = h % 2
        P.op("dve", lambda e, yb=yb: e.tensor_tensor(out=ysp[yb][:, :], in0=hf[:, 0:TH], in1=g2t[:, 0:TH], op=ALU.mult),
             reads=["hf", "g2t"], writes=[("ysp", yb)])
        P.op("sp", lambda e, yb=yb, h=h: e.dma_start(out=yT_d[h], in_=ysp[yb][:, :]), reads=[("ysp", yb)], dma=True)

    if STOP == "B2":
        return finish()
    co, xc, sq = WB["u"], WB["r"], WB["i"]
    for g in range(NG):
        sa = ensure(ui); ui += 1
        sb = ensure(ui); ui += 1
        pre_issue(4)
        za = zmm(sa, 0, TW)
        zb = zmm(sb, 0, TW)
        sgt = WB["a"]
        for (bk, t0, n) in zb:
            P.op("act", lambda e, bk=bk, t0=t0, n=n: e.activation(out=sgt[:, t0:t0 + n], in_=PS[bk][:, 0:n],
                                                                  func=AF.Sigmoid),
                 reads=[("ps", bk)], writes=["sgt"])
        for (bk, t0, n) in za:
            P.op("dve", lambda e, bk=bk, t0=t0, n=n: e.tensor_tensor(out=cbuf[:, t0:t0 + n], in0=sgt[:, t0:t0 + n],
                                                                    in1=PS[bk][:, 0:n], op=ALU.mult),
                 reads=[("ps", bk), "sgt"], writes=["cbuf"])
        for k in range(CK):
            P.op("dve", lambda e, k=k, g=g: e.tensor_scalar(out=dgt[:, k, :], in0=ident, scalar1=spc(O_CW + g * CK + k),
                                                           scalar2=None, op0=ALU.mult),
                 reads=["cst", "smallp"], writes=["dgt"])
        cz = []
        for (t0, n) in _chunks(TH, 512):
            bk = bank()
            for k in range(CK):
                P.op("pe", lambda e, bk=bk, k=k, t0=t0, n=n: e.matmul(
                    PS[bk][:, 0:n], lhsT=dgt[:, k, :], rhs=cbuf[:, HALO - 15 + k + t0:HALO - 15 + k + t0 + n],
                    start=(k == 0), stop=(k == CK - 1)), reads=["dgt", "cbuf"], writes=[("ps", bk)])
            cz.append((bk, t0, n))
        for (bk, t0, n) in cz:
            P.op("act", lambda e, bk=bk, t0=t0, n=n, g=g: e.activation(out=co[:, t0:t0 + n], in_=PS[bk][:, 0:n],
                                                                       func=AF.Identity, bias=spc(O_CB + g)),
                 reads=[("ps", bk), "smallp"], writes=["co"])
        for (t0, n) in _chunks(TH, 512):
            bk = bank()
            P.op("pe", lambda e, bk=bk, t0=t0, n=n: e.matmul(PS[bk][:, 0:n], lhsT=onesM, rhs=co[:, t0:t0 + n],
                                                            start=True, stop=True),
                 reads=["co", "cst"], writes=[("ps", bk)])
            P.op("dve", lambda e, bk=bk, t0=t0, n=n: e.tensor_tensor(out=xc[:, t0:t0 + n], in0=co[:, t0:t0 + n],
                                                                    in1=PS[bk][:, 0:n], op=ALU.subtract),
                 reads=[("ps", bk), "co"], writes=["xc"])
            P.op("act", lambda e, t0=t0, n=n: e.activation(out=sq[:, t0:t0 + n], in_=xc[:, t0:t0 + n], func=AF.Square),
                 reads=["xc"], writes=["sq"])
            bk2 = bank()
            P.op("pe", lambda e, bk2=bk2, t0=t0, n=n: e.matmul(PS[bk2][:, 0:n], lhsT=onesM, rhs=sq[:, t0:t0 + n],
                                                              start=True, stop=True),
                 reads=["sq", "cst"], writes=[("ps", bk2)])
            P.op("dve", lambda e, bk2=bk2, t0=t0, n=n: e.tensor_scalar(out=sq[:, t0:t0 + n], in0=PS[bk2][:, 0:n],
                                                                      scalar1=EPS, scalar2=None, op0=ALU.add),
                 reads=[("ps", bk2)], writes=["sq"])
            P.op("act", lambda e, t0=t0, n=n: e.activation(out=sq[:, t0:t0 + n], in_=sq[:, t0:t0 + n], func=AF.Ln),
                 reads=["sq"], writes=["sq"])
            P.op("act", lambda e, t0=t0, n=n: e.activation(out=sq[:, t0:t0 + n], in_=sq[:, t0:t0 + n], func=AF.Exp,
                                                           scale=-0.5), reads=["sq"], writes=["sq"])
            P.op("pool", lambda e, t0=t0, n=n: e.tensor_tensor(out=xc[:, t0:t0 + n], in0=xc[:, t0:t0 + n],
                                                              in1=sq[:, t0:t0 + n], op=ALU.mult),
                 reads=["xc", "sq"], writes=["xc"])
        yb = g % 2
        P.op("act", lambda e, yb=yb, g=g: e.activation(out=ysp[yb][:, :], in_=xc[:, 0:TH], func=AF.Silu,
                                                       scale=spc(O_CG + g), bias=spc(O_CBETA + g)),
             reads=["xc", "smallp"], writes=[("ysp", yb)])
        P.op("sp", lambda e, yb=yb, g=g: e.dma_start(out=yT_d[NH + g], in_=ysp[yb][:, :]), reads=[("ysp", yb)],
             dma=True)
    P.barrier()

    if STOP == "B3":
        return finish()
    A.release(m_persist)
    yT = A.alloc([128, 2 * NH, TH], BF16, "yT")
    wo_t = [A.alloc([128, 2 * NH, 512], BF16, "wo") for _ in range(2)]
    xblk = [A.alloc([128, 512], F32, "xblk") for _ in range(3)]
    x1blk = [A.alloc([128, 512], F32, "x1blk") for _ in range(3)]
    jk5 = A.alloc([128, 512], F32, "jk5")
    ssq2 = A.alloc([128, NCH, DB], F32, "ssq2")
    rstd2 = A.alloc([128, NCH], F32, "rstd2")
    m_b4 = A.mark()
    for u_ in range(2 * NH):
        P.op("sp", lambda e, u_=u_: e.dma_start(out=yT[:, u_, :], in_=yT_d[u_]), writes=["yT"], dma=True)
    P.op("dve", lambda e: e.memset(ssq2[:], 0.0), writes=["ssq2"])

    def load_wo(db):
        s = db % 2
        src = wout_d[db]
        dst = wo_t[s][:, :, :].rearrange("p k c -> p (k c)")
        P.op("pool", lambda e: e.dma_start(out=dst, in_=src), writes=[("wo", s)], dma=True)

    load_wo(0)
    it = 0
    for db in range(DB):
        pre_issue((len(pre_list) - pre_pos[0] + DB - db - 1) // (DB - db))
        if db + 1 < DB:
            load_wo(db + 1)
        s = db % 2
        for ci, (r0, n) in enumerate(tch):
            b3 = it % 3
            it += 1
            P.op("sp", lambda e, b3=b3, r0=r0, n=n, db=db: e.dma_start(
                out=xblk[b3][0:n, :], in_=xn_d[HALO + r0:HALO + r0 + n, db * 512:(db + 1) * 512]),
                writes=[("xblk", b3)], dma=True)
            bk = bank()
            for cc in range(2 * NH):
                P.op("pe", lambda e, bk=bk, cc=cc, r0=r0, n=n, s=s: e.matmul(
                    PS[bk][0:n, :], lhsT=yT[:, cc, r0:r0 + n], rhs=wo_t[s][:, cc, :],
                    start=(cc == 0), stop=(cc == 2 * NH - 1)), reads=["yT", ("wo", s)], writes=[("ps", bk)])
            P.op("dve", lambda e, bk=bk, b3=b3, n=n: e.tensor_tensor(out=x1blk[b3][0:n, :], in0=xblk[b3][0:n, :],
                                                                    in1=PS[bk][0:n, :], op=ALU.add),
                 reads=[("ps", bk), ("xblk", b3)], writes=[("x1blk", b3)])
            P.op("act", lambda e, b3=b3, n=n, ci=ci, db=db: e.activation(
                out=jk5[0:n, :], in_=x1blk[b3][0:n, :], func=AF.Square, accum_out=ssq2[0:n, ci, db:db + 1]),
                reads=[("x1blk", b3)], writes=["jk5", "ssq2"])
            P.op("sp", lambda e, b3=b3, r0=r0, n=n, db=db: e.dma_start(
                out=x1_d[r0:r0 + n, db * 512:(db + 1) * 512], in_=x1blk[b3][0:n, :]),
                reads=[("x1blk", b3)], writes=["x1_d"], dma=True)
    P.op("dve", lambda e: e.tensor_reduce(out=rstd2[:, :], in_=ssq2[:, :, :], axis=AX.X, op=ALU.add),
         reads=["ssq2"], writes=["rstd2"])
    P.op("dve", lambda e: e.tensor_scalar(out=rstd2[:, :], in0=rstd2[:, :], scalar1=1.0 / D, scalar2=EPS,
                                          op0=ALU.mult, op1=ALU.add), reads=["rstd2"], writes=["rstd2"])
    P.op("act", lambda e: e.activation(out=rstd2[:, :], in_=rstd2[:, :], func=AF.Ln), reads=["rstd2"], writes=["rstd2"])
    P.op("act", lambda e: e.activation(out=rstd2[:, :], in_=rstd2[:, :], func=AF.Exp, scale=-0.5), reads=["rstd2"],
         writes=["rstd2"])
    P.barrier()

    if STOP == "B4":
        return finish()
    A.release(m_persist)
    rstd2b = A.alloc([128, NCH], F32, "rstd2b")
    P.op("dve", lambda e: e.tensor_copy(out=rstd2b[:, :], in_=rstd2[:, :]), reads=["rstd2"], writes=["rstd2b"])
    P.barrier()
    Gt = A.alloc([128, NCH, E], F32, "G")
    GHL = A.alloc([128, NCH, E, 2], BF16, "GHL")
    maskf = A.alloc([128, NCH, E], F32, "maskf")
    maskb = A.alloc([128, NCH, E], BF16, "maskb")
    pos = A.alloc([128, NCH, E], F32, "pos")
    idxi = A.alloc([128, NCH, K], I32, "idxi")
    m_tables = A.mark()
    h2 = A.alloc([128, NCH, D], BF16, "h2")
    m_moe = A.mark()
    x1c = [A.alloc([128, D], F32, "x1c") for _ in range(2)]
    hT2 = A.alloc([128, DC, 128], F32, "hT2")
    wr_t = A.alloc([128, DC, E], F32, "wr")
    diag2 = A.alloc([128, 128], F32, "diag2")
    lg = A.alloc([128, E], F32, "lg")
    wk = A.alloc([128, E], F32, "wk")
    eq = A.alloc([128, E], F32, "eq")
    mx = A.alloc([128, 8], F32, "mx")
    rk = A.alloc([128, E], F32, "rk")
    sl = A.alloc([128, E], F32, "sl")
    idxf = A.alloc([128, K], F32, "idxf")
    P.op("sp", lambda e: e.dma_start(out=wr_t[:], in_=wr_d.rearrange("(k p) c -> p k c", p=128)), writes=["wr"],
         dma=True)
    for ci, (r0, n) in enumerate(tch):
        b = ci % 2
        P.op("sp", lambda e, b=b, r0=r0, n=n: e.dma_start(out=x1c[b][0:n, :], in_=x1_d[r0:r0 + n, :]),
             reads=["x1_d"], writes=[("x1c", b)], dma=True)
        if LVL < -2:
            continue
        P.op("act", lambda e, b=b, n=n, ci=ci: e.activation(out=h2[0:n, ci, :], in_=x1c[b][0:n, :], func=AF.Identity,
                                                          scale=rstd2b[0:n, ci:ci + 1]),
             reads=[("x1c", b), "rstd2b"], writes=["h2"])
        if LVL < -1:
            continue
        P.op("dve", lambda e, n=n, ci=ci: e.tensor_scalar(out=diag2[0:n, 0:n], in0=ident[0:n, 0:n],
                                                        scalar1=rstd2b[0:n, ci:ci + 1], scalar2=None, op0=ALU.mult),
             reads=["rstd2b", "cst"], writes=["diag2"])
        for q in range(DC // 4):
            bk = bank()
            for j in range(4):
                dc = q * 4 + j
                P.op("pe", lambda e, b=b, n=n, dc=dc, bk=bk, j=j: e.matmul(
                    PS[bk][:, j * 128:j * 128 + n], lhsT=x1c[b][0:n, dc * 128:(dc + 1) * 128],
                    rhs=diag2[0:n, 0:n], start=True, stop=True),
                    reads=[("x1c", b), "diag2"], writes=[("ps", bk)])
            for j in range(4):
                dc = q * 4 + j
                eng = "act" if (j % 2 == 0 and cfg.get("EVAC", "dve") == "mix") else "dve"
                if eng == "act":
                    P.op("act", lambda e, bk=bk, j=j, dc=dc, n=n: e.activation(
                        out=hT2[:, dc, 0:n], in_=PS[bk][:, j * 128:j * 128 + n], func=AF.Identity, scale=spc(O_G2 + dc)),
                        reads=[("ps", bk), "smallp"], writes=[("hT2", dc)])
                else:
                    P.op("dve", lambda e, bk=bk, j=j, dc=dc, n=n: e.tensor_scalar(
                        out=hT2[:, dc, 0:n], in0=PS[bk][:, j * 128:j * 128 + n], scalar1=spc(O_G2 + dc), scalar2=None,
                        op0=ALU.mult), reads=[("ps", bk), "smallp"], writes=[("hT2", dc)])
        if LVL < 0:
            continue
        bk = bank()
        for dc in range(DC):
            P.op("pe", lambda e, bk=bk, dc=dc, n=n: e.matmul(PS[bk][0:n, 0:E], lhsT=hT2[:, dc, 0:n], rhs=wr_t[:, dc, :],
                                                            start=(dc == 0), stop=(dc == DC - 1)),
                 reads=[("hT2", dc), "wr"], writes=[("ps", bk)])
        P.op("dve", lambda e, bk=bk, n=n: e.tensor_tensor(out=lg[0:n, :], in0=PS[bk][0:n, 0:E], in1=brt[0:n, :],
                                                         op=ALU.add), reads=[("ps", bk), "cst"], writes=["lg"])
        if debug:
            P.op("sp", lambda e, r0=r0, n=n: e.dma_start(out=dbg_lg[r0:r0 + n, :], in_=lg[0:n, :]), reads=["lg"],
                 dma=True)
        if LVL < 1:
            continue
        P.op("dve", lambda e, n=n: e.tensor_copy(out=wk[0:n, :], in_=lg[0:n, :]), reads=["lg"], writes=["wk"])
        for kk in range(K):
            P.op("dve", lambda e, n=n, kk=kk: e.tensor_reduce(out=mx[0:n, kk:kk + 1], in_=wk[0:n, :], axis=AX.X,
                                                            op=ALU.max), reads=["wk"], writes=["mx"])
            if kk < K - 1:
                P.op("dve", lambda e, n=n, kk=kk: e.tensor_scalar(out=eq[0:n, :], in0=wk[0:n, :],
                                                                scalar1=mx[0:n, kk:kk + 1], scalar2=-1e30,
                                                                op0=ALU.is_equal, op1=ALU.mult),
                     reads=["wk", "mx"], writes=["eq"])
                P.op("dve", lambda e, n=n: e.tensor_tensor(out=wk[0:n, :], in0=wk[0:n, :], in1=eq[0:n, :], op=ALU.add),
                     reads=["wk", "eq"], writes=["wk"])
        P.op("dve", lambda e, n=n, ci=ci: e.tensor_scalar(out=maskf[0:n, ci, :], in0=lg[0:n, :],
                                                        scalar1=mx[0:n, K - 1:K], scalar2=None, op0=ALU.is_ge),
             reads=["lg", "mx"], writes=["maskf"])
        P.op("dve", lambda e, n=n, ci=ci: e.tensor_copy(out=maskb[0:n, ci, :], in_=maskf[0:n, ci, :]),
             reads=["maskf"], writes=["maskb"])
        P.op("dve", lambda e, n=n: e.tensor_scalar(out=mx[0:n, 4:5], in0=mx[0:n, 0:1], scalar1=-1.0, scalar2=None,
                                                 op0=ALU.mult), reads=["mx"], writes=["mx"])
        P.op("act", lambda e, n=n: e.activation(out=wk[0:n, :], in_=lg[0:n, :], func=AF.Exp, bias=mx[0:n, 4:5]),
             reads=["lg", "mx"], writes=["wk"])
        P.op("dve", lambda e, n=n, ci=ci: e.tensor_tensor(out=wk[0:n, :], in0=wk[0:n, :], in1=maskf[0:n, ci, :],
                                                        op=ALU.mult), reads=["wk", "maskf"], writes=["wk"])
        P.op("dve", lambda e, n=n: e.tensor_reduce(out=mx[0:n, 5:6], in_=wk[0:n, :], axis=AX.X, op=ALU.add),
             reads=["wk"], writes=["mx"])
        P.op("dve", lambda e, n=n: e.reciprocal(out=mx[0:n, 6:7], in_=mx[0:n, 5:6]), reads=["mx"], writes=["mx"])
        P.op("dve", lambda e, n=n, ci=ci: e.tensor_scalar(out=Gt[0:n, ci, :], in0=wk[0:n, :], scalar1=mx[0:n, 6:7],
                                                        scalar2=None, op0=ALU.mult),
             reads=["wk", "mx"], writes=["G"])
        if LVL < 2:
            continue
        P.op("dve", lambda e, n=n, ci=ci: e.tensor_copy(out=GHL[0:n, ci, :, 0], in_=Gt[0:n, ci, :]),
             reads=["G"], writes=["GHL"])
        P.op("dve", lambda e, n=n, ci=ci: e.tensor_tensor(out=eq[0:n, :], in0=Gt[0:n, ci, :], in1=GHL[0:n, ci, :, 0],
                                                        op=ALU.subtract), reads=["G", "GHL"], writes=["eq"])
        P.op("dve", lambda e, n=n, ci=ci: e.tensor_copy(out=GHL[0:n, ci, :, 1], in_=eq[0:n, :]),
             reads=["eq"], writes=["GHL"])
        if debug:
            P.op("sp", lambda e, r0=r0, n=n, ci=ci: e.dma_start(out=dbg_G[r0:r0 + n, :], in_=Gt[0:n, ci, :]),
                 reads=["G"], dma=True)
        if LVL < 3:
            continue
        bk = bank()
        for kc in range(ci + 1):
            m = tch[kc][1]
            lhs = onesb[0:m, 0:n] if kc < ci else Ub[0:m, 0:n]
            P.op("pe", lambda e, bk=bk, kc=kc, m=m, n=n, lhs=lhs, ci=ci: e.matmul(
                PS[bk][0:n, 0:E], lhsT=lhs, rhs=maskb[0:m, kc, :], start=(kc == 0), stop=(kc == ci)),
                reads=["maskb", "cbf"], writes=[("ps", bk)])
        P.op("act", lambda e, bk=bk, n=n, ci=ci: e.activation(out=pos[0:n, ci, :], in_=PS[bk][0:n, 0:E], func=AF.Copy),
             reads=[("ps", bk)], writes=["pos"])
        if LVL < 4:
            continue
        P.op("dve", lambda e, n=n, ci=ci: e.tensor_tensor(out=sl[0:n, :], in0=pos[0:n, ci, :], in1=eoff[0:n, :],
                                                        op=ALU.add), reads=["pos", "cst"], writes=["sl"])
        P.op("dve", lambda e, n=n, ci=ci: e.tensor_tensor_scan(out=rk[0:n, :], data0=ones1[0:n, 0:E],
                                                             data1=maskf[0:n, ci, :], initial=0.0, op0=ALU.mult,
                                                             op1=ALU.add), reads=["maskf", "cst"], writes=["rk"])
        for kk in range(K):
            P.op("dve", lambda e, n=n, ci=ci, kk=kk: e.scalar_tensor_tensor(
                out=eq[0:n, :], in0=rk[0:n, :], scalar=float(kk + 1), in1=maskf[0:n, ci, :], op0=ALU.is_equal,
                op1=ALU.mult), reads=["rk", "maskf"], writes=["eq"])
            P.op("dve", lambda e, n=n: e.tensor_tensor(out=eq[0:n, :], in0=eq[0:n, :], in1=sl[0:n, :], op=ALU.mult),
                 reads=["eq", "sl"], writes=["eq"])
            P.op("dve", lambda e, n=n, kk=kk: e.tensor_reduce(out=idxf[0:n, kk:kk + 1], in_=eq[0:n, :], axis=AX.X,
                                                            op=ALU.add), reads=["eq"], writes=["idxf"])
        P.op("dve", lambda e, n=n, ci=ci: e.tensor_copy(out=idxi[0:n, ci, :], in_=idxf[0:n, :]),
             reads=["idxf"], writes=["idxi"])
        if debug:
            P.op("sp", lambda e, r0=r0, n=n, ci=ci: e.dma_start(out=dbg_idx[r0:r0 + n, :], in_=idxi[0:n, ci, :]),
                 reads=["idxi"], dma=True)
    P.barrier()

    if STOP == "B5":
        return finish()
    A.release(m_moe)
    RING = cfg.get("RING", 5)
    wt = [A.alloc([128, 16 * 512], BF16, "wt") for _ in range(RING)]
    xT = A.alloc([128, DC, CAP], BF16, "xT")
    actT = A.alloc([128, FC, CAP], BF16, "actT")
    sel_one = A.alloc([128, NCH, CAP], BF16, "sel")
    sel = [sel_one, sel_one]
    gsl = [A.alloc([128, CC], F32, "gsl") for _ in range(2)]
    NT = 2
    hgt = [A.alloc([128, CAP], F32, "hg") for _ in range(NT)]
    sgt2 = [A.alloc([128, CAP], F32, "sg") for _ in range(NT)]
    hut = [A.alloc([128, CAP], F32, "hu") for _ in range(NT)]
    yst = [A.alloc([128, 512], F32, "yst") for _ in range(2)]
    if cfg.get("VERBOSE"):
        print("MoE phase SBUF end", A.cur, "of", A.hi)

    tiles = []
    for e_ in range(E):
        for fb in range(FB):
            tiles.append(("g", e_, fb))
            tiles.append(("u", e_, fb))
        for db in range(DB):
            tiles.append(("d", e_, db))
    tslot = {}
    tnext = [0]

    def tile_view(s, kind):
        if kind == "d":
            return wt[s][:, 0:FC * 512].rearrange("p (k c) -> p k c", k=FC)
        return wt[s][:, 0:DC * 256].rearrange("p (k c) -> p k c", k=DC)

    def ensure_tile(i):
        while tnext[0] < len(tiles) and tnext[0] <= i + RING - 2:
            j = tnext[0]
            kind, e_, blk = tiles[j]
            s = j % RING
            pre = e_ < NPRE
            if kind == "d":
                src = (wdb_d if pre else wd_d)[e_ * DB + blk]
                dst = wt[s][:, 0:FC * 512]
                pk = ("pre", "d", e_ * DB + blk)
            elif kind == "g":
                src = (wgb_d if pre else wg_d)[e_ * FB + blk]
                dst = wt[s][:, 0:DC * 256]
                pk = ("pre", "g", e_ * FB + blk)
            else:
                src = (wub_d if pre else wu_d)[e_ * FB + blk]
                dst = wt[s][:, 0:DC * 256]
                pk = ("pre", "u", e_ * FB + blk)
            P.op("pool", lambda e, dst=dst, src=src: e.dma_start(out=dst, in_=src), reads=([pk] if pre else []),
                 writes=[("wt", s)], dma=True)
            tslot[j] = s
            tnext[0] += 1
        return tslot[i]

    def build_sel(e_):
        sb = e_ % 2
        for ci, (r0, n) in enumerate(tch):
            P.op("dve", lambda e, sb=sb, ci=ci, n=n, e_=e_: e.tensor_scalar(
                out=sel[sb][0:n, ci, :], in0=iotaC[0:n, :], scalar1=pos[0:n, ci, e_:e_ + 1],
                scalar2=maskf[0:n, ci, e_:e_ + 1], op0=ALU.is_equal, op1=ALU.mult),
                reads=["pos", "maskf", "cst"], writes=["sel"])
        bk = bank()
        for cc in range(CC):
            for ci, (r0, n) in enumerate(tch):
                P.op("pe", lambda e, sb=sb, ci=ci, n=n, cc=cc, bk=bk, e_=e_: e.matmul(
                    PS[bk][:, cc * 2:cc * 2 + 2], lhsT=sel[sb][0:n, ci, cc * 128:(cc + 1) * 128],
                    rhs=GHL[0:n, ci, e_, :], start=(ci == 0 and cc == 0), stop=(ci == NCH - 1),
                    skip_group_check=True),
                    reads=["sel", "GHL"], writes=[("ps", bk)])
        for cc in range(CC):
            P.op("dve", lambda e, sb=sb, cc=cc, bk=bk: e.tensor_reduce(
                out=gsl[sb][:, cc:cc + 1], in_=PS[bk][:, cc * 2:cc * 2 + 2], axis=AX.X, op=ALU.add),
                reads=[("ps", bk)], writes=[("gsl", sb)])

    def gather(e_, dcs=None):
        sb = e_ % 2
        for dc in (range(DC) if dcs is None else dcs):
            bk = bank()
            for ci, (r0, n) in enumerate(tch):
                P.op("pe", lambda e, sb=sb, ci=ci, n=n, dc=dc, bk=bk: e.matmul(
                    PS[bk][:, 0:CAP], lhsT=h2[0:n, ci, dc * 128:(dc + 1) * 128], rhs=sel[sb][0:n, ci, :],
                    start=(ci == 0), stop=(ci == NCH - 1)), reads=["h2", "sel"], writes=[("ps", bk)])
            if False:
                pass
            else:
                P.op("dve", lambda e, dc=dc, bk=bk: e.tensor_scalar(out=xT[:, dc, :], in0=PS[bk][:, 0:CAP],
                                                                   scalar1=spc(O_G2 + dc), scalar2=None, op0=ALU.mult),
                     reads=[("ps", bk), "smallp"], writes=[("xT", dc)])

    ti = 0
    build_sel(0)
    gather(0)
    tcount = 0
    ycount = 0
    for e_ in range(E):
        for fb in range(FB):
            sg_ = ensure_tile(ti); ti += 1
            su_ = ensure_tile(ti); ti += 1
            wgv = tile_view(sg_, "g")
            wuv = tile_view(su_, "u")
            for fcl in range(2):
                fc = fb * 2 + fcl
                bg_ = bank()
                for dc in range(DC):
                    P.op("pe", lambda e, bg_=bg_, dc=dc, fcl=fcl, wgv=wgv: e.matmul(
                        PS[bg_][:, 0:CAP], lhsT=wgv[:, dc, fcl * 128:(fcl + 1) * 128], rhs=xT[:, dc, :],
                        start=(dc == 0), stop=(dc == DC - 1)), reads=[("wt", sg_), ("xT", dc)], writes=[("ps", bg_)])
                bu_ = bank()
                for dc in range(DC):
                    P.op("pe", lambda e, bu_=bu_, dc=dc, fcl=fcl, wuv=wuv: e.matmul(
                        PS[bu_][:, 0:CAP], lhsT=wuv[:, dc, fcl * 128:(fcl + 1) * 128], rhs=xT[:, dc, :],
                        start=(dc == 0), stop=(dc == DC - 1)), reads=[("wt", su_), ("xT", dc)], writes=[("ps", bu_)])
                tb = tcount % NT
                tcount += 1
                bgc = spc(O_BG + e_ * FC + fc)
                buc = spc(O_BU + e_ * FC + fc)
                P.op("dve", lambda e, tb=tb, bg_=bg_, bgc=bgc: e.tensor_scalar(
                    out=hgt[tb][:, :], in0=PS[bg_][:, 0:CAP], scalar1=bgc, scalar2=7.0, op0=ALU.add, op1=ALU.min),
                    reads=[("ps", bg_), "smallp"], writes=[("hg", tb)])
                P.op("act", lambda e, tb=tb: e.activation(out=sgt2[tb][:, :], in_=hgt[tb][:, :], func=AF.Sigmoid,
                                                         scale=1.702), reads=[("hg", tb)], writes=[("sg", tb)])
                P.op("dve", lambda e, tb=tb, bu_=bu_, buc=buc: e.tensor_scalar(
                    out=hut[tb][:, :], in0=PS[bu_][:, 0:CAP], scalar1=buc, scalar2=7.0, op0=ALU.add, op1=ALU.min),
                    reads=[("ps", bu_), "smallp"], writes=[("hu", tb)])
                P.op("dve", lambda e, tb=tb: e.tensor_scalar(out=hut[tb][:, :], in0=hut[tb][:, :], scalar1=-7.0,
                                                            scalar2=1.0, op0=ALU.max, op1=ALU.add),
                     reads=[("hu", tb)], writes=[("hu", tb)])
                P.op("dve", lambda e, tb=tb: e.tensor_tensor(out=hgt[tb][:, :], in0=hgt[tb][:, :], in1=sgt2[tb][:, :],
                                                            op=ALU.mult), reads=[("hg", tb), ("sg", tb)],
                     writes=[("hg", tb)])
                P.op("dve", lambda e, tb=tb, fc=fc: e.tensor_tensor(out=actT[:, fc, :], in0=hgt[tb][:, :],
                                                                   in1=hut[tb][:, :], op=ALU.mult),
                     reads=[("hg", tb), ("hu", tb)], writes=["actT"])
        if e_ + 1 < E:
            build_sel(e_ + 1)
        sb = e_ % 2
        for db in range(DB):
            if e_ + 1 < E:
                gather(e_ + 1, range(db * DC // DB, (db + 1) * DC // DB))
            sd_ = ensure_tile(ti); ti += 1
            wdv = tile_view(sd_, "d")
            for cc in range(CC):
                yb = ycount % 2
                ycount += 1
                bk = bank()
                for fc in range(FC):
                    P.op("pe", lambda e, bk=bk, fc=fc, cc=cc, wdv=wdv: e.matmul(
                        PS[bk][:, :], lhsT=actT[:, fc, cc * 128:(cc + 1) * 128], rhs=wdv[:, fc, :],
                        start=(fc == 0), stop=(fc == FC - 1)), reads=["actT", ("wt", sd_)], writes=[("ps", bk)])
                P.op("dve", lambda e, bk=bk, cc=cc, yb=yb, sb=sb: e.tensor_scalar(
                    out=yst[yb][:, :], in0=PS[bk][:, :], scalar1=gsl[sb][:, cc:cc + 1], scalar2=None, op0=ALU.mult),
                    reads=[("ps", bk), ("gsl", sb)], writes=[("yst", yb)])
                dst = Y_d[e_ * CAP + cc * 128:e_ * CAP + (cc + 1) * 128, db * 512:(db + 1) * 512]
                P.op("sp", lambda e, yb=yb, dst=dst: e.dma_start(out=dst, in_=yst[yb][:, :]), reads=[("yst", yb)],
                     writes=["Y_d"], dma=True)
    P.barrier()

    if STOP == "B6":
        return finish()
    A.release(m_tables)
    ga2 = [[A.alloc([128, D], F32, "ga") for _ in range(K)] for _ in range(2)]
    x1f = A.alloc([128, D], F32, "x1f")
    gfb = A.alloc([128, D], F32, "gfb")
    bdn = A.alloc([128, D], F32, "bdn")
    GT = A.alloc([128, 128], F32, "GT")
    s7 = A.alloc([128, 4], F32, "s7")
    P.op("sp", lambda e: e.dma_start(out=gfb[:], in_=gfb_d), writes=["gfb"], dma=True)
    P.op("sp", lambda e: e.dma_start(out=bdn[0:E, :], in_=bd_d), writes=["bdn"], dma=True)
    for ci, (r0, n) in enumerate(tch):
        ga = ga2[ci % 2]
        gp = ci % 2
        P.op("sp", lambda e, r0=r0, n=n: e.dma_start(out=x1f[0:n, :], in_=x1_d[r0:r0 + n, :]), reads=["x1_d"],
             writes=["x1f"], dma=True)
        for kk in range(K):
            P.op("pool", lambda e, kk=kk, n=n, ci=ci, ga=ga: e.indirect_dma_start(
                out=ga[kk][0:n, :], out_offset=None, in_=Y_d,
                in_offset=bass.IndirectOffsetOnAxis(ap=idxi[0:n, ci, kk:kk + 1], axis=0)),
                reads=["Y_d", "idxi"], writes=[("ga", gp, kk)], dma=True)
        bk = bank()
        P.op("pe", lambda e, bk=bk, n=n, ci=ci: e.matmul(PS[bk][0:E, 0:n], lhsT=Gt[0:n, ci, :], rhs=ident[0:n, 0:n],
                                                        start=True, stop=True), reads=["G", "cst"], writes=[("ps", bk)])
        P.op("act", lambda e, bk=bk, n=n: e.activation(out=GT[0:E, 0:n], in_=PS[bk][0:E, 0:n], func=AF.Copy),
             reads=[("ps", bk)], writes=["GT"])
        for db in range(DB):
            bk = bank()
            P.op("pe", lambda e, bk=bk, n=n, db=db: e.matmul(PS[bk][0:n, :], lhsT=GT[0:E, 0:n],
                                                            rhs=bdn[0:E, db * 512:(db + 1) * 512], start=True,
                                                            stop=True), reads=["GT", "bdn"], writes=[("ps", bk)])
            P.op("dve", lambda e, bk=bk, n=n, db=db: e.tensor_tensor(
                out=x1f[0:n, db * 512:(db + 1) * 512], in0=x1f[0:n, db * 512:(db + 1) * 512], in1=PS[bk][0:n, :],
                op=ALU.add), reads=[("ps", bk), "x1f"], writes=["x1f"])
        P.op("pool", lambda e, n=n, ga=ga: e.tensor_tensor(out=ga[0][0:n, :], in0=ga[0][0:n, :], in1=ga[1][0:n, :],
                                                   op=ALU.add), reads=[("ga", gp, 0), ("ga", gp, 1)], writes=[("ga", gp, 0)])
        P.op("dve", lambda e, n=n, ga=ga: e.tensor_tensor(out=ga[2][0:n, :], in0=ga[2][0:n, :], in1=ga[3][0:n, :],
                                                  op=ALU.add), reads=[("ga", gp, 2), ("ga", gp, 3)], writes=[("ga", gp, 2)])
        P.op("pool", lambda e, n=n, ga=ga: e.tensor_tensor(out=ga[0][0:n, :], in0=ga[0][0:n, :], in1=ga[2][0:n, :],
                                                   op=ALU.add), reads=[("ga", gp, 0), ("ga", gp, 2)], writes=[("ga", gp, 0)])
        P.op("dve", lambda e, n=n, ga=ga: e.tensor_tensor(out=x1f[0:n, :], in0=x1f[0:n, :], in1=ga[0][0:n, :], op=ALU.add),
             reads=[("ga", gp, 0), "x1f"], writes=["x1f"])
        P.op("dve", lambda e: e.memset(s7[:, 0:1], 0.0), writes=["s7"])
        jk7 = ga[3]
        P.op("act", lambda e, n=n, jk7=jk7: e.activation(out=jk7[0:n, :], in_=x1f[0:n, :], func=AF.Square,
                                                        accum_out=s7[0:n, 0:1]),
             reads=["x1f", ("ga", gp, 2)], writes=[("ga", gp, 3), "s7"])
        P.op("dve", lambda e, n=n: e.tensor_scalar(out=s7[0:n, 1:2], in0=s7[0:n, 0:1], scalar1=1.0 / D, scalar2=EPS,
                                                 op0=ALU.mult, op1=ALU.add), reads=["s7"], writes=["s7b"])
        P.op("act", lambda e, n=n: e.activation(out=s7[0:n, 3:4], in_=s7[0:n, 1:2], func=AF.Ln), reads=["s7b"],
             writes=["s7d"])
        P.op("act", lambda e, n=n: e.activation(out=s7[0:n, 2:3], in_=s7[0:n, 3:4], func=AF.Exp, scale=-0.5),
             reads=["s7d"], writes=["s7c"])
        P.op("dve", lambda e, n=n, jk7=jk7: e.scalar_tensor_tensor(out=jk7[0:n, :], in0=x1f[0:n, :],
                                                                 scalar=s7[0:n, 2:3], in1=gfb[0:n, :], op0=ALU.mult,
                                                                 op1=ALU.mult),
             reads=["x1f", "s7c", "gfb", ("ga", gp, 3)], writes=[("ga", gp, 3)])
        P.op("sp", lambda e, r0=r0, n=n, jk7=jk7: e.dma_start(out=out_d[r0:r0 + n, :], in_=jk7[0:n, :]),
             reads=[("ga", gp, 3)], dma=True)
    P.barrier()
    P.emit(nc)
    return nc


def _pcol(v):
    v = np.asarray(v, np.float32)
    return np.ascontiguousarray(v.reshape(-1, 128).T)


def make_in_maps(cfg, inp):
    D = cfg["D"]; SEQ = cfg["SEQ"]; B = cfg["B"]; E = cfg["E"]; CAP = cfg["CAP"]; HALO = cfg["HALO"]
    ST = SEQ + cfg["NMETA"]; TH = ST // 2
    LW = D // 2; CW = D // 2; FF = D // 2
    NH = LW // 128; NG = CW // 128; DC = D // 128; FC = FF // 128
    f32 = np.float32
    x = np.asarray(inp["x"], f32)
    meta = np.asarray(inp["meta_tokens"], f32)
    g1 = np.asarray(inp["norm1_g"], f32)[0]
    g2 = np.asarray(inp["norm2_g"], f32)[0]
    gfin = np.asarray(inp["final_norm_g"], f32)
    lcw = np.asarray(inp["lru_conv_w"], f32)[0]
    lcb = np.asarray(inp["lru_conv_b"], f32)[0]
    lwa = np.asarray(inp["lru_w_a"], f32)[0]
    lba = np.asarray(inp["lru_b_a"], f32)[0]
    lwi = np.asarray(inp["lru_w_i"], f32)[0]
    lbi = np.asarray(inp["lru_b_i"], f32)[0]
    lam = np.asarray(inp["lru_lambda"], f32)[0]
    ccw = np.asarray(inp["conf_conv_w"], f32)[0]
    ccb = np.asarray(inp["conf_conv_b"], f32)[0]
    cng = np.asarray(inp["conf_norm_g"], f32)[0]
    cnb = np.asarray(inp["conf_norm_b"], f32)[0]
    bg = np.asarray(inp["b_gate"], f32)[0]
    bu = np.asarray(inp["b_up"], f32)[0]
    brt = np.asarray(inp["b_router"], f32)[0]

    ident = np.eye(128, dtype=f32)
    U = np.triu(np.ones((128, 128), f32), 1)
    onesM = np.full((128, 128), 1.0 / 128, f32)
    ones1 = np.ones((128, 128), f32)
    iotaC = np.tile(np.arange(CAP, dtype=f32)[None, :], (128, 1))
    eoff = np.tile((np.arange(E, dtype=f32) * CAP)[None, :], (128, 1))
    brtb = np.tile(brt[None, :], (128, 1))
    consts = np.ascontiguousarray(np.concatenate([ident, U, onesM, ones1, iotaC, eoff, brtb], axis=1))

    shared = dict(
        consts=consts,
        g1b=np.ascontiguousarray(np.tile(g1[None, :], (128, 1))),
        gfb=np.ascontiguousarray(np.tile(gfin[None, :], (128, 1))),
        w_in=np.ascontiguousarray(np.asarray(inp["w_in"], f32)[0].reshape(DC, 128, 4 * LW // 128, 128)
                                  .transpose(2, 1, 0, 3)).reshape(4 * LW // 128, 128, DC * 128),
        w_out=np.ascontiguousarray(np.asarray(inp["w_out"], f32)[0].reshape(D // 128, 128, D // 512, 512)
                                   .transpose(2, 1, 0, 3)).reshape(D // 512, 128, (D // 128) * 512),
        w_router=np.asarray(inp["w_router"], f32)[0],
        w_gate=np.ascontiguousarray(np.asarray(inp["w_gate"], f32)[0].reshape(E, DC, 128, FF // 256, 256)
                                    .transpose(0, 3, 2, 1, 4)).reshape(E * (FF // 256), 128, DC * 256),
        w_up=np.ascontiguousarray(np.asarray(inp["w_up"], f32)[0].reshape(E, DC, 128, FF // 256, 256)
                                  .transpose(0, 3, 2, 1, 4)).reshape(E * (FF // 256), 128, DC * 256),
        w_down=np.ascontiguousarray(np.asarray(inp["w_down"], f32)[0].reshape(E, FC, 128, D // 512, 512)
                                    .transpose(0, 3, 2, 1, 4)).reshape(E * (D // 512), 128, FC * 512),
        b_down=np.asarray(inp["b_down"], f32)[0],
    )

    def smallp(dirs, rev_taps):
        cols = [_pcol(g1), _pcol(g2)]
        for dr in dirs:
            cw = lcw[dr].reshape(4, NH, 128)
            cols.append(np.ascontiguousarray(cw.transpose(2, 1, 0)).reshape(128, NH * 4))
            cols.append(_pcol(lcb[dr]))
            cols.append(_pcol(lba[dr].reshape(-1)))
            cols.append(_pcol(lbi[dr].reshape(-1)))
            cols.append(_pcol(lam[dr]))
        w = ccw[::-1] if rev_taps else ccw
        cw = w.reshape(31, NG, 128)
        cols.append(np.ascontiguousarray(cw.transpose(2, 1, 0)).reshape(128, NG * 31))
        cols += [_pcol(ccb), _pcol(cng), _pcol(cnb)]
        cols.append(np.ascontiguousarray(bg.reshape(E, FC, 128).transpose(2, 0, 1)).reshape(128, E * FC))
        cols.append(np.ascontiguousarray(bu.reshape(E, FC, 128).transpose(2, 0, 1)).reshape(128, E * FC))
        return np.ascontiguousarray(np.concatenate(cols, axis=1))

    maps = []
    for b in range(B):
        S = np.concatenate([meta, x[b]], axis=0)
        for half in range(2):
            Sl = S if half == 1 else S[::-1]
            dirs = (0, 1) if half == 1 else (1, 0)
            m = dict(shared)
            m["xo"] = np.ascontiguousarray(Sl[:TH])
            m["xn"] = np.ascontiguousarray(Sl[TH - HALO:])
            m["smallp"] = smallp(dirs, rev_taps=(half == 0))
            m["wa_f"] = np.ascontiguousarray(lwa[dirs[0]].reshape(NH * 128, 128))
            m["wa_b"] = np.ascontiguousarray(lwa[dirs[1]].reshape(NH * 128, 128))
            m["wi_f"] = np.ascontiguousarray(lwi[dirs[0]].reshape(NH * 128, 128))
            m["wi_b"] = np.ascontiguousarray(lwi[dirs[1]].reshape(NH * 128, 128))
            maps.append(m)
    return maps


def assemble(cfg, results):
    D = cfg["D"]; SEQ = cfg["SEQ"]; B = cfg["B"]; NM = cfg["NMETA"]
    ST = SEQ + NM; TH = ST // 2
    out = np.empty((B, SEQ, D), np.float32)
    i = 0
    for b in range(B):
        full = np.empty((ST, D), np.float32)
        for half in range(2):
            o = np.asarray(results[i]["out"], np.float32)
            i += 1
            if half == 1:
                full[TH:] = o
            else:
                full[:TH] = o[::-1]
        out[b] = full[NM:]
    return out


_NC_CACHE = {}


def run(cfg, inp, debug=False):
    key = (tuple(sorted(cfg.items())), debug)
    if key not in _NC_CACHE:
        _NC_CACHE[key] = build_program(cfg, debug=debug)
    nc = _NC_CACHE[key]
    maps = make_in_maps(cfg, inp)
    n = len(maps)
    res = run_bass_kernel_spmd(nc, maps, core_ids=list(range(n)))
    return res


def kernel(**inputs):
    cfg = dict(REAL_CFG)
    res = run(cfg, inputs)
    return assemble(cfg, res.results)
```
